# Optimizing a Trainium2 kernel written in Bass

```python
import jax, jax.numpy as jnp
from jax import lax
import numpy as np

D_MODEL = 1024
BATCH = 4
SEQ = 4096
DEPTH = 2

CHUNK = 64
N_MIXERS = 2
N_CONV_LAYERS = (DEPTH + 1) // 2
N_GDN_LAYERS = DEPTH // 2
CONV_WIDTH = 3
GDN_HEADS = 8
GDN_HEAD_DIM = D_MODEL // GDN_HEADS
GDN_QK_DIM = GDN_HEADS * GDN_HEAD_DIM
GDN_V_DIM = GDN_HEADS * GDN_HEAD_DIM
GDN_CONV_WIDTH = 4
GDN_IN_DIM = 2 * GDN_QK_DIM + 2 * GDN_V_DIM + 2 * GDN_HEADS
N_GROUPS = 8
EXPERTS_PER_GROUP = 8
N_EXPERTS = N_GROUPS * EXPERTS_PER_GROUP
TOP_K = 2
D_EXPERT = D_MODEL // 4
D_SHARED = D_MODEL // 2
MOE_BLOCK = 128
NORM_EPS = 1e-6

kernel_name = "hybrid_conv_gdn_hier_moe_adaln"


def rmsnorm(x, w):
    xf = x.astype(jnp.float32)
    y = xf * lax.rsqrt(jnp.mean(xf * xf, axis=-1, keepdims=True) + NORM_EPS)
    return (y * w.astype(jnp.float32)).astype(x.dtype)


def l2norm(x):
    xf = x.astype(jnp.float32)
    return xf * lax.rsqrt(jnp.sum(xf * xf, axis=-1, keepdims=True) + NORM_EPS)


def causal_dwconv(x, w):
    K = w.shape[0]
    T = x.shape[1]
    xp = jnp.pad(x, ((0, 0), (K - 1, 0), (0, 0)))
    y = xp[:, 0:T] * w[0]
    for k in range(1, K):
        y = y + xp[:, k:k + T] * w[k]
    return y


def short_conv_mixer(h, w_in, conv_w, w_out):
    bcx = h @ w_in
    b, cg, xv = jnp.split(bcx, 3, axis=-1)
    y = b * causal_dwconv(cg * xv, conv_w)
    return y @ w_out


def chunk_gated_delta_rule(q, k, v, g, beta):
    out_dtype = v.dtype
    B, T, H, Dk = q.shape
    Dv = v.shape[-1]
    n = T // CHUNK

    def blocks(a):
        a = a.astype(jnp.float32).reshape((B, n, CHUNK, H) + a.shape[3:])
        return jnp.moveaxis(a, 3, 1)

    q, k, v, g, beta = (blocks(a) for a in (q, k, v, g, beta))
    gc = jnp.cumsum(g, axis=-1)
    idx = jnp.arange(CHUNK)
    causal = idx[:, None] >= idx[None, :]
    strict = idx[:, None] > idx[None, :]
    diff = gc[..., :, None] - gc[..., None, :]
    decay = jnp.exp(jnp.where(causal, diff, -jnp.inf))
    kb = k * beta[..., None]
    lower = jnp.where(strict, jnp.einsum('bhncd,bhnsd->bhncs', kb, k) * decay, 0.0)
    rhs = jnp.concatenate([v * beta[..., None], kb * jnp.exp(gc)[..., None]], axis=-1)
    sol = lax.linalg.triangular_solve(lower, rhs, left_side=True, lower=True,
                                      unit_diagonal=True)
    u, w = sol[..., :Dv], sol[..., Dv:]
    attn = jnp.einsum('bhncd,bhnsd->bhncs', q, k) * decay
    q_dec = q * jnp.exp(gc)[..., None]
    g_last = gc[..., -1]
    k_dec = k * jnp.exp(g_last[..., None] - gc)[..., None]

    def step(S, inp):
        u_n, w_n, qd_n, kd_n, a_n, gl_n = inp
        v_new = u_n - jnp.einsum('bhcd,bhde->bhce', w_n, S)
        o_n = (jnp.einsum('bhcd,bhde->bhce', qd_n, S)
               + jnp.einsum('bhcs,bhse->bhce', a_n, v_new))
        S = S * jnp.exp(gl_n)[..., None, None] + jnp.einsum('bhcd,bhce->bhde', kd_n, v_new)
        return S, o_n

    xs = tuple(jnp.moveaxis(a, 2, 0) for a in (u, w, q_dec, k_dec, attn, g_last))
    S0 = jnp.zeros((B, H, Dk, Dv), jnp.float32)
    _, o = lax.scan(step, S0, xs)
    o = jnp.transpose(o, (1, 0, 3, 2, 4)).reshape(B, T, H, Dv)
    return o.astype(out_dtype)


def gated_deltanet_mixer(h, w_in, conv_w, a_log, dt_bias, norm_w, w_out):
    B, T, _ = h.shape
    H, Dh = GDN_HEADS, GDN_HEAD_DIM
    proj = h @ w_in
    s1 = 2 * GDN_QK_DIM + GDN_V_DIM
    s2 = s1 + GDN_V_DIM
    qkv = proj[..., :s1]
    z = proj[..., s1:s2]
    b = proj[..., s2:s2 + H]
    a = proj[..., s2 + H:]
    qkv = jax.nn.silu(causal_dwconv(qkv, conv_w))
    q = qkv[..., :GDN_QK_DIM].reshape(B, T, H, Dh)
    k = qkv[..., GDN_QK_DIM:2 * GDN_QK_DIM].reshape(B, T, H, Dh)
    v = qkv[..., 2 * GDN_QK_DIM:].reshape(B, T, H, Dh)
    q = l2norm(q) * (Dh ** -0.5)
    k = l2norm(k)
    beta = jax.nn.sigmoid(b.astype(jnp.float32))
    g = -jnp.exp(a_log.astype(jnp.float32)) * jax.nn.softplus(
        a.astype(jnp.float32) + dt_bias.astype(jnp.float32))
    o = chunk_gated_delta_rule(q, k, v, g, beta)
    o = rmsnorm(o, norm_w) * jax.nn.silu(z.reshape(B, T, H, Dh))
    return o.reshape(B, T, GDN_V_DIM) @ w_out


def hier_moe(h, w_rg, b_rg, w_re, b_re, w_gate, w_up, w_down, ws_gate, ws_up, ws_down, w_sg):
    B, T, D = h.shape
    xt = h.reshape(-1, D)
    N = xt.shape[0]
    gl = (xt @ w_rg + b_rg).astype(jnp.float32)
    gp = jax.nn.softmax(gl, axis=-1)
    g_sel = jnp.argmax(gl, axis=-1)
    p_sel = jnp.take_along_axis(gp, g_sel[:, None], axis=-1)[:, 0]
    el = (xt @ w_re + b_re).astype(jnp.float32).reshape(N, N_GROUPS, EXPERTS_PER_GROUP)
    el_sel = jnp.take_along_axis(el, g_sel[:, None, None], axis=1)[:, 0]
    top_v, top_i = lax.top_k(el_sel, TOP_K)
    gate = jax.nn.softmax(top_v, axis=-1) * p_sel[:, None]
    eid = g_sel[:, None] * EXPERTS_PER_GROUP + top_i
    S = N * TOP_K
    flat_e = eid.reshape(-1)
    flat_tok = jnp.repeat(jnp.arange(N), TOP_K)
    flat_gate = gate.reshape(-1)
    order = jnp.argsort(flat_e)
    e_sorted = flat_e[order]
    tok_sorted = flat_tok[order]
    gate_sorted = flat_gate[order].astype(xt.dtype)
    counts = jnp.bincount(flat_e, length=N_EXPERTS)
    starts = jnp.cumsum(counts) - counts
    padded = (counts + MOE_BLOCK - 1) // MOE_BLOCK * MOE_BLOCK
    pends = jnp.cumsum(padded)
    pstarts = pends - padded
    dest = pstarts[e_sorted] + (jnp.arange(S) - starts[e_sorted])
    n_blocks = -(-S // MOE_BLOCK) + N_EXPERTS
    P = n_blocks * MOE_BLOCK
    buf = jnp.zeros((P, D), xt.dtype).at[dest].set(xt[tok_sorted])
    block_e = jnp.minimum(
        jnp.searchsorted(pends, jnp.arange(n_blocks) * MOE_BLOCK, side='right'), N_EXPERTS - 1)

    def expert_block(args):
        xb, e = args
        hid = jax.nn.silu(xb @ w_gate[e]) * (xb @ w_up[e])
        return hid @ w_down[e]

    out = lax.map(expert_block, (buf.reshape(n_blocks, MOE_BLOCK, D), block_e)).reshape(P, D)
    y = jnp.zeros((N, D), xt.dtype).at[tok_sorted].add(out[dest] * gate_sorted[:, None])
    ys = (jax.nn.silu(xt @ ws_gate) * (xt @ ws_up)) @ ws_down
    y = y + jax.nn.sigmoid(xt @ w_sg) * ys
    return y.reshape(B, T, D)


def setup_inputs(seed: int = 0) -> dict:
    key = jax.random.key(seed)
    ks = jax.random.split(key, 32)
    D = D_MODEL
    nrm = jax.random.normal
    f32 = jnp.float32
    dt = jnp.exp(jax.random.uniform(ks[11], (N_GDN_LAYERS, GDN_HEADS), f32,
                                    np.log(1e-3), np.log(1e-1)))
    return {
        "x": nrm(ks[0], (BATCH, SEQ, D), f32),
        "c": nrm(ks[1], (BATCH, D), f32),
        "ada_w": nrm(ks[2], (DEPTH, D, 6 * D), f32) * (0.3 * D ** -0.5),
        "ada_b": nrm(ks[3], (DEPTH, 6 * D), f32) * 0.02,
        "norm_w": 1.0 + 0.05 * nrm(ks[4], (DEPTH, 4, D), f32),
        "conv_in_w": nrm(ks[5], (N_CONV_LAYERS, D, 3 * D), f32) * D ** -0.5,
        "conv_w": nrm(ks[6], (N_CONV_LAYERS, CONV_WIDTH, D), f32) * CONV_WIDTH ** -0.5,
        "conv_out_w": nrm(ks[7], (N_CONV_LAYERS, D, D), f32) * D ** -0.5,
        "gdn_in_w": nrm(ks[8], (N_GDN_LAYERS, D, GDN_IN_DIM), f32) * D ** -0.5,
        "gdn_conv_w": nrm(ks[9], (N_GDN_LAYERS, GDN_CONV_WIDTH, 2 * GDN_QK_DIM + GDN_V_DIM), f32) * 0.5,
        "gdn_a_log": jnp.log(jax.random.uniform(ks[10], (N_GDN_LAYERS, GDN_HEADS), f32, 1.0, 16.0)),
        "gdn_dt_bias": dt + jnp.log(-jnp.expm1(-dt)),
        "gdn_norm_w": 1.0 + 0.05 * nrm(ks[12], (N_GDN_LAYERS, GDN_HEAD_DIM), f32),
        "gdn_out_w": nrm(ks[13], (N_GDN_LAYERS, GDN_V_DIM, D), f32) * GDN_V_DIM ** -0.5,
        "moe_group_w": nrm(ks[14], (DEPTH, D, N_GROUPS), f32) * D ** -0.5,
        "moe_group_b": nrm(ks[15], (DEPTH, N_GROUPS), f32) * 0.01,
        "moe_expert_w": nrm(ks[16], (DEPTH, D, N_EXPERTS), f32) * D ** -0.5,
        "moe_expert_b": nrm(ks[17], (DEPTH, N_EXPERTS), f32) * 0.01,
        "moe_w_gate": nrm(ks[18], (DEPTH, N_EXPERTS, D, D_EXPERT), f32) * D ** -0.5,
        "moe_w_up": nrm(ks[19], (DEPTH, N_EXPERTS, D, D_EXPERT), f32) * D ** -0.5,
        "moe_w_down": nrm(ks[20], (DEPTH, N_EXPERTS, D_EXPERT, D), f32) * D_EXPERT ** -0.5,
        "shared_w_gate": nrm(ks[21], (DEPTH, D, D_SHARED), f32) * D ** -0.5,
        "shared_w_up": nrm(ks[22], (DEPTH, D, D_SHARED), f32) * D ** -0.5,
        "shared_w_down": nrm(ks[23], (DEPTH, D_SHARED, D), f32) * D_SHARED ** -0.5,
        "shared_gate_w": nrm(ks[24], (DEPTH, D, 1), f32) * D ** -0.5,
    }


def reference(x, c, ada_w, ada_b, norm_w, conv_in_w, conv_w, conv_out_w,
              gdn_in_w, gdn_conv_w, gdn_a_log, gdn_dt_bias, gdn_norm_w, gdn_out_w,
              moe_group_w, moe_group_b, moe_expert_w, moe_expert_b,
              moe_w_gate, moe_w_up, moe_w_down,
              shared_w_gate, shared_w_up, shared_w_down, shared_gate_w):
    cs = jax.nn.silu(c)
    for i in range(DEPTH):
        mod = (cs @ ada_w[i] + ada_b[i])[:, None, :]
        sh1, sc1, gt1, sh2, sc2, gt2 = jnp.split(mod, 6, axis=-1)
        h = rmsnorm(x, norm_w[i, 0]) * (1.0 + sc1) + sh1
        j = i // N_MIXERS
        if i % N_MIXERS == 0:
            y = short_conv_mixer(h, conv_in_w[j], conv_w[j], conv_out_w[j])
        else:
            y = gated_deltanet_mixer(h, gdn_in_w[j], gdn_conv_w[j], gdn_a_log[j],
                                     gdn_dt_bias[j], gdn_norm_w[j], gdn_out_w[j])
        x = x + gt1 * rmsnorm(y, norm_w[i, 1])
        h = rmsnorm(x, norm_w[i, 2]) * (1.0 + sc2) + sh2
        y = hier_moe(h, moe_group_w[i], moe_group_b[i], moe_expert_w[i], moe_expert_b[i],
                     moe_w_gate[i], moe_w_up[i], moe_w_down[i],
                     shared_w_gate[i], shared_w_up[i], shared_w_down[i], shared_gate_w[i])
        x = x + gt2 * rmsnorm(y, norm_w[i, 3])
    return x
```

```python
import types
import numpy as np
from contextlib import ExitStack
import concourse.bass as bass
import concourse.mybir as mybir
from concourse.bass_utils import run_bass_kernel_spmd

F32 = mybir.dt.float32
BF16 = mybir.dt.bfloat16
I32 = mybir.dt.int32
U8 = mybir.dt.uint8
AF = mybir.ActivationFunctionType
ALU = mybir.AluOpType
AX = mybir.AxisListType

D = 1024
NCORES = 8
SEQ = 4096
TOK = 2048
NT = TOK // 128
EPS = 1e-6
NEXP = 64
CAP = 256
NSLOT = NEXP * CAP
BIG = 30000.0
BIGC = 4194304.0
OVF = 1048576.0


class Clock:
    def __init__(self, sem, name):
        self.sem = sem
        self.name = name
        self.count = 0


class Region:
    __slots__ = ("name", "w", "r")

    def __init__(self, name):
        self.name = name
        self.w = None
        self.r = []


def _freeze(fn):
    if fn.__closure__ is None:
        return fn
    cells = tuple(types.CellType(c.cell_contents) for c in fn.__closure__)
    return types.FunctionType(fn.__code__, fn.__globals__, fn.__name__, fn.__defaults__, cells)


def _prune(lst):
    best = {}
    for clk, cnt in lst:
        if best.get(clk, 0) < cnt:
            best[clk] = cnt
    return list(best.items())


class Eng:
    def __init__(self, ctx, name, clock, same_engine_sync=True):
        self.ctx = ctx
        self.name = name
        self.clock = clock
        self.waited = {}
        self.same_engine_sync = same_engine_sync
        self.q = []

    def emit(self, h):
        for a in self.q:
            if a[0] == 0:
                h.wait_ge(a[1], a[2])
            else:
                a[1](h).then_inc(a[2], a[3])

    def _need(self, dep, needs):
        if dep is None:
            return
        clk, cnt = dep
        if clk is self.clock and not self.same_engine_sync:
            return
        if self.waited.get(clk, 0) >= cnt:
            return
        if needs.get(clk, 0) < cnt:
            needs[clk] = cnt

    def deps(self, reads, writes):
        needs = {}
        for r in reads:
            self._need(r.w, needs)
        for w in writes:
            self._need(w.w, needs)
            for d in w.r:
                self._need(d, needs)
        for clk, cnt in needs.items():
            self.q.append((0, clk.sem, cnt))
            self.waited[clk] = cnt

    def _mark(self, me, reads, writes):
        for r in reads:
            r.r.append(me)
            if len(r.r) > 24:
                r.r = _prune(r.r)
        for w in writes:
            w.w = me
            w.r = []

    def op(self, fn, reads=(), writes=()):
        self.deps(reads, writes)
        self.clock.count += 1
        self.q.append((1, _freeze(fn), self.clock.sem, 1))
        self._mark((self.clock, self.clock.count), reads, writes)
        self.ctx.nops += 1

    def dma(self, stream, fn, reads=(), writes=(), inc=16):
        self.deps(reads, writes)
        stream.count += inc
        self.q.append((1, _freeze(fn), stream.sem, inc))
        self._mark((stream, stream.count), reads, writes)
        self.ctx.ndma += 1

    def wait_clock(self, clk):
        if clk.count > 0 and self.waited.get(clk, 0) < clk.count:
            if clk is self.clock and not self.same_engine_sync:
                return
            self.q.append((0, clk.sem, clk.count))
            self.waited[clk] = clk.count


class Ctx:
    def __init__(self, nc, stack):
        self.nc = nc
        self.stack = stack
        self.nops = 0
        self.ndma = 0
        self._n = 0
        self.clocks = []
        self.pe = Eng(self, "pe", self.clock("pe"), same_engine_sync=False)
        self.act = Eng(self, "act", self.clock("act"))
        self.dve = Eng(self, "dve", self.clock("dve"))
        self.pool = Eng(self, "pool", self.clock("pool"))
        self.sp = Eng(self, "sp", self.clock("sp"))
        self.engs = [self.pe, self.act, self.dve, self.pool, self.sp]

    def clock(self, name):
        sem = self.stack.enter_context(self.nc.semaphore(f"s{self._n}_{name}"))
        self._n += 1
        c = Clock(sem, name)
        self.clocks.append(c)
        return c

    def stream(self, name="d"):
        return self.clock(name)

    def region(self, name="r"):
        return Region(name)

    def regions(self, n, name="r"):
        return [Region(f"{name}{i}") for i in range(n)]

    def barrier(self):
        for e in self.engs:
            for c in self.clocks:
                e.wait_clock(c)

    def finish(self):
        with self.nc.Block() as block:
            @block.tensor
            def _(h):
                self.pe.emit(h)

            @block.scalar
            def _(h):
                self.act.emit(h)

            @block.vector
            def _(h):
                self.dve.emit(h)

            @block.gpsimd
            def _(h):
                self.pool.emit(h)

            @block.sync
            def _(h):
                self.sp.emit(h)


_DTSIZE = {F32: 4, BF16: 2, I32: 4, U8: 1}


class Arena:
    def __init__(self, tensor, size):
        self.t = tensor
        self.size = size
        self.off = 0
        self.peak = 0

    def alloc(self, shape, dt):
        assert shape[0] == 128
        n = 1
        for s in shape[1:]:
            n *= s
        nb = n * _DTSIZE[dt]
        nb = (nb + 63) // 64 * 64
        assert self.off + nb <= self.size, f"arena overflow {self.off + nb} > {self.size}"
        v = self.t[:, self.off:self.off + nb].bitcast(dt)
        if n * _DTSIZE[dt] != nb:
            v = v[:, 0:n]
        self.off += nb
        self.peak = max(self.peak, self.off)
        if len(shape) == 3:
            v = v.rearrange("p (a b) -> p a b", a=shape[1])
        elif len(shape) == 4:
            v = v.rearrange("p (a b c) -> p a b c", a=shape[1], b=shape[2])
        return v

    def mark(self):
        return self.off

    def release(self, m):
        self.off = m


class Prog:
    def __init__(self, nc, st):
        self.nc = nc
        self.st = st
        self.K = Ctx(nc, st)
        K = self.K
        self.pe, self.act, self.dve, self.pool, self.sp = K.pe, K.act, K.dve, K.pool, K.sp
        ARENA = 200 * 1024
        self.arena = Arena(st.enter_context(nc.sbuf_tensor("arena", [128, ARENA], U8)), ARENA)
        self.pb = [st.enter_context(nc.psum_tensor(f"pb{i}", [128, 512], F32)) for i in range(8)]
        self.rpb = K.regions(8, "pb")
        self._ns = 0
        self._free = []
        self._live = []
        A = self.arena
        self.identf = A.alloc([128, 128], F32)
        self.identb = A.alloc([128, 128], BF16)
        self.rconst = K.region("const")
        pool = self.pool
        pool.op(lambda h: h.memset(self.identf, 0.0), writes=[self.rconst])
        pool.op(lambda h: h.affine_select(out=self.identf, in_=self.identf, pattern=[[-1, 128]],
                                          compare_op=ALU.not_equal, fill=1.0, base=0, channel_multiplier=1),
                reads=[self.rconst], writes=[self.rconst])
        pool.op(lambda h: h.tensor_copy(out=self.identb, in_=self.identf), reads=[self.rconst], writes=[self.rconst])

    def stream(self):
        if self._free:
            c = self._free.pop()
        else:
            self._ns += 1
            c = self.K.stream(f"d{self._ns}")
        self._live.append(c)
        return c

    def stream_mark(self):
        return len(self._live)

    def stream_release(self, m):
        self._free.extend(self._live[m:])
        del self._live[m:]

    def dram(self, name, shape, dt, kind):
        return self.nc.dram_tensor(name, shape, dt, kind=kind).ap()


def rms_rstd(P, src_ap, src_reg, junk, rjunk, col, rcol, ncols=D):
    P.act.op(lambda h: h.activation(out=junk, in_=src_ap, func=AF.Square, accum_out=col),
             reads=[src_reg], writes=[rjunk, rcol])
    P.act.op(lambda h: h.activation(out=col, in_=col, func=AF.Sqrt, scale=1.0 / ncols, bias=EPS),
             reads=[rcol], writes=[rcol])
    P.dve.op(lambda h: h.reciprocal(out=col, in_=col), reads=[rcol], writes=[rcol])


def phase_adaln(P, mod, rmod, c_col, ada_w, ada_b, norm_w):
    K, A = P.K, P.arena
    pe, act, dve, pool, sp = P.pe, P.act, P.dve, P.pool, P.sp
    m0 = A.mark()
    sm0 = P.stream_mark()
    cs = A.alloc([128, 8], F32)
    csb = A.alloc([128, 8], BF16)
    csbb = A.alloc([128, 8, 128], BF16)
    nwb = A.alloc([128, 4, D], F32)
    bb = A.alloc([128, 6 * D], F32)
    wt = [A.alloc([128, 8, 512], BF16) for _ in range(2)]
    rcs, rnwb, rbb = K.region(), K.region(), K.region()
    rwt = K.regions(2, "wt")
    swt = [P.stream(), P.stream()]
    s0 = P.stream()
    sp.dma(s0, lambda h: h.dma_start(out=cs, in_=c_col), writes=[rcs])
    s1 = P.stream()
    sp.dma(s1, lambda h: h.dma_start(out=bb, in_=ada_b.partition_broadcast(128)), writes=[rbb])
    s2 = P.stream()
    sp.dma(s2, lambda h: h.dma_start(out=nwb.rearrange("p a b -> p (a b)"),
                                     in_=norm_w.rearrange("a b -> (a b)").rearrange("(o n) -> o n", o=1).partition_broadcast(128)),
           writes=[rnwb])
    act.op(lambda h: h.activation(out=csb, in_=cs, func=AF.Silu), reads=[rcs], writes=[rcs])
    for k in range(8):
        dve.op(lambda h, k=k: h.tensor_copy(out=csbb[:, k, :], in_=csb[:, k:k + 1].to_broadcast([128, 128])),
               reads=[rcs], writes=[rcs])
    dest = [1, 0, 2, 4, 3, 5]
    for ct in range(12):
        s = ct % 2
        pool.dma(swt[s], lambda h, s=s, ct=ct: h.dma_start(
            out=wt[s], in_=ada_w[:, ct * 512:(ct + 1) * 512].rearrange("(k p) n -> p k n", p=128)),
            writes=[rwt[s]])
        bank = P.pb[s]
        for k in range(8):
            pe.op(lambda h, k=k, s=s, bank=bank: h.matmul(bank[:, :], lhsT=csbb[:, k, :], rhs=wt[s][:, k, :],
                                                          start=(k == 0), stop=(k == 7)),
                  reads=[rcs, rwt[s]], writes=[P.rpb[s]])
        di = dest[ct // 2]
        half = ct % 2
        dve.op(lambda h, bank=bank, di=di, half=half, ct=ct: h.tensor_tensor(
            out=mod[:, di, half * 512:(half + 1) * 512], in0=bank[:, :], in1=bb[:, ct * 512:(ct + 1) * 512], op=ALU.add),
            reads=[P.rpb[s], rbb], writes=[rmod[di]])
    dve.op(lambda h: h.scalar_tensor_tensor(out=mod[:, 0, :], in0=mod[:, 0, :], scalar=1.0, in1=nwb[:, 0, :],
                                            op0=ALU.add, op1=ALU.mult), reads=[rmod[0], rnwb], writes=[rmod[0]])
    dve.op(lambda h: h.tensor_tensor(out=mod[:, 2, :], in0=mod[:, 2, :], in1=nwb[:, 1, :], op=ALU.mult),
           reads=[rmod[2], rnwb], writes=[rmod[2]])
    dve.op(lambda h: h.scalar_tensor_tensor(out=mod[:, 3, :], in0=mod[:, 3, :], scalar=1.0, in1=nwb[:, 2, :],
                                            op0=ALU.add, op1=ALU.mult), reads=[rmod[3], rnwb], writes=[rmod[3]])
    dve.op(lambda h: h.tensor_tensor(out=mod[:, 5, :], in0=mod[:, 5, :], in1=nwb[:, 3, :], op=ALU.mult),
           reads=[rmod[5], rnwb], writes=[rmod[5]])
    K.barrier()
    A.release(m0)
    P.stream_release(sm0)


def phase_conv(P, xres, rx, mod, rmod, x_halo, hflag_in, w_in, cw_in, w_out):
    K, A = P.K, P.arena
    pe, act, dve, pool, sp = P.pe, P.act, P.dve, P.pool, P.sp
    m0 = A.mark()
    sm0 = P.stream_mark()
    Win = A.alloc([128, 8, 3 * D], BF16)
    Wout = A.alloc([128, 8, D], BF16)
    hT = A.alloc([128, 8, 512], BF16)
    hTh = A.alloc([128, 8, 2], BF16)
    ycT = A.alloc([128, 8, 512], BF16)
    hb = [A.alloc([128, D], BF16) for _ in range(2)]
    xh = A.alloc([128, D], F32)
    ysb = xh
    u = [A.alloc([128, 514], F32) for _ in range(2)]
    t1 = [A.alloc([128, 512], F32) for _ in range(2)]
    pcsb = A.alloc([128, 512], F32)
    junk = A.alloc([128, D], BF16)
    tmp = A.alloc([128, D], F32)
    cw = A.alloc([128, 24], F32)
    hflag = A.alloc([128, 1], F32)
    uh = A.alloc([128, 8, 2], F32)
    cols = A.alloc([128, 4], F32)

    rWin = K.regions(3, "win")
    rWout, rhT, rhTh, rycT, rxh, rpcsb, rjunk, rtmp, rcw, ruh, rcols = (K.region() for _ in range(11))
    rhb = K.regions(2, "hb")
    ru = K.regions(2, "u")
    rt1 = K.regions(2, "t1")

    for i in range(3):
        s = P.stream()
        pool.dma(s, lambda h, i=i: h.dma_start(
            out=Win[:, :, i * D:(i + 1) * D], in_=w_in[:, i * D:(i + 1) * D].rearrange("(k p) n -> p k n", p=128)),
            writes=[rWin[i]])
    s = P.stream()
    pool.dma(s, lambda h: h.dma_start(out=Wout, in_=w_out.rearrange("(k p) n -> p k n", p=128)), writes=[rWout])
    s = P.stream()
    sp.dma(s, lambda h: h.dma_start(out=cw, in_=cw_in), writes=[rcw])
    s = P.stream()
    sp.dma(s, lambda h: h.dma_start(out=hflag, in_=hflag_in), writes=[rcw])
    pool.op(lambda h: h.memset(xh, 0.0), writes=[rxh])
    s = P.stream()
    sp.dma(s, lambda h: h.dma_start(out=xh[0:2, :], in_=x_halo), writes=[rxh])

    def make_h(src, rsrc, slot):
        rms_rstd(P, src, rsrc, junk, rjunk, cols[:, 0:1], rcols)
        dve.op(lambda h: h.scalar_tensor_tensor(out=tmp, in0=src, scalar=cols[:, 0:1], in1=mod[:, 0, :],
                                                op0=ALU.mult, op1=ALU.mult),
               reads=[rsrc, rcols, rmod[0]], writes=[rtmp])
        dve.op(lambda h: h.tensor_tensor(out=hb[slot], in0=tmp, in1=mod[:, 1, :], op=ALU.add),
               reads=[rtmp, rmod[1]], writes=[rhb[slot]])

    def transpose_h(slot, bank, dst, rdst, ncol):
        pst = P.pb[bank][:, :].bitcast(BF16).rearrange("p (k n) -> p k n", k=8)
        for k in range(8):
            pe.op(lambda h, k=k: h.transpose(out=pst[:, k, :], in_=hb[slot][:, k * 128:(k + 1) * 128], identity=P.identb),
                  reads=[rhb[slot], P.rconst], writes=[P.rpb[bank]])
        act.op(lambda h: h.activation(out=dst, in_=pst[:, :, 0:ncol], func=AF.Copy),
               reads=[P.rpb[bank]], writes=[rdst])

    make_h(xh, rxh, 0)
    transpose_h(0, 4, hTh, rhTh, 2)
    for j in range(8):
        pc, px = P.pb[2], P.pb[3]
        for k in range(8):
            pe.op(lambda h, k=k, j=j: h.matmul(pc[:, 0:2], lhsT=Win[:, k, D + j * 128:D + (j + 1) * 128], rhs=hTh[:, k, :],
                                               start=(k == 0), stop=(k == 7)),
                  reads=[rWin[1], rhTh], writes=[P.rpb[2]])
        for k in range(8):
            pe.op(lambda h, k=k, j=j: h.matmul(px[:, 0:2], lhsT=Win[:, k, 2 * D + j * 128:2 * D + (j + 1) * 128], rhs=hTh[:, k, :],
                                               start=(k == 0), stop=(k == 7)),
                  reads=[rWin[2], rhTh], writes=[P.rpb[3]])
        act.op(lambda h: h.activation(out=pcsb[:, 0:2], in_=pc[:, 0:2], func=AF.Copy), reads=[P.rpb[2]], writes=[rpcsb])
        dve.op(lambda h, j=j: h.scalar_tensor_tensor(out=uh[:, j, :], in0=pcsb[:, 0:2], scalar=hflag[:, 0:1], in1=px[:, 0:2],
                                                     op0=ALU.mult, op1=ALU.mult),
               reads=[rpcsb, P.rpb[3], rcw], writes=[ruh])

    for tt in range(TOK // 512):
        for q in range(4):
            ti = tt * 4 + q
            slot = ti % 2
            make_h(xres[:, ti, :], rx[ti], slot)
            transpose_h(slot, 4 + (ti % 2), hT[:, :, q * 128:(q + 1) * 128], rhT, 128)
        for j in range(8):
            banks = (1, 2, 3) if j % 2 == 0 else (6, 7, 0)
            pbk, pck, pxk = (P.pb[b] for b in banks)
            for which, bk in enumerate(banks):
                for k in range(8):
                    pe.op(lambda h, k=k, j=j, which=which, bk=bk: h.matmul(
                        P.pb[bk][:, :], lhsT=Win[:, k, which * D + j * 128:which * D + (j + 1) * 128], rhs=hT[:, k, :],
                        start=(k == 0), stop=(k == 7)),
                        reads=[rWin[which], rhT], writes=[P.rpb[bk]])
            us = u[j % 2]
            rus = ru[j % 2]
            ts = t1[j % 2]
            rts = rt1[j % 2]
            act.op(lambda h, pck=pck: h.activation(out=pcsb, in_=pck[:, :], func=AF.Copy), reads=[P.rpb[banks[1]]], writes=[rpcsb])
            dve.op(lambda h, us=us, pxk=pxk: h.tensor_tensor(out=us[:, 2:514], in0=pcsb, in1=pxk[:, :], op=ALU.mult),
                   reads=[rpcsb, P.rpb[banks[2]]], writes=[rus])
            pool.op(lambda h, us=us, j=j: h.tensor_copy(out=us[:, 0:2], in_=uh[:, j, :]), reads=[ruh], writes=[rus])
            dve.op(lambda h, us=us, ts=ts, j=j: h.tensor_scalar(out=ts, in0=us[:, 2:514], scalar1=cw[:, j * 3 + 2:j * 3 + 3], scalar2=None,
                                                                op0=ALU.mult), reads=[rus, rcw], writes=[rts])
            dve.op(lambda h, us=us, ts=ts, j=j: h.scalar_tensor_tensor(out=ts, in0=us[:, 1:513], scalar=cw[:, j * 3 + 1:j * 3 + 2], in1=ts,
                                                                       op0=ALU.mult, op1=ALU.add), reads=[rus, rcw, rts], writes=[rts])
            dve.op(lambda h, us=us, ts=ts, j=j: h.scalar_tensor_tensor(out=ts, in0=us[:, 0:512], scalar=cw[:, j * 3:j * 3 + 1], in1=ts,
                                                                       op0=ALU.mult, op1=ALU.add), reads=[rus, rcw, rts], writes=[rts])
            dve.op(lambda h, ts=ts, j=j, pbk=pbk: h.tensor_tensor(out=ycT[:, j, :], in0=ts, in1=pbk[:, :], op=ALU.mult),
                   reads=[rts, P.rpb[banks[0]]], writes=[rycT])
            pool.op(lambda h, us=us, j=j: h.tensor_copy(out=uh[:, j, :], in_=us[:, 512:514]), reads=[rus], writes=[ruh])
        for q in range(4):
            ti = tt * 4 + q
            for nh in range(2):
                bank = 4 + nh
                for k in range(8):
                    pe.op(lambda h, k=k, q=q, nh=nh, bank=bank: h.matmul(
                        P.pb[bank][:, :], lhsT=ycT[:, k, q * 128:(q + 1) * 128], rhs=Wout[:, k, nh * 512:(nh + 1) * 512],
                        start=(k == 0), stop=(k == 7)),
                        reads=[rycT, rWout], writes=[P.rpb[bank]])
                act.op(lambda h, nh=nh, bank=bank: h.activation(out=ysb[:, nh * 512:(nh + 1) * 512], in_=P.pb[bank][:, :], func=AF.Copy),
                       reads=[P.rpb[bank]], writes=[rxh])
            rms_rstd(P, ysb, rxh, junk, rjunk, cols[:, 1:2], rcols)
            dve.op(lambda h: h.scalar_tensor_tensor(out=tmp, in0=ysb, scalar=cols[:, 1:2], in1=mod[:, 2, :],
                                                    op0=ALU.mult, op1=ALU.mult),
                   reads=[rxh, rcols, rmod[2]], writes=[rtmp])
            dve.op(lambda h, ti=ti: h.tensor_tensor(out=xres[:, ti, :], in0=xres[:, ti, :], in1=tmp, op=ALU.add),
                   reads=[rx[ti], rtmp], writes=[rx[ti]])
    K.barrier()
    A.release(m0)
    P.stream_release(sm0)


def phase_moe(P, xres, rx, mod, rmod, wr_in, rb_in, ec_in, wg_in, wu_in, wd_in, sg_in, su_in, sd_in, xg, og, stop=None):
    K, A = P.K, P.arena
    pe, act, dve, pool, sp = P.pe, P.act, P.dve, P.pool, P.sp
    m0 = A.mark()
    sm0 = P.stream_mark()
    wr = A.alloc([128, 8, 73], F32)
    rbias = A.alloc([128, 72], F32)
    eC = A.alloc([128, 65], F32)
    cnt = A.alloc([128, 64], F32)
    idx = A.alloc([128, 2 * NT], I32)
    gts = A.alloc([128, 3 * NT], F32)
    triU = A.alloc([128, 128], BF16)
    ones = A.alloc([128, 128], BF16)
    h2T = A.alloc([128, 8, TOK], BF16)
    rwr, rcnt, rgts, rtri = (K.region() for _ in range(4))
    ridx = K.regions(NT, "idx")
    rh2T = K.regions(NT, "h2T")
    m1 = A.mark()
    hf = A.alloc([128, D], F32)
    hb = [A.alloc([128, D], BF16) for _ in range(2)]
    h2Tf = A.alloc([128, 8, 128], F32)
    junk = A.alloc([128, D], BF16)
    tmp = A.alloc([128, D], F32)
    sm = A.alloc([128, 1024], F32)
    rhf, rh2Tf, rjunk, rtmp = (K.region() for _ in range(4))
    rhb = K.regions(2, "hb")
    rsm = K.region()

    s = P.stream()
    sp.dma(s, lambda h: h.dma_start(out=wr, in_=wr_in.rearrange("(k p) n -> p k n", p=128)), writes=[rwr])
    s = P.stream()
    sp.dma(s, lambda h: h.dma_start(out=rbias, in_=rb_in.partition_broadcast(128)), writes=[rwr])
    s = P.stream()
    sp.dma(s, lambda h: h.dma_start(out=eC, in_=ec_in), writes=[rwr])
    pool.op(lambda h: h.memset(cnt, 0.0), writes=[rcnt])
    rogz = K.region("ogz")
    pool.op(lambda h: h.memset(junk, 0.0), writes=[rjunk])
    sz = P.stream()
    sp.dma(sz, lambda h: h.dma_start(out=og[NSLOT:NSLOT + 128, :], in_=junk), reads=[rjunk], writes=[rogz])
    pool.op(lambda h: h.memset(tmp[:, 0:128], 0.0), writes=[rtmp])
    pool.op(lambda h: h.affine_select(out=tmp[:, 0:128], in_=tmp[:, 0:128], pattern=[[-1, 128]], compare_op=ALU.is_ge,
                                      fill=1.0, base=0, channel_multiplier=1), reads=[rtmp], writes=[rtmp])
    pool.op(lambda h: h.tensor_copy(out=triU, in_=tmp[:, 0:128]), reads=[rtmp], writes=[rtri])
    pool.op(lambda h: h.memset(ones, 1.0), writes=[rtri])

    o = [0]

    def col(n):
        v = sm[:, o[0]:o[0] + n]
        o[0] += n
        return v
    lg = col(72)
    gmax, ngmax, sumexp, psel = col(1), col(1), col(1), col(1)
    goh, m18, ejunk = col(8), col(8), col(8)
    elm = col(64)
    top8 = col(8)
    sel = col(64)
    nv1, dcol, e2, gsc = col(1), col(1), col(1), col(1)
    ex = col(64)
    G = col(64)
    pos = col(64)
    val = col(64)
    val2 = col(64)
    mhi = col(64)
    vmax, vmax2, shi, slo, ghi, gtot = col(1), col(1), col(1), col(1), col(1), col(1)
    cols = col(4)
    selb = A.alloc([128, 64], BF16)

    sxg = [P.stream(), P.stream()]
    rxg = K.regions(2 * NT, "xg")

    for ti in range(NT):
        slot = ti % 2
        src = xres[:, ti, :]
        rms_rstd(P, src, rx[ti], junk, rjunk, cols[:, 0:1], rsm)
        dve.op(lambda h, src=src: h.scalar_tensor_tensor(out=tmp, in0=src, scalar=cols[:, 0:1], in1=mod[:, 3, :],
                                                         op0=ALU.mult, op1=ALU.mult),
               reads=[rx[ti], rsm, rmod[3]], writes=[rtmp])
        dve.op(lambda h: h.tensor_tensor(out=hf, in0=tmp, in1=mod[:, 4, :], op=ALU.add),
               reads=[rtmp, rmod[4]], writes=[rhf])
        act.op(lambda h, slot=slot: h.activation(out=hb[slot], in_=hf, func=AF.Copy), reads=[rhf], writes=[rhb[slot]])
        pst = P.pb[0][:, :].bitcast(BF16).rearrange("p (k n) -> p k n", k=8)
        for k in range(8):
            pe.op(lambda h, k=k, slot=slot: h.transpose(out=pst[:, k, :], in_=hb[slot][:, k * 128:(k + 1) * 128], identity=P.identb),
                  reads=[rhb[slot], P.rconst], writes=[P.rpb[0]])
        act.op(lambda h, ti=ti: h.activation(out=h2T[:, :, ti * 128:(ti + 1) * 128], in_=pst, func=AF.Copy),
               reads=[P.rpb[0]], writes=[rh2T[ti]])
        for k in range(8):
            bank = 1 + k // 4
            pe.op(lambda h, k=k, bank=bank: h.transpose(out=P.pb[bank][:, (k % 4) * 128:(k % 4 + 1) * 128],
                                                        in_=hf[:, k * 128:(k + 1) * 128], identity=P.identf),
                  reads=[rhf, P.rconst], writes=[P.rpb[bank]])
        for half in range(2):
            act.op(lambda h, half=half: h.activation(out=h2Tf[:, half * 4:(half + 1) * 4, :],
                                                     in_=P.pb[1 + half][:, :].rearrange("p (k n) -> p k n", k=4), func=AF.Copy),
                   reads=[P.rpb[1 + half]], writes=[rh2Tf])
        for k in range(8):
            pe.op(lambda h, k=k: h.matmul(P.pb[3][:, 0:73], lhsT=h2Tf[:, k, :], rhs=wr[:, k, :], start=(k == 0), stop=(k == 7)),
                  reads=[rh2Tf, rwr], writes=[P.rpb[3]])
        R3 = [P.rpb[3]]
        S = [rsm]
        act.op(lambda h: h.activation(out=lg, in_=P.pb[3][:, 0:72], func=AF.Copy), reads=R3, writes=S)
        dve.op(lambda h: h.tensor_tensor(out=lg, in0=lg, in1=rbias, op=ALU.add), reads=S + [rwr], writes=S)
        act.op(lambda h, ti=ti: h.activation(out=gts[:, 2 * NT + ti:2 * NT + ti + 1], in_=P.pb[3][:, 72:73], func=AF.Sigmoid),
               reads=R3, writes=[rgts])
        dve.op(lambda h: h.tensor_reduce(out=gmax, in_=lg[:, 0:8], axis=AX.X, op=ALU.max), reads=S, writes=S)
        dve.op(lambda h: h.tensor_scalar(out=goh, in0=lg[:, 0:8], scalar1=gmax, scalar2=None, op0=ALU.is_equal), reads=S, writes=S)
        dve.op(lambda h: h.tensor_scalar(out=ngmax, in0=gmax, scalar1=-1.0, scalar2=None, op0=ALU.mult), reads=S, writes=S)
        act.op(lambda h: h.activation(out=ejunk, in_=lg[:, 0:8], func=AF.Exp, bias=ngmax, scale=1.0, accum_out=sumexp), reads=S, writes=S)
        dve.op(lambda h: h.reciprocal(out=psel, in_=sumexp), reads=S, writes=S)
        dve.op(lambda h: h.tensor_scalar(out=m18, in0=goh, scalar1=BIG, scalar2=-BIG, op0=ALU.mult, op1=ALU.add), reads=S, writes=S)
        dve.op(lambda h: h.tensor_tensor(out=elm.rearrange("p (g e) -> p g e", g=8), in0=lg[:, 8:72].rearrange("p (g e) -> p g e", g=8),
                                         in1=m18.rearrange("p (g o) -> p g o", o=1).to_broadcast([128, 8, 8]), op=ALU.add), reads=S, writes=S)
        dve.op(lambda h: h.max(out=top8, in_=elm), reads=S, writes=S)
        dve.op(lambda h: h.tensor_scalar(out=sel, in0=elm, scalar1=top8[:, 1:2], scalar2=None, op0=ALU.is_ge), reads=S, writes=S)
        dve.op(lambda h: h.tensor_copy(out=selb, in_=sel), reads=S, writes=S)
        dve.op(lambda h: h.tensor_scalar(out=nv1, in0=top8[:, 0:1], scalar1=-1.0, scalar2=None, op0=ALU.mult), reads=S, writes=S)
        dve.op(lambda h: h.tensor_scalar(out=ex, in0=elm, scalar1=nv1, scalar2=-80.0, op0=ALU.add, op1=ALU.max), reads=S, writes=S)
        act.op(lambda h: h.activation(out=ex, in_=ex, func=AF.Exp), reads=S, writes=S)
        dve.op(lambda h: h.tensor_tensor(out=dcol, in0=top8[:, 1:2], in1=top8[:, 0:1], op=ALU.subtract), reads=S, writes=S)
        act.op(lambda h: h.activation(out=e2, in_=dcol, func=AF.Exp), reads=S, writes=S)
        dve.op(lambda h: h.tensor_scalar(out=e2, in0=e2, scalar1=1.0, scalar2=None, op0=ALU.add), reads=S, writes=S)
        dve.op(lambda h: h.reciprocal(out=e2, in_=e2), reads=S, writes=S)
        dve.op(lambda h: h.tensor_tensor(out=gsc, in0=e2, in1=psel, op=ALU.mult), reads=S, writes=S)
        dve.op(lambda h: h.scalar_tensor_tensor(out=G, in0=ex, scalar=gsc, in1=sel, op0=ALU.mult, op1=ALU.mult), reads=S, writes=S)
        pe.op(lambda h: h.matmul(P.pb[4][:, 0:64], lhsT=triU, rhs=selb, start=True, stop=True), reads=[rtri, rsm], writes=[P.rpb[4]])
        pe.op(lambda h: h.matmul(P.pb[5][:, 0:64], lhsT=ones, rhs=selb, start=True, stop=True), reads=[rtri, rsm], writes=[P.rpb[5]])
        act.op(lambda h: h.activation(out=pos, in_=P.pb[4][:, 0:64], func=AF.Copy), reads=[P.rpb[4]], writes=S)
        act.op(lambda h: h.activation(out=val, in_=P.pb[5][:, 0:64], func=AF.Copy), reads=[P.rpb[5]], writes=S)
        dve.op(lambda h: h.tensor_tensor(out=pos, in0=pos, in1=cnt, op=ALU.add), reads=S + [rcnt], writes=S)
        dve.op(lambda h: h.tensor_tensor(out=cnt, in0=val, in1=cnt, op=ALU.add), reads=S + [rcnt], writes=[rcnt])
        dve.op(lambda h: h.tensor_scalar(out=val, in0=pos, scalar1=float(CAP), scalar2=None, op0=ALU.is_ge), reads=S, writes=S)
        dve.op(lambda h: h.tensor_tensor(out=pos, in0=pos, in1=eC[:, 0:64], op=ALU.add), reads=S + [rwr], writes=S)
        dve.op(lambda h: h.tensor_scalar(out=val2, in0=pos, scalar1=-1.0, scalar2=eC[:, 64:65], op0=ALU.mult, op1=ALU.add), reads=S + [rwr], writes=S)
        dve.op(lambda h: h.tensor_tensor(out=val2, in0=val2, in1=val, op=ALU.mult), reads=S, writes=S)
        dve.op(lambda h: h.tensor_tensor(out=pos, in0=pos, in1=val2, op=ALU.add), reads=S, writes=S)
        dve.op(lambda h: h.scalar_tensor_tensor(out=val, in0=pos, scalar=1.0, in1=sel, op0=ALU.add, op1=ALU.mult), reads=S, writes=S)
        dve.op(lambda h: h.tensor_scalar(out=val2, in0=pos, scalar1=-1.0, scalar2=BIGC, op0=ALU.mult, op1=ALU.add), reads=S, writes=S)
        dve.op(lambda h: h.tensor_tensor(out=val2, in0=val2, in1=sel, op=ALU.mult), reads=S, writes=S)
        dve.op(lambda h: h.tensor_reduce(out=vmax, in_=val, axis=AX.X, op=ALU.max), reads=S, writes=S)
        dve.op(lambda h: h.tensor_reduce(out=vmax2, in_=val2, axis=AX.X, op=ALU.max), reads=S, writes=S)
        dve.op(lambda h: h.tensor_scalar(out=shi, in0=vmax, scalar1=-1.0, scalar2=None, op0=ALU.add), reads=S, writes=S)
        dve.op(lambda h: h.tensor_scalar(out=slo, in0=vmax2, scalar1=-1.0, scalar2=BIGC, op0=ALU.mult, op1=ALU.add), reads=S, writes=S)
        dve.op(lambda h, ti=ti: h.tensor_copy(out=idx[:, ti:ti + 1], in_=slo), reads=S, writes=[ridx[ti]])
        dve.op(lambda h, ti=ti: h.tensor_copy(out=idx[:, NT + ti:NT + ti + 1], in_=shi), reads=S, writes=[ridx[ti]])
        dve.op(lambda h: h.tensor_scalar(out=mhi, in0=val, scalar1=vmax, scalar2=None, op0=ALU.is_equal), reads=S, writes=S)
        dve.op(lambda h: h.tensor_tensor(out=mhi, in0=mhi, in1=G, op=ALU.mult), reads=S, writes=S)
        dve.op(lambda h: h.tensor_reduce(out=ghi, in_=mhi, axis=AX.X, op=ALU.add), reads=S, writes=S)
        dve.op(lambda h: h.tensor_reduce(out=gtot, in_=G, axis=AX.X, op=ALU.add), reads=S, writes=S)
        dve.op(lambda h, ti=ti: h.tensor_copy(out=gts[:, NT + ti:NT + ti + 1], in_=ghi), reads=S, writes=[rgts])
        dve.op(lambda h, ti=ti: h.tensor_tensor(out=gts[:, ti:ti + 1], in0=gtot, in1=ghi, op=ALU.subtract), reads=S, writes=[rgts])
        for w in range(2):
            pool.dma(sxg[w], lambda h, w=w, ti=ti, slot=slot: h.indirect_dma_start(
                out=xg, out_offset=bass.IndirectOffsetOnAxis(ap=idx[:, w * NT + ti:w * NT + ti + 1], axis=0),
                in_=hb[slot], in_offset=None),
                reads=[rhb[slot], ridx[ti]], writes=[rxg[2 * ti + w]])
    K.barrier()
    A.release(m1)
    if stop == "route":
        A.release(m0)
        P.stream_release(sm0)
        return

    NW = 3
    wgs = [A.alloc([128, 8, 256], BF16) for _ in range(NW)]
    wus = [A.alloc([128, 8, 256], BF16) for _ in range(NW)]
    wds = [A.alloc([128, 2, D], BF16) for _ in range(NW)]
    rwg, rwu, rwd = K.regions(NW, "ewg"), K.regions(NW, "ewu"), K.regions(NW, "ewd")
    swg, swu, swd = ([P.stream() for _ in range(NW)] for _ in range(3))
    NXB = 4
    xb = [A.alloc([128, D], BF16) for _ in range(NXB)]
    rxb = K.regions(NXB, "xb")
    sxb = [P.stream() for _ in range(NXB)]
    xbT = [A.alloc([128, 8, 256], BF16) for _ in range(2)]
    rxbT = K.regions(2, "xbT")
    sg = [A.alloc([128, 2, 256], F32) for _ in range(2)]
    rsg = K.regions(2, "sg")
    hid = [A.alloc([128, 2, 256], BF16) for _ in range(2)]
    rhid = K.regions(2, "hid")
    osb = [A.alloc([128, D], BF16) for _ in range(2)]
    rosb = K.regions(2, "osb")
    sob = [P.stream(), P.stream()]
    rog = K.regions(2 * NEXP, "og")

    def issue_weights(e):
        ws = e % NW
        pool.dma(swg[ws], lambda h: h.dma_start(out=wgs[ws], in_=wg_in[e].rearrange("(k p) n -> p k n", p=128)), writes=[rwg[ws]])
        pool.dma(swu[ws], lambda h: h.dma_start(out=wus[ws], in_=wu_in[e].rearrange("(k p) n -> p k n", p=128)), writes=[rwu[ws]])
        pool.dma(swd[ws], lambda h: h.dma_start(out=wds[ws], in_=wd_in[e].rearrange("(k p) n -> p k n", p=128)), writes=[rwd[ws]])

    def issue_loads(e):
        for blk in range(2):
            n = e * 2 + blk
            xs = n % NXB
            sp.dma(sxb[xs], lambda h, n=n, xs=xs: h.dma_start(out=xb[xs], in_=xg[n * 128:(n + 1) * 128, :]),
                   reads=rxg if n == 0 else [], writes=[rxb[xs]])

    import os
    NRUN = int(os.environ.get("NEXP_RUN", NEXP))
    STAGE = int(os.environ.get("EXP_STAGE", 9))
    issue_weights(0)
    issue_weights(1)
    issue_loads(0)
    for e in range(NRUN):
        ws = e % NW
        es = e % 2
        if e + 2 < NRUN:
            issue_weights(e + 2)
        if e + 1 < NRUN:
            issue_loads(e + 1)
        if STAGE < 2:
            continue
        for blk in range(2):
            n = e * 2 + blk
            xs = n % NXB
            bank = n % 2
            pst = P.pb[bank][:, :].bitcast(BF16).rearrange("p (k n) -> p k n", k=8)
            for k in range(8):
                pe.op(lambda h, k=k, xs=xs, pst=pst: h.transpose(out=pst[:, k, :], in_=xb[xs][:, k * 128:(k + 1) * 128], identity=P.identb),
                      reads=[rxb[xs], P.rconst], writes=[P.rpb[bank]])
            act.op(lambda h, es=es, blk=blk, pst=pst: h.activation(out=xbT[es][:, :, blk * 128:(blk + 1) * 128], in_=pst, func=AF.Copy),
                   reads=[P.rpb[bank]], writes=[rxbT[es]])
        if STAGE < 3:
            continue
        for which, (wsrc, rws_, bank) in enumerate(((wgs, rwg, 2), (wus, rwu, 3))):
            for hc in range(2):
                for k in range(8):
                    pe.op(lambda h, k=k, hc=hc, wsrc=wsrc, bank=bank, ws=ws, es=es: h.matmul(
                        P.pb[bank][:, hc * 256:(hc + 1) * 256], lhsT=wsrc[ws][:, k, hc * 128:(hc + 1) * 128], rhs=xbT[es][:, k, :],
                        start=(k == 0), stop=(k == 7)),
                        reads=[rws_[ws], rxbT[es]], writes=[P.rpb[bank]])
        act.op(lambda h, es=es: h.activation(out=sg[es].rearrange("p a b -> p (a b)"), in_=P.pb[2][:, :], func=AF.Silu),
               reads=[P.rpb[2]], writes=[rsg[es]])
        dve.op(lambda h, es=es: h.tensor_tensor(out=hid[es].rearrange("p a b -> p (a b)"), in0=sg[es].rearrange("p a b -> p (a b)"),
                                                in1=P.pb[3][:, :], op=ALU.mult),
               reads=[rsg[es], P.rpb[3]], writes=[rhid[es]])
        if STAGE < 4:
            continue
        for blk in range(2):
            osl = blk
            for nh in range(2):
                bank = 4 + blk * 2 + nh
                for hc in range(2):
                    pe.op(lambda h, hc=hc, nh=nh, blk=blk, bank=bank, ws=ws, es=es: h.matmul(
                        P.pb[bank][:, :], lhsT=hid[es][:, hc, blk * 128:(blk + 1) * 128], rhs=wds[ws][:, hc, nh * 512:(nh + 1) * 512],
                        start=(hc == 0), stop=(hc == 1)),
                        reads=[rhid[es], rwd[ws]], writes=[P.rpb[bank]])
                if nh == 0:
                    act.op(lambda h, bank=bank, osl=osl: h.activation(out=osb[osl][:, 0:512], in_=P.pb[bank][:, :], func=AF.Copy),
                           reads=[P.rpb[bank]], writes=[rosb[osl]])
                else:
                    dve.op(lambda h, bank=bank, osl=osl: h.tensor_copy(out=osb[osl][:, 512:1024], in_=P.pb[bank][:, :]),
                           reads=[P.rpb[bank]], writes=[rosb[osl]])
            if STAGE < 5:
                continue
            sp.dma(sob[osl], lambda h, e=e, blk=blk, osl=osl: h.dma_start(out=og[(e * 2 + blk) * 128:(e * 2 + blk + 1) * 128, :], in_=osb[osl]),
                   reads=[rosb[osl]], writes=[rog[e * 2 + blk]])
    K.barrier()
    A.release(m1)
    if stop == "experts":
        A.release(m0)
        P.stream_release(sm0)
        return

    wsg = A.alloc([128, 8, 512], BF16)
    wsu = A.alloc([128, 8, 512], BF16)
    wsd = A.alloc([128, 4, D], BF16)
    rws = K.regions(3, "ws")
    for i, (dst, srcw) in enumerate(((wsg, sg_in), (wsu, su_in), (wsd, sd_in))):
        s = P.stream()
        pool.dma(s, lambda h, dst=dst, srcw=srcw: h.dma_start(out=dst, in_=srcw.rearrange("(k p) n -> p k n", p=128)), writes=[rws[i]])
    hsT = A.alloc([128, 4, 512], BF16)
    rhsT = K.region()
    ssg = A.alloc([128, 512], F32)
    rssg = K.region()
    ra = [A.alloc([128, D], BF16) for _ in range(2)]
    rbb = [A.alloc([128, D], BF16) for _ in range(2)]
    rra = K.regions(2, "ra")
    rrb = K.regions(2, "rb")
    sga = [P.stream(), P.stream()]
    sgb = [P.stream(), P.stream()]
    acc = A.alloc([128, D], F32)
    racc = K.region()
    junk = A.alloc([128, D], BF16)
    rjunk = K.region()
    tmp = A.alloc([128, D], F32)
    rtmp = K.region()
    cols = A.alloc([128, 4], F32)
    rcols = K.region()
    for tt in range(TOK // 512):
        for hc in range(4):
            for which, (wsrc, bank) in enumerate(((wsg, 0), (wsu, 1))):
                for k in range(8):
                    pe.op(lambda h, k=k, hc=hc, wsrc=wsrc, bank=bank, tt=tt: h.matmul(
                        P.pb[bank][:, :], lhsT=wsrc[:, k, hc * 128:(hc + 1) * 128], rhs=h2T[:, k, tt * 512:(tt + 1) * 512],
                        start=(k == 0), stop=(k == 7)),
                        reads=[rws[which]] + rh2T[tt * 4:tt * 4 + 4], writes=[P.rpb[bank]])
            act.op(lambda h: h.activation(out=ssg, in_=P.pb[0][:, :], func=AF.Silu), reads=[P.rpb[0]], writes=[rssg])
            dve.op(lambda h, hc=hc: h.tensor_tensor(out=hsT[:, hc, :], in0=ssg, in1=P.pb[1][:, :], op=ALU.mult),
                   reads=[rssg, P.rpb[1]], writes=[rhsT])
        for q in range(4):
            ti = tt * 4 + q
            gs = ti % 2
            pool.op(lambda h, gs=gs: h.memset(ra[gs], 0.0), writes=[rra[gs]])
            pool.op(lambda h, gs=gs: h.memset(rbb[gs], 0.0), writes=[rrb[gs]])
            pool.dma(sga[gs], lambda h, gs=gs, ti=ti: h.indirect_dma_start(
                out=ra[gs], out_offset=None, in_=og, in_offset=bass.IndirectOffsetOnAxis(ap=idx[:, ti:ti + 1], axis=0)), reads=rog + [rogz, ridx[ti]], writes=[rra[gs]])
            pool.dma(sgb[gs], lambda h, gs=gs, ti=ti: h.indirect_dma_start(
                out=rbb[gs], out_offset=None, in_=og, in_offset=bass.IndirectOffsetOnAxis(ap=idx[:, NT + ti:NT + ti + 1], axis=0)), reads=rog + [rogz, ridx[ti]], writes=[rrb[gs]])
            for nh in range(2):
                bank = 2 + nh
                for hc in range(4):
                    pe.op(lambda h, hc=hc, nh=nh, q=q, bank=bank: h.matmul(
                        P.pb[bank][:, :], lhsT=hsT[:, hc, q * 128:(q + 1) * 128], rhs=wsd[:, hc, nh * 512:(nh + 1) * 512],
                        start=(hc == 0), stop=(hc == 3)),
                        reads=[rhsT, rws[2]], writes=[P.rpb[bank]])
                act.op(lambda h, nh=nh, bank=bank, ti=ti: h.activation(out=acc[:, nh * 512:(nh + 1) * 512], in_=P.pb[bank][:, :], func=AF.Copy,
                                                                       scale=gts[:, 2 * NT + ti:2 * NT + ti + 1]),
                       reads=[P.rpb[bank], rgts], writes=[racc])
            dve.op(lambda h, gs=gs, ti=ti: h.scalar_tensor_tensor(out=acc, in0=ra[gs], scalar=gts[:, ti:ti + 1], in1=acc,
                                                                  op0=ALU.mult, op1=ALU.add), reads=[rra[gs], rgts, racc], writes=[racc])
            dve.op(lambda h, gs=gs, ti=ti: h.scalar_tensor_tensor(out=acc, in0=rbb[gs], scalar=gts[:, NT + ti:NT + ti + 1], in1=acc,
                                                                  op0=ALU.mult, op1=ALU.add), reads=[rrb[gs], rgts, racc], writes=[racc])
            rms_rstd(P, acc, racc, junk, rjunk, cols[:, 0:1], rcols)
            dve.op(lambda h: h.scalar_tensor_tensor(out=tmp, in0=acc, scalar=cols[:, 0:1], in1=mod[:, 5, :],
                                                    op0=ALU.mult, op1=ALU.mult), reads=[racc, rcols, rmod[5]], writes=[rtmp])
            dve.op(lambda h, ti=ti: h.tensor_tensor(out=xres[:, ti, :], in0=xres[:, ti, :], in1=tmp, op=ALU.add),
                   reads=[rx[ti], rtmp], writes=[rx[ti]])
    K.barrier()
    A.release(m0)
    P.stream_release(sm0)


def load_x(P, xres, rx, x_in):
    for g in range(4):
        s = P.stream()
        P.sp.dma(s, lambda h, g=g: h.dma_start(out=xres[:, g * 4:(g + 1) * 4, :],
                                               in_=x_in[g * 512:(g + 1) * 512, :].rearrange("(t p) d -> p t d", p=128)),
                 writes=rx[g * 4:(g + 1) * 4])


def store_x(P, xres, rx, out):
    s = P.stream()
    for g in range(4):
        P.sp.dma(s, lambda h, g=g: h.dma_start(out=out[g * 512:(g + 1) * 512, :].rearrange("(t p) d -> p t d", p=128),
                                               in_=xres[:, g * 4:(g + 1) * 4, :]),
                 reads=rx[g * 4:(g + 1) * 4])
    P.sp.wait_clock(s)


def moe_inputs(P, tag):
    d = {}
    d["wr"] = P.dram(f"wr{tag}", [D, 73], F32, "ExternalInput")
    d["rb"] = P.dram(f"rb{tag}", [1, 72], F32, "ExternalInput")
    d["wg"] = P.dram(f"wg{tag}", [NEXP, D, 256], F32, "ExternalInput")
    d["wu"] = P.dram(f"wu{tag}", [NEXP, D, 256], F32, "ExternalInput")
    d["wd"] = P.dram(f"wd{tag}", [NEXP, 256, D], F32, "ExternalInput")
    d["sg"] = P.dram(f"sg{tag}", [D, 512], F32, "ExternalInput")
    d["su"] = P.dram(f"su{tag}", [D, 512], F32, "ExternalInput")
    d["sd"] = P.dram(f"sd{tag}", [512, D], F32, "ExternalInput")
    return d


def adaln_inputs(P, tag):
    return dict(c_col=P.dram(f"c_col{tag}", [128, 8], F32, "ExternalInput"),
                ada_w=P.dram(f"ada_w{tag}", [D, 6 * D], F32, "ExternalInput"),
                ada_b=P.dram(f"ada_b{tag}", [1, 6 * D], F32, "ExternalInput"),
                norm_w=P.dram(f"norm_w{tag}", [4, D], F32, "ExternalInput"))


def build_prog1(stop_after=None):
    nc = bass.Bass("TRN2", target_bir_lowering=False)
    with ExitStack() as st:
        P = Prog(nc, st)
        K, A = P.K, P.arena
        x_in = P.dram("x_in", [TOK, D], F32, "ExternalInput")
        x_halo = P.dram("x_halo", [2, D], F32, "ExternalInput")
        hflag = P.dram("hflag", [128, 1], F32, "ExternalInput")
        ad = adaln_inputs(P, "0")
        w_in = P.dram("conv_in_w", [D, 3 * D], F32, "ExternalInput")
        cw = P.dram("conv_cw", [128, 24], F32, "ExternalInput")
        w_out = P.dram("conv_out_w", [D, D], F32, "ExternalInput")
        ec = P.dram("ec", [128, 65], F32, "ExternalInput")
        mo = moe_inputs(P, "0")
        xg = P.dram("xg", [NSLOT + 128, D], BF16, "Internal")
        og = P.dram("og", [NSLOT + 128, D], BF16, "Internal")
        out = P.dram("x_out", [TOK, D], F32, "ExternalOutput")

        xres = A.alloc([128, NT, D], F32)
        rx = K.regions(NT, "x")
        mod = A.alloc([128, 6, D], F32)
        rmod = K.regions(6, "mod")
        load_x(P, xres, rx, x_in)
        phase_adaln(P, mod, rmod, ad["c_col"], ad["ada_w"], ad["ada_b"], ad["norm_w"])
        if stop_after != "adaln":
            phase_conv(P, xres, rx, mod, rmod, x_halo, hflag, w_in, cw, w_out)
            if stop_after != "conv":
                phase_moe(P, xres, rx, mod, rmod, mo["wr"], mo["rb"], ec, mo["wg"], mo["wu"], mo["wd"],
                          mo["sg"], mo["su"], mo["sd"], xg, og, stop=stop_after)
        if stop_after == "adaln":
            s = P.stream()
            P.sp.dma(s, lambda h: h.dma_start(out=out[0:128 * 6, :].rearrange("(t p) d -> p t d", p=128), in_=mod), reads=rmod)
            P.sp.wait_clock(s)
        else:
            store_x(P, xres, rx, out)
        K.finish()
        print("prog1 ops", K.nops, "dma", K.ndma, "arena peak", A.peak)
    return nc


def _c(a):
    return np.ascontiguousarray(a, dtype=np.float32)


def host_common(inputs, layer):
    i = layer
    d = {}
    t = str(i)
    d[f"ada_w{t}"] = _c(inputs["ada_w"][i])
    d[f"ada_b{t}"] = _c(inputs["ada_b"][i][None, :])
    d[f"norm_w{t}"] = _c(inputs["norm_w"][i])
    d[f"wr{t}"] = _c(np.concatenate([inputs["moe_group_w"][i], inputs["moe_expert_w"][i], inputs["shared_gate_w"][i]], axis=1))
    d[f"rb{t}"] = _c(np.concatenate([inputs["moe_group_b"][i], inputs["moe_expert_b"][i]])[None, :])
    d[f"wg{t}"] = _c(inputs["moe_w_gate"][i])
    d[f"wu{t}"] = _c(inputs["moe_w_up"][i])
    d[f"wd{t}"] = _c(inputs["moe_w_down"][i])
    d[f"sg{t}"] = _c(inputs["shared_w_gate"][i])
    d[f"su{t}"] = _c(inputs["shared_w_up"][i])
    d[f"sd{t}"] = _c(inputs["shared_w_down"][i])
    return d


def c_col_of(c, b):
    return _c(np.asarray(c)[b].reshape(8, 128).T)


EC = np.concatenate([np.tile((np.arange(NEXP, dtype=np.float32) * CAP)[None, :], (128, 1)),
                     (NSLOT + np.arange(128, dtype=np.float32))[:, None]], axis=1).astype(np.float32)


def run_prog1(inputs, stop_after=None):
    x = np.asarray(inputs["x"])
    nc = build_prog1(stop_after)
    com = host_common(inputs, 0)
    com["conv_in_w"] = _c(inputs["conv_in_w"][0])
    com["conv_out_w"] = _c(inputs["conv_out_w"][0])
    com["conv_cw"] = _c(np.asarray(inputs["conv_w"][0]).reshape(3, 8, 128).transpose(2, 1, 0).reshape(128, 24))
    com["ec"] = EC
    in_maps = []
    for core in range(NCORES):
        b, hf = core // 2, core % 2
        m = dict(com)
        m["x_in"] = _c(x[b, hf * TOK:(hf + 1) * TOK])
        m["x_halo"] = _c(x[b, TOK - 2:TOK]) if hf == 1 else np.zeros((2, D), np.float32)
        m["hflag"] = np.full((128, 1), float(hf), np.float32)
        m["c_col0"] = c_col_of(inputs["c"], b)
        in_maps.append(m)
    import os
    ncr = int(os.environ.get("NCORES_RUN", NCORES))
    res = run_bass_kernel_spmd(nc, in_maps[:ncr], core_ids=list(range(ncr)))
    outs = [r["x_out"] for r in res.results]
    outs = outs + [np.zeros_like(outs[0])] * (NCORES - ncr)
    xo = np.stack(outs).reshape(4, SEQ, D)
    return xo


HL = 4
GT = SEQ // 128
QSCALE = 128.0 ** -0.5


def phase_gdn(P, mod, rmod, x_in, h_in, wq_in, wba_in, cw_in, hp_in, gnw_in, out):
    import os
    K, A = P.K, P.arena
    pe, act, dve, pool, sp = P.pe, P.act, P.dve, P.pool, P.sp
    m0 = A.mark()
    sm0 = P.stream_mark()
    W = A.alloc([128, 8, 2048], BF16)
    Wba = A.alloc([128, 8, 8], BF16)
    cw = A.alloc([128, 48], F32)
    hp = A.alloc([128, 8], F32)
    negA = A.alloc([128, 4], F32)
    gnw = A.alloc([128, 128], F32)
    Mtri = A.alloc([128, 128], F32)
    SL = A.alloc([128, 128], F32)
    onesf = A.alloc([128, 128], F32)
    onesb = A.alloc([128, 128], BF16)
    rW = K.regions(4, "W")
    rc = K.region("c2")
    for i in range(4):
        s = P.stream()
        pool.dma(s, lambda h, i=i: h.dma_start(out=W[:, :, i * 512:(i + 1) * 512],
                                               in_=wq_in[:, i * 512:(i + 1) * 512].rearrange("(k p) n -> p k n", p=128)), writes=[rW[i]])
    s = P.stream()
    pool.dma(s, lambda h: h.dma_start(out=Wba, in_=wba_in.rearrange("(k p) n -> p k n", p=128)), writes=[rc])
    s = P.stream()
    sp.dma(s, lambda h: h.dma_start(out=cw, in_=cw_in), writes=[rc])
    s = P.stream()
    sp.dma(s, lambda h: h.dma_start(out=hp, in_=hp_in.partition_broadcast(128)), writes=[rc])
    s = P.stream()
    sp.dma(s, lambda h: h.dma_start(out=gnw, in_=gnw_in.partition_broadcast(128)), writes=[rc])
    act.op(lambda h: h.activation(out=negA, in_=hp[:, 0:4], func=AF.Exp), reads=[rc], writes=[rc])
    dve.op(lambda h: h.tensor_scalar(out=negA, in0=negA, scalar1=-1.0, scalar2=None, op0=ALU.mult), reads=[rc], writes=[rc])
    pool.op(lambda h: h.memset(onesf, 1.0), writes=[rc])
    pool.op(lambda h: h.memset(onesb, 1.0), writes=[rc])
    pool.op(lambda h: h.memset(Mtri, 1.0), writes=[rc])
    pool.op(lambda h: h.affine_select(out=Mtri, in_=Mtri, pattern=[[1, 128]], compare_op=ALU.is_ge, fill=0.0,
                                      base=0, channel_multiplier=-1), reads=[rc], writes=[rc])
    pool.op(lambda h: h.memset(SL, 1.0), writes=[rc])
    pool.op(lambda h: h.affine_select(out=SL, in_=SL, pattern=[[-1, 128]], compare_op=ALU.is_ge, fill=0.0,
                                      base=-1, channel_multiplier=1), reads=[rc], writes=[rc])

    import os
    GTR = int(os.environ.get("GT_RUN", GT))
    GST = int(os.environ.get("GDN_STAGE", 99))
    BT = 256
    NB = SEQ // BT
    xt = [A.alloc([128, D], F32) for _ in range(2)] if h_in is None else None
    rxt = K.regions(2, "xt")
    sxt = [P.stream(), P.stream()]
    hb = [A.alloc([128, D], BF16) for _ in range(2)]
    rhb = K.regions(2, "hb")
    hTb = [A.alloc([128, 8, BT], BF16) for _ in range(2)]
    rhTb = K.regions(2, "hTb")
    junk = A.alloc([128, D], BF16)
    tmp = A.alloc([128, D], F32) if h_in is None else None
    cols = A.alloc([128, 8], F32)
    rjunk, rtmp, rcols = K.region(), K.region(), K.region()
    pre = A.alloc([128, 12, BT + 3], F32)
    rpre = K.regions(12, "pre")
    qkv = A.alloc([128, 12, BT], F32)
    rqkv = K.regions(12, "qkv")
    acc = [A.alloc([128, BT], F32) for _ in range(2)]
    racc = K.regions(2, "acc")
    sq = [A.alloc([128, BT], BF16) for _ in range(2)]
    rsq = K.regions(2, "sq")
    rn = [A.alloc([128, BT], F32) for _ in range(2)]
    rrn = K.regions(2, "rn")
    zs = [A.alloc([128, 2, 512], BF16) for _ in range(2)]
    rzs = [K.regions(2, "zsa"), K.regions(2, "zsb")]
    sm = A.alloc([128, 2, 8, 8], F32)
    rsm = K.regions(2, "sm")
    sm2 = A.alloc([128, 16], F32)
    rsm2 = K.region()

    def F(name):
        return [[A.alloc([128, 128], F32) for _ in range(HL)] for _ in range(2)], \
               [K.regions(HL, name + "a"), K.regions(HL, name + "b")]
    Ub, rU = F("U")
    WTb, rWT = F("WT")
    ATb, rAT = F("AT")
    kdb, rkd = F("kd")
    QTb, rQT = F("QT")

    def G(name):
        return [A.alloc([128, 128], F32) for _ in range(HL)], K.regions(HL, name)
    Kbg, rKbg = G("Kbg")
    Vb, rVb = G("Vb")
    dec, rdec = G("dec")
    decT, rdecT = G("decT")
    TriG, rTriG = G("TriG")
    t1b, rt1 = G("t1")
    Lm, rL = G("L")
    Nm, rN = G("N")
    Xa, rXa = G("Xa")
    Xb_, rXb = G("Xb")
    Pa, rPa = G("Pa")
    Pb_, rPb = G("Pb")
    PTa, rPTa = G("PTa")
    PTb, rPTb = G("PTb")
    stgA, rstgA = G("stgA")
    stgB, rstgB = G("stgB")
    vnew, rvnew = G("vnew")
    o1s, ro1s = G("o1s")
    otok, rotok = G("otok")
    Sst = [[A.alloc([128, 128], F32) for _ in range(HL)] for _ in range(2)]
    rS = [K.regions(HL, "Sa"), K.regions(HL, "Sb")]
    ogt = [A.alloc([128, 512], BF16) for _ in range(2)]
    rogt = K.regions(2, "ogt")
    sog = [P.stream(), P.stream()]
    for h_ in range(HL):
        pool.op(lambda h, h_=h_: h.memset(Sst[0][h_], 0.0), writes=[rS[0][h_]])
    for c in range(12):
        pool.op(lambda h, c=c: h.memset(pre[:, c, 0:3], 0.0), writes=[rpre[c]])

    ring = [0]

    def nb():
        b = ring[0] % 8
        ring[0] += 1
        return b

    def load_tile(ti):
        sl = ti % 2
        if h_in is None:
            sp.dma(sxt[sl], lambda h: h.dma_start(out=xt[sl], in_=x_in[ti * 128:(ti + 1) * 128, :]), writes=[rxt[sl]])
        else:
            r_, c_, i0 = ti // 16, (ti % 16) // 8, (ti % 8) * 128
            row = c_ * 2048 + r_ * 1024 + i0
            sp.dma(sxt[sl], lambda h: h.dma_start(out=hb[sl], in_=h_in[row:row + 128, :]), writes=[rhb[sl]])

    load_tile(0)
    load_tile(1)

    def project_block(bb):
        bp = bb % 2
        for q in range(2):
            ti = bb * 2 + q
            sl = ti % 2
            if h_in is None:
                rms_rstd(P, xt[sl], rxt[sl], junk, rjunk, cols[:, 0:1], rcols)
                dve.op(lambda h: h.scalar_tensor_tensor(out=tmp, in0=xt[sl], scalar=cols[:, 0:1], in1=mod[:, 0, :],
                                                        op0=ALU.mult, op1=ALU.mult), reads=[rxt[sl], rcols, rmod[0]], writes=[rtmp])
                dve.op(lambda h: h.tensor_tensor(out=hb[sl], in0=tmp, in1=mod[:, 1, :], op=ALU.add),
                       reads=[rtmp, rmod[1]], writes=[rhb[sl]])
                if ti + 2 < GTR:
                    load_tile(ti + 2)
            bank = nb()
            pst = P.pb[bank][:, :].bitcast(BF16).rearrange("p (k n) -> p k n", k=8)
            for k in range(8):
                pe.op(lambda h, k=k: h.transpose(out=pst[:, k, :], in_=hb[sl][:, k * 128:(k + 1) * 128], identity=P.identb),
                      reads=[rhb[sl], P.rconst], writes=[P.rpb[bank]])
            if h_in is not None and ti + 2 < GTR:
                load_tile(ti + 2)
            act.op(lambda h: h.activation(out=hTb[bp][:, :, q * 128:(q + 1) * 128], in_=pst, func=AF.Copy),
                   reads=[P.rpb[bank]], writes=[rhTb[bp]])
        for c in range(12):
            bank = nb()
            for k in range(8):
                pe.op(lambda h, k=k: h.matmul(P.pb[bank][:, 0:BT], lhsT=W[:, k, c * 128:(c + 1) * 128], rhs=hTb[bp][:, k, :],
                                              start=(k == 0), stop=(k == 7)), reads=[rW[c // 4], rhTb[bp]], writes=[P.rpb[bank]])
            act.op(lambda h: h.activation(out=pre[:, c, 3:3 + BT], in_=P.pb[bank][:, 0:BT], func=AF.Copy),
                   reads=[P.rpb[bank]], writes=[rpre[c]])
            a = acc[c % 2]
            ra = racc[c % 2]
            pool.op(lambda h: h.tensor_scalar(out=a, in0=pre[:, c, 0:BT], scalar1=cw[:, c * 4:c * 4 + 1], scalar2=None, op0=ALU.mult),
                    reads=[rpre[c], rc], writes=[ra])
            dve.op(lambda h: h.scalar_tensor_tensor(out=a, in0=pre[:, c, 1:1 + BT], scalar=cw[:, c * 4 + 1:c * 4 + 2], in1=a,
                                                    op0=ALU.mult, op1=ALU.add), reads=[rpre[c], rc, ra], writes=[ra])
            dve.op(lambda h: h.scalar_tensor_tensor(out=a, in0=pre[:, c, 2:2 + BT], scalar=cw[:, c * 4 + 2:c * 4 + 3], in1=a,
                                                    op0=ALU.mult, op1=ALU.add), reads=[rpre[c], rc, ra], writes=[ra])
            dve.op(lambda h: h.scalar_tensor_tensor(out=a, in0=pre[:, c, 3:3 + BT], scalar=cw[:, c * 4 + 3:c * 4 + 4], in1=a,
                                                    op0=ALU.mult, op1=ALU.add), reads=[rpre[c], rc, ra], writes=[ra])
            act.op(lambda h: h.activation(out=qkv[:, c, :], in_=a, func=AF.Silu), reads=[ra], writes=[rqkv[c]])
            pool.op(lambda h: h.tensor_copy(out=pre[:, c, 0:3], in_=pre[:, c, BT:BT + 3]), reads=[rpre[c]], writes=[rpre[c]])
            if c < 8:
                s_ = sq[c % 2]
                act.op(lambda h: h.activation(out=s_, in_=qkv[:, c, :], func=AF.Square), reads=[rqkv[c]], writes=[rsq[c % 2]])
                bank2 = nb()
                pe.op(lambda h: h.matmul(P.pb[bank2][:, 0:BT], lhsT=onesb, rhs=s_, start=True, stop=True),
                      reads=[rc, rsq[c % 2]], writes=[P.rpb[bank2]])
                r_ = rn[c % 2]
                act.op(lambda h: h.activation(out=r_, in_=P.pb[bank2][:, 0:BT], func=AF.Sqrt, bias=EPS, scale=1.0),
                       reads=[P.rpb[bank2]], writes=[rrn[c % 2]])
                dve.op(lambda h: h.reciprocal(out=r_, in_=r_), reads=[rrn[c % 2]], writes=[rrn[c % 2]])
                sc = QSCALE if c < 4 else 1.0
                dve.op(lambda h: h.scalar_tensor_tensor(out=qkv[:, c, :], in0=qkv[:, c, :], scalar=sc, in1=r_,
                                                        op0=ALU.mult, op1=ALU.mult), reads=[rqkv[c], rrn[c % 2]], writes=[rqkv[c]])
        for q in range(2):
            bank = nb()
            for k in range(8):
                pe.op(lambda h, k=k: h.matmul(P.pb[bank][:, :], lhsT=hTb[bp][:, k, q * 128:(q + 1) * 128], rhs=W[:, k, 1536:2048],
                                              start=(k == 0), stop=(k == 7)), reads=[rhTb[bp], rW[3]], writes=[P.rpb[bank]])
            act.op(lambda h: h.activation(out=zs[bp][:, q, :], in_=P.pb[bank][:, :], func=AF.Silu), reads=[P.rpb[bank]], writes=[rzs[bp][q]])
        bank = nb()
        for q in range(2):
            for k in range(8):
                pe.op(lambda h, k=k: h.matmul(P.pb[bank][:, q * 8:(q + 1) * 8], lhsT=hTb[bp][:, k, q * 128:(q + 1) * 128], rhs=Wba[:, k, :],
                                              start=(k == 0), stop=(k == 7)), reads=[rhTb[bp], rc], writes=[P.rpb[bank]])
        S_ = sm[:, bp]
        R = [rsm[bp]]
        pba = P.pb[bank][:, 0:16].rearrange("p (q j) -> p q j", q=2)
        v3 = lambda kind: S_[:, kind, :].rearrange("p (q j) -> p q j", q=2)
        act.op(lambda h: h.activation(out=v3(0), in_=pba[:, :, 0:4], func=AF.Sigmoid), reads=[P.rpb[bank]], writes=R)
        act.op(lambda h: h.activation(out=v3(1), in_=pba[:, :, 4:8], func=AF.Copy), reads=[P.rpb[bank]], writes=R)
        dve.op(lambda h: h.tensor_tensor(out=v3(1), in0=v3(1), in1=hp[:, 4:8].rearrange("p (o j) -> p o j", o=1).to_broadcast([128, 2, 4]),
                                         op=ALU.add), reads=R + [rc], writes=R)
        act.op(lambda h: h.activation(out=S_[:, 1, :], in_=S_[:, 1, :], func=AF.Exp), reads=R, writes=R)
        act.op(lambda h: h.activation(out=S_[:, 1, :], in_=S_[:, 1, :], func=AF.Ln, bias=1.0, scale=1.0), reads=R, writes=R)
        dve.op(lambda h: h.tensor_tensor(out=v3(1), in0=v3(1), in1=negA.rearrange("p (o j) -> p o j", o=1).to_broadcast([128, 2, 4]),
                                         op=ALU.mult), reads=R + [rc], writes=R)
        bank = nb()
        pe.op(lambda h: h.matmul(P.pb[bank][:, 0:8], lhsT=Mtri, rhs=S_[:, 1, :], start=True, stop=True), reads=R + [rc], writes=[P.rpb[bank]])
        pe.op(lambda h: h.matmul(P.pb[bank][:, 8:16], lhsT=onesf, rhs=S_[:, 1, :], start=True, stop=True), reads=R + [rc], writes=[P.rpb[bank]])
        act.op(lambda h: h.activation(out=S_[:, 2:4, :], in_=P.pb[bank][:, 0:16].rearrange("p (a b) -> p a b", a=2), func=AF.Copy),
               reads=[P.rpb[bank]], writes=R)
        act.op(lambda h: h.activation(out=S_[:, 4, :], in_=S_[:, 2, :], func=AF.Exp), reads=R, writes=R)
        dve.op(lambda h: h.tensor_tensor(out=S_[:, 5, :], in0=S_[:, 3, :], in1=S_[:, 2, :], op=ALU.subtract), reads=R, writes=R)
        act.op(lambda h: h.activation(out=S_[:, 5, :], in_=S_[:, 5, :], func=AF.Exp), reads=R, writes=R)
        act.op(lambda h: h.activation(out=S_[:, 6, :], in_=S_[:, 3, :], func=AF.Exp), reads=R, writes=R)
        dve.op(lambda h: h.tensor_tensor(out=S_[:, 7, :], in0=S_[:, 0, :], in1=S_[:, 4, :], op=ALU.mult), reads=R, writes=R)

    def scal(bp, kind, q, hl):
        j = q * 4 + hl
        return sm[:, bp, kind, j:j + 1]

    def precompute(ti):
        bb, q = ti // 2, ti % 2
        bp = bb % 2
        tp = ti % 2
        R = [rsm[bp]]
        tok = slice(q * 128, (q + 1) * 128)
        hs = range(HL)
        KT = lambda hl: qkv[:, 4 + hl, tok]
        QT = lambda hl: qkv[:, hl, tok]
        VT = lambda hl: qkv[:, 8 + hl, tok]
        for hl in hs:
            b1 = nb()
            pe.op(lambda h: h.transpose(out=P.pb[b1][:, 0:128], in_=KT(hl), identity=P.identf), reads=[rqkv[4 + hl], P.rconst], writes=[P.rpb[b1]])
            act.op(lambda h: h.activation(out=t1b[hl], in_=P.pb[b1][:, 0:128], func=AF.Copy), reads=[P.rpb[b1]], writes=[rt1[hl]])
            pool.op(lambda h: h.tensor_scalar(out=Kbg[hl], in0=t1b[hl], scalar1=scal(bp, 7, q, hl), scalar2=None, op0=ALU.mult),
                    reads=[rt1[hl]] + R, writes=[rKbg[hl]])
            dve.op(lambda h: h.tensor_scalar(out=kdb[tp][hl], in0=t1b[hl], scalar1=scal(bp, 5, q, hl), scalar2=None, op0=ALU.mult),
                   reads=[rt1[hl]] + R, writes=[rkd[tp][hl]])
            b2 = nb()
            pe.op(lambda h: h.transpose(out=P.pb[b2][:, 0:128], in_=VT(hl), identity=P.identf), reads=[rqkv[8 + hl], P.rconst], writes=[P.rpb[b2]])
            act.op(lambda h: h.activation(out=Vb[hl], in_=P.pb[b2][:, 0:128], func=AF.Copy, scale=scal(bp, 0, q, hl)),
                   reads=[P.rpb[b2]] + R, writes=[rVb[hl]])
            pool.op(lambda h: h.tensor_copy(out=QTb[tp][hl], in_=QT(hl)), reads=[rqkv[hl]], writes=[rQT[tp][hl]])
            pool.op(lambda h: h.tensor_scalar(out=TriG[hl], in0=Mtri, scalar1=scal(bp, 1, q, hl), scalar2=None, op0=ALU.mult),
                    reads=[rc] + R, writes=[rTriG[hl]])
        if GST < 3:
            return
        for hl in hs:
            b1 = nb()
            pe.op(lambda h: h.matmul(P.pb[b1][:, 0:128], lhsT=TriG[hl], rhs=SL, start=True, stop=True), reads=[rTriG[hl], rc], writes=[P.rpb[b1]])
            act.op(lambda h: h.activation(out=dec[hl], in_=P.pb[b1][:, 0:128], func=AF.Exp), reads=[P.rpb[b1]], writes=[rdec[hl]])
            b2 = nb()
            pe.op(lambda h: h.matmul(P.pb[b2][:, 0:128], lhsT=SL, rhs=TriG[hl], start=True, stop=True), reads=[rTriG[hl], rc], writes=[P.rpb[b2]])
            act.op(lambda h: h.activation(out=decT[hl], in_=P.pb[b2][:, 0:128], func=AF.Exp), reads=[P.rpb[b2]], writes=[rdecT[hl]])
        for hl in hs:
            b1 = nb()
            pe.op(lambda h: h.matmul(P.pb[b1][:, 0:128], lhsT=KT(hl), rhs=KT(hl), start=True, stop=True), reads=[rqkv[4 + hl]], writes=[P.rpb[b1]])
            act.op(lambda h: h.activation(out=stgA[hl], in_=P.pb[b1][:, 0:128], func=AF.Copy), reads=[P.rpb[b1]], writes=[rstgA[hl]])
            dve.op(lambda h: h.tensor_tensor(out=t1b[hl], in0=stgA[hl], in1=dec[hl], op=ALU.mult),
                   reads=[rstgA[hl], rdec[hl]], writes=[rt1[hl]])
            pool.op(lambda h: h.tensor_tensor(out=t1b[hl], in0=t1b[hl], in1=SL, op=ALU.mult),
                    reads=[rt1[hl], rc], writes=[rt1[hl]])
            pool.op(lambda h: h.tensor_scalar(out=Lm[hl], in0=t1b[hl], scalar1=scal(bp, 0, q, hl), scalar2=None, op0=ALU.mult),
                    reads=[rt1[hl]] + R, writes=[rL[hl]])
            b2 = nb()
            pe.op(lambda h: h.matmul(P.pb[b2][:, 0:128], lhsT=KT(hl), rhs=QT(hl), start=True, stop=True), reads=[rqkv[4 + hl], rqkv[hl]], writes=[P.rpb[b2]])
            act.op(lambda h: h.activation(out=stgB[hl], in_=P.pb[b2][:, 0:128], func=AF.Copy), reads=[P.rpb[b2]], writes=[rstgB[hl]])
            dve.op(lambda h: h.tensor_tensor(out=ATb[tp][hl], in0=stgB[hl], in1=decT[hl], op=ALU.mult),
                   reads=[rstgB[hl], rdecT[hl]], writes=[rAT[tp][hl]])
            pool.op(lambda h: h.tensor_tensor(out=ATb[tp][hl], in0=ATb[tp][hl], in1=Mtri, op=ALU.mult),
                    reads=[rAT[tp][hl], rc], writes=[rAT[tp][hl]])
        if GST < 4:
            return
        for hl in hs:
            b1 = nb()
            pe.op(lambda h: h.transpose(out=P.pb[b1][:, 0:128], in_=Lm[hl], identity=P.identf), reads=[rL[hl], P.rconst], writes=[P.rpb[b1]])
            act.op(lambda h: h.activation(out=Nm[hl], in_=P.pb[b1][:, 0:128], func=AF.Copy), reads=[P.rpb[b1]], writes=[rN[hl]])
            dve.op(lambda h: h.tensor_tensor(out=Xa[hl], in0=P.identf, in1=Nm[hl], op=ALU.subtract),
                   reads=[P.rconst, rN[hl]], writes=[rXa[hl]])
        if GST < 5:
            return
        Pc = [(Nm[hl], rN[hl]) for hl in hs]
        PTc = [(Lm[hl], rL[hl]) for hl in hs]
        Xc = [(Xa[hl], rXa[hl]) for hl in hs]
        for lvl in range(1, 7):
            newP, newPT, newX = [], [], []
            for hl in hs:
                (p_, rp_), (pt_, rpt_), (x_, rx_) = Pc[hl], PTc[hl], Xc[hl]
                pn, rpn = (Pa[hl], rPa[hl]) if lvl % 2 == 1 else (Pb_[hl], rPb[hl])
                ptn, rptn = (PTa[hl], rPTa[hl]) if lvl % 2 == 1 else (PTb[hl], rPTb[hl])
                xn, rxn = (Xb_[hl], rXb[hl]) if lvl % 2 == 1 else (Xa[hl], rXa[hl])
                if lvl < 6:
                    b1 = nb()
                    pe.op(lambda h: h.matmul(P.pb[b1][:, 0:128], lhsT=pt_, rhs=p_, start=True, stop=True), reads=[rpt_, rp_], writes=[P.rpb[b1]])
                    act.op(lambda h: h.activation(out=pn, in_=P.pb[b1][:, 0:128], func=AF.Copy), reads=[P.rpb[b1]], writes=[rpn])
                b2 = nb()
                pe.op(lambda h: h.matmul(P.pb[b2][:, 0:128], lhsT=p_, rhs=pt_, start=True, stop=True), reads=[rpt_, rp_], writes=[P.rpb[b2]])
                act.op(lambda h: h.activation(out=ptn, in_=P.pb[b2][:, 0:128], func=AF.Copy), reads=[P.rpb[b2]], writes=[rptn])
                newP.append((pn, rpn))
                newPT.append((ptn, rptn))
                newX.append((xn, rxn))
            for hl in hs:
                (x_, rx_) = Xc[hl]
                (ptn, rptn) = newPT[hl]
                (xn, rxn) = newX[hl]
                b3 = nb()
                pe.op(lambda h: h.matmul(P.pb[b3][:, 0:128], lhsT=ptn, rhs=x_, start=True, stop=True), reads=[rptn, rx_], writes=[P.rpb[b3]])
                act.op(lambda h: h.activation(out=stgA[hl], in_=P.pb[b3][:, 0:128], func=AF.Copy), reads=[P.rpb[b3]], writes=[rstgA[hl]])
                dve.op(lambda h: h.tensor_tensor(out=xn, in0=stgA[hl], in1=x_, op=ALU.add), reads=[rstgA[hl], rx_], writes=[rxn])
            Pc, PTc, Xc = newP, newPT, newX
        if GST < 6:
            return
        for hl in hs:
            (x_, rx_) = Xc[hl]
            b1 = nb()
            pe.op(lambda h: h.matmul(P.pb[b1][:, 0:128], lhsT=x_, rhs=Vb[hl], start=True, stop=True), reads=[rx_, rVb[hl]], writes=[P.rpb[b1]])
            act.op(lambda h: h.activation(out=Ub[tp][hl], in_=P.pb[b1][:, 0:128], func=AF.Copy), reads=[P.rpb[b1]], writes=[rU[tp][hl]])
            b2 = nb()
            pe.op(lambda h: h.matmul(P.pb[b2][:, 0:128], lhsT=Kbg[hl], rhs=x_, start=True, stop=True), reads=[rx_, rKbg[hl]], writes=[P.rpb[b2]])
            act.op(lambda h: h.activation(out=WTb[tp][hl], in_=P.pb[b2][:, 0:128], func=AF.Copy), reads=[P.rpb[b2]], writes=[rWT[tp][hl]])

    def scan(ti):
        bb, q = ti // 2, ti % 2
        bp = bb % 2
        tp = ti % 2
        si, so = ti % 2, (ti + 1) % 2
        R = [rsm[bp]]
        hs = range(HL)
        bpv, bpo1, bpo2, bpS = {}, {}, {}, {}
        for hl in hs:
            bpv[hl] = nb()
            pe.op(lambda h: h.matmul(P.pb[bpv[hl]][:, 0:128], lhsT=WTb[tp][hl], rhs=Sst[si][hl], start=True, stop=True),
                  reads=[rWT[tp][hl], rS[si][hl]], writes=[P.rpb[bpv[hl]]])
            act.op(lambda h: h.activation(out=stgA[hl], in_=P.pb[bpv[hl]][:, 0:128], func=AF.Copy), reads=[P.rpb[bpv[hl]]], writes=[rstgA[hl]])
            dve.op(lambda h: h.tensor_tensor(out=vnew[hl], in0=Ub[tp][hl], in1=stgA[hl], op=ALU.subtract),
                   reads=[rU[tp][hl], rstgA[hl]], writes=[rvnew[hl]])
        for hl in hs:
            bpo1[hl] = nb()
            pe.op(lambda h: h.matmul(P.pb[bpo1[hl]][:, 0:128], lhsT=QTb[tp][hl], rhs=Sst[si][hl], start=True, stop=True),
                  reads=[rQT[tp][hl], rS[si][hl]], writes=[P.rpb[bpo1[hl]]])
            act.op(lambda h: h.activation(out=o1s[hl], in_=P.pb[bpo1[hl]][:, 0:128], func=AF.Copy, scale=scal(bp, 4, q, hl)),
                   reads=[P.rpb[bpo1[hl]]] + R, writes=[ro1s[hl]])
        for hl in hs:
            bpS[hl] = nb()
            pe.op(lambda h: h.matmul(P.pb[bpS[hl]][:, 0:128], lhsT=kdb[tp][hl], rhs=vnew[hl], start=True, stop=True),
                  reads=[rkd[tp][hl], rvnew[hl]], writes=[P.rpb[bpS[hl]]])
            act.op(lambda h: h.activation(out=stgB[hl], in_=P.pb[bpS[hl]][:, 0:128], func=AF.Copy), reads=[P.rpb[bpS[hl]]], writes=[rstgB[hl]])
            dve.op(lambda h: h.scalar_tensor_tensor(out=Sst[so][hl], in0=Sst[si][hl], scalar=scal(bp, 6, q, hl), in1=stgB[hl],
                                                    op0=ALU.mult, op1=ALU.add), reads=[rS[si][hl], rstgB[hl]] + R, writes=[rS[so][hl]])
        for hl in hs:
            bpo2[hl] = nb()
            pe.op(lambda h: h.matmul(P.pb[bpo2[hl]][:, 0:128], lhsT=ATb[tp][hl], rhs=vnew[hl], start=True, stop=True),
                  reads=[rAT[tp][hl], rvnew[hl]], writes=[P.rpb[bpo2[hl]]])
            act.op(lambda h: h.activation(out=stgA[hl], in_=P.pb[bpo2[hl]][:, 0:128], func=AF.Copy), reads=[P.rpb[bpo2[hl]]], writes=[rstgA[hl]])
            dve.op(lambda h: h.tensor_tensor(out=otok[hl], in0=o1s[hl], in1=stgA[hl], op=ALU.add),
                   reads=[ro1s[hl], rstgA[hl]], writes=[rotok[hl]])
        og_ = ogt[tp]
        for hl in hs:
            cc = cols[:, 1 + hl:2 + hl]
            rms_rstd(P, otok[hl], rotok[hl], junk[:, 0:128], rjunk, cc, rcols, ncols=128)
            dve.op(lambda h: h.scalar_tensor_tensor(out=otok[hl], in0=otok[hl], scalar=cc, in1=gnw, op0=ALU.mult, op1=ALU.mult),
                   reads=[rotok[hl], rcols, rc], writes=[rotok[hl]])
            pool.op(lambda h: h.tensor_tensor(out=og_[:, hl * 128:(hl + 1) * 128], in0=otok[hl], in1=zs[bp][:, q, hl * 128:(hl + 1) * 128], op=ALU.mult),
                    reads=[rotok[hl], rzs[bp][q]], writes=[rogt[tp]])
        sp.dma(sog[tp], lambda h: h.dma_start(out=out[ti * 128:(ti + 1) * 128, :], in_=og_), reads=[rogt[tp]])

    project_block(0)
    if GST >= 2:
        precompute(0)
    for ti in range(GTR):
        nxt = ti + 1
        if nxt < GTR:
            if nxt % 2 == 0:
                project_block(nxt // 2)
            if GST >= 2:
                precompute(nxt)
        if GST >= 7:
            scan(ti)
    for s_ in sog:
        sp.wait_clock(s_)
    K.barrier()
    A.release(m0)
    P.stream_release(sm0)


def build_prog2():
    nc = bass.Bass("TRN2", target_bir_lowering=False)
    with ExitStack() as st:
        P = Prog(nc, st)
        K, A = P.K, P.arena
        x_in = P.dram("x_in", [SEQ, D], F32, "ExternalInput")
        ad = adaln_inputs(P, "1")
        wq_in = P.dram("wqkvz", [D, 2048], F32, "ExternalInput")
        wba_in = P.dram("wba", [D, 8], F32, "ExternalInput")
        cw_in = P.dram("gcw", [128, 48], F32, "ExternalInput")
        hp_in = P.dram("hp", [1, 8], F32, "ExternalInput")
        gnw_in = P.dram("gnw", [1, 128], F32, "ExternalInput")
        out = P.dram("og_out", [SEQ, 512], BF16, "ExternalOutput")
        mod = A.alloc([128, 6, D], F32)
        rmod = K.regions(6, "mod")
        phase_adaln(P, mod, rmod, ad["c_col"], ad["ada_w"], ad["ada_b"], ad["norm_w"])
        phase_gdn(P, mod, rmod, x_in, None, wq_in, wba_in, cw_in, hp_in, gnw_in, out)
        K.finish()
        print("prog2 ops", K.nops, "dma", K.ndma, "arena peak", A.peak)
    return nc


def run_prog2(inputs, x1):
    nc = build_prog2()
    i = 1
    com = {"ada_w1": _c(inputs["ada_w"][i]), "ada_b1": _c(inputs["ada_b"][i][None, :]), "norm_w1": _c(inputs["norm_w"][i])}
    gw = np.asarray(inputs["gdn_in_w"][0])
    gcw = np.asarray(inputs["gdn_conv_w"][0])
    in_maps = []
    for core in range(NCORES):
        b, hh = core // 2, core % 2
        m = dict(com)
        cs = slice(hh * 512, (hh + 1) * 512)
        m["wqkvz"] = _c(np.concatenate([gw[:, 0:1024][:, cs], gw[:, 1024:2048][:, cs], gw[:, 2048:3072][:, cs], gw[:, 3072:4096][:, cs]], axis=1))
        m["wba"] = _c(np.concatenate([gw[:, 4096 + hh * 4:4096 + hh * 4 + 4], gw[:, 4104 + hh * 4:4104 + hh * 4 + 4]], axis=1))
        secs = [gcw[:, s0 + hh * 512:s0 + (hh + 1) * 512] for s0 in (0, 1024, 2048)]
        cwc = np.concatenate(secs, axis=1).reshape(4, 12, 128)
        m["gcw"] = _c(cwc.transpose(2, 1, 0).reshape(128, 48))
        m["hp"] = _c(np.concatenate([np.asarray(inputs["gdn_a_log"][0])[hh * 4:hh * 4 + 4],
                                     np.asarray(inputs["gdn_dt_bias"][0])[hh * 4:hh * 4 + 4]])[None, :])
        m["gnw"] = _c(np.asarray(inputs["gdn_norm_w"][0])[None, :])
        m["x_in"] = _c(x1[b])
        m["c_col1"] = c_col_of(inputs["c"], b)
        in_maps.append(m)
    import os
    ncr = int(os.environ.get("NCORES_RUN", NCORES))
    res = run_bass_kernel_spmd(nc, in_maps[:ncr], core_ids=list(range(ncr)))
    r0 = np.asarray(res.results[0]["og_out"])
    o = np.zeros((4, SEQ, D), r0.dtype)
    for core in range(ncr):
        b, hh = core // 2, core % 2
        o[b, :, hh * 512:(hh + 1) * 512] = np.asarray(res.results[core]["og_out"])
    return o


def phase_outproj(P, xres, rx, mod, rmod, og_in, w_out, gather=None):
    K, A = P.K, P.arena
    pe, act, dve, pool, sp = P.pe, P.act, P.dve, P.pool, P.sp
    m0 = A.mark()
    sm0 = P.stream_mark()
    if gather is not None:
        tidx = A.alloc([128, 2 * NT], I32)
        rtidx = K.region()
        s = P.stream()
        sp.dma(s, lambda h: h.dma_start(out=tidx, in_=gather[1]), writes=[rtidx])
    Wout = A.alloc([128, 8, D], BF16)
    rWout = K.region()
    s = P.stream()
    pool.dma(s, lambda h: h.dma_start(out=Wout, in_=w_out.rearrange("(k p) n -> p k n", p=128)), writes=[rWout])
    ogb = [A.alloc([128, D], BF16) for _ in range(2)]
    rogb = K.regions(2, "ogb")
    sogb = [P.stream(), P.stream()]
    sogb2 = [[P.stream(), P.stream()], [P.stream(), P.stream()]]
    rogb2 = [K.regions(2, "ogb2a"), K.regions(2, "ogb2b")]
    ogh2 = [[A.alloc([128, 512], BF16) for _ in range(2)] for _ in range(2)] if gather is not None else None
    ogT = [A.alloc([128, 8, 128], BF16) for _ in range(2)]
    rogT = K.regions(2, "ogT")
    ysb = A.alloc([128, D], F32)
    junk = A.alloc([128, D], BF16)
    tmp = A.alloc([128, D], F32)
    cols = A.alloc([128, 4], F32)
    rysb, rjunk, rtmp, rcols = (K.region() for _ in range(4))
    for ti in range(NT):
        sl = ti % 2
        if gather is None:
            sp.dma(sogb[sl], lambda h: h.dma_start(out=ogb[sl], in_=og_in[ti * 128:(ti + 1) * 128, :]), writes=[rogb[sl]])
        else:
            for r in range(2):
                pool.dma(sogb2[sl][r], lambda h: h.indirect_dma_start(
                    out=ogh2[sl][r], out_offset=None, in_=gather[0],
                    in_offset=bass.IndirectOffsetOnAxis(ap=tidx[:, ti * 2 + r:ti * 2 + r + 1], axis=0)),
                    reads=[rtidx], writes=[rogb2[sl][r]])
        bank = sl
        pst = P.pb[bank][:, :].bitcast(BF16).rearrange("p (k n) -> p k n", k=8)
        for k in range(8):
            src_ = ogb[sl][:, k * 128:(k + 1) * 128] if gather is None else ogh2[sl][k // 4][:, (k % 4) * 128:(k % 4 + 1) * 128]
            pe.op(lambda h, k=k: h.transpose(out=pst[:, k, :], in_=src_, identity=P.identb),
                  reads=[rogb[sl], rogb2[sl][k // 4], P.rconst], writes=[P.rpb[bank]])
        act.op(lambda h: h.activation(out=ogT[sl], in_=pst, func=AF.Copy), reads=[P.rpb[bank]], writes=[rogT[sl]])
        for nh in range(2):
            b2 = 2 + sl * 2 + nh
            for k in range(8):
                pe.op(lambda h, k=k: h.matmul(P.pb[b2][:, :], lhsT=ogT[sl][:, k, :], rhs=Wout[:, k, nh * 512:(nh + 1) * 512],
                                              start=(k == 0), stop=(k == 7)), reads=[rogT[sl], rWout], writes=[P.rpb[b2]])
            act.op(lambda h: h.activation(out=ysb[:, nh * 512:(nh + 1) * 512], in_=P.pb[b2][:, :], func=AF.Copy),
                   reads=[P.rpb[b2]], writes=[rysb])
        rms_rstd(P, ysb, rysb, junk, rjunk, cols[:, 0:1], rcols)
        dve.op(lambda h: h.scalar_tensor_tensor(out=tmp, in0=ysb, scalar=cols[:, 0:1], in1=mod[:, 2, :], op0=ALU.mult, op1=ALU.mult),
               reads=[rysb, rcols, rmod[2]], writes=[rtmp])
        dve.op(lambda h: h.tensor_tensor(out=xres[:, ti, :], in0=xres[:, ti, :], in1=tmp, op=ALU.add),
               reads=[rx[ti], rtmp], writes=[rx[ti]])
    K.barrier()
    A.release(m0)
    P.stream_release(sm0)


def build_prog3(stop_after=None):
    nc = bass.Bass("TRN2", target_bir_lowering=False)
    with ExitStack() as st:
        P = Prog(nc, st)
        K, A = P.K, P.arena
        x_in = P.dram("x_in", [TOK, D], F32, "ExternalInput")
        og_in = P.dram("og_in", [TOK, D], BF16, "ExternalInput")
        ad = adaln_inputs(P, "1")
        w_out = P.dram("gdn_out_w", [D, D], F32, "ExternalInput")
        ec = P.dram("ec", [128, 65], F32, "ExternalInput")
        mo = moe_inputs(P, "1")
        xg = P.dram("xg", [NSLOT + 128, D], BF16, "Internal")
        og = P.dram("og", [NSLOT + 128, D], BF16, "Internal")
        out = P.dram("x_out", [TOK, D], F32, "ExternalOutput")
        xres = A.alloc([128, NT, D], F32)
        rx = K.regions(NT, "x")
        mod = A.alloc([128, 6, D], F32)
        rmod = K.regions(6, "mod")
        load_x(P, xres, rx, x_in)
        phase_adaln(P, mod, rmod, ad["c_col"], ad["ada_w"], ad["ada_b"], ad["norm_w"])
        phase_outproj(P, xres, rx, mod, rmod, og_in, w_out)
        if stop_after != "outproj":
            phase_moe(P, xres, rx, mod, rmod, mo["wr"], mo["rb"], ec, mo["wg"], mo["wu"], mo["wd"],
                      mo["sg"], mo["su"], mo["sd"], xg, og)
        store_x(P, xres, rx, out)
        K.finish()
        print("prog3 ops", K.nops, "dma", K.ndma, "arena peak", A.peak)
    return nc


def run_prog3(inputs, x1, ogf, stop_after=None):
    nc = build_prog3(stop_after)
    com = host_common(inputs, 1)
    com["gdn_out_w"] = _c(inputs["gdn_out_w"][0])
    com["ec"] = EC
    in_maps = []
    for core in range(NCORES):
        b, hf = core // 2, core % 2
        m = dict(com)
        m["x_in"] = _c(x1[b, hf * TOK:(hf + 1) * TOK])
        m["og_in"] = np.ascontiguousarray(ogf[b, hf * TOK:(hf + 1) * TOK])
        m["c_col1"] = c_col_of(inputs["c"], b)
        in_maps.append(m)
    res = run_bass_kernel_spmd(nc, in_maps, core_ids=list(range(NCORES)))
    return np.stack([r["x_out"] for r in res.results]).reshape(4, SEQ, D)


def kernel_unfused(**inputs):
    inputs = {k: np.asarray(v) for k, v in inputs.items()}
    x1 = run_prog1(inputs)
    og = run_prog2(inputs, x1)
    out = run_prog3(inputs, x1, og)
    return out.astype(np.float32)


PAIRS = [[0, 1], [2, 3], [4, 5], [6, 7]]


def build_fused():
    nc = bass.Bass("TRN2", target_bir_lowering=False)
    with ExitStack() as st:
        P = Prog(nc, st)
        K, A = P.K, P.arena
        pe, act, dve, pool, sp = P.pe, P.act, P.dve, P.pool, P.sp
        x_in = P.dram("x_in", [TOK, D], F32, "ExternalInput")
        x_halo = P.dram("x_halo", [2, D], F32, "ExternalInput")
        hflag = P.dram("hflag", [128, 1], F32, "ExternalInput")
        c_col = P.dram("c_col", [128, 8], F32, "ExternalInput")
        ad = []
        for t in ("0", "1"):
            ad.append(dict(ada_w=P.dram(f"ada_w{t}", [D, 6 * D], F32, "ExternalInput"),
                           ada_b=P.dram(f"ada_b{t}", [1, 6 * D], F32, "ExternalInput"),
                           norm_w=P.dram(f"norm_w{t}", [4, D], F32, "ExternalInput")))
        w_in = P.dram("conv_in_w", [D, 3 * D], F32, "ExternalInput")
        cw = P.dram("conv_cw", [128, 24], F32, "ExternalInput")
        w_out = P.dram("conv_out_w", [D, D], F32, "ExternalInput")
        ec = P.dram("ec", [128, 65], F32, "ExternalInput")
        mo0 = moe_inputs(P, "0")
        mo1 = moe_inputs(P, "1")
        wq_in = P.dram("wqkvz", [D, 2048], F32, "ExternalInput")
        wba_in = P.dram("wba", [D, 8], F32, "ExternalInput")
        gcw_in = P.dram("gcw", [128, 48], F32, "ExternalInput")
        hp_in = P.dram("hp", [1, 8], F32, "ExternalInput")
        gnw_in = P.dram("gnw", [1, 128], F32, "ExternalInput")
        gout_w = P.dram("gdn_out_w", [D, D], F32, "ExternalInput")
        tokidx = P.dram("tokidx", [128, 2 * NT], I32, "ExternalInput")
        xg = P.dram("xg", [NSLOT + 128, D], BF16, "Internal")
        og = P.dram("og", [NSLOT + 128, D], BF16, "Internal")
        hsh = P.dram("hsh", [TOK, D], BF16, "Internal")
        hfull = P.dram("hfull", [SEQ, D], BF16, "Internal")
        ogh = P.dram("ogh", [SEQ, 512], BF16, "Internal")
        ogfull = P.dram("ogfull", [2 * SEQ, 512], BF16, "Internal")
        xsp = P.dram("xsp", [TOK, D], F32, "Internal")
        out = P.dram("x_out", [TOK, D], F32, "ExternalOutput")

        mod = A.alloc([128, 6, D], F32)
        rmod = K.regions(6, "mod")
        mx = A.mark()
        xres = A.alloc([128, NT, D], F32)
        rx = K.regions(NT, "x")
        import os
        FS = os.environ.get("FUSED_STOP", "")
        load_x(P, xres, rx, x_in)
        phase_adaln(P, mod, rmod, c_col, ad[0]["ada_w"], ad[0]["ada_b"], ad[0]["norm_w"])
        if FS != "skipl0":
            phase_conv(P, xres, rx, mod, rmod, x_halo, hflag, w_in, cw, w_out)
            phase_moe(P, xres, rx, mod, rmod, mo0["wr"], mo0["rb"], ec, mo0["wg"], mo0["wu"], mo0["wd"],
                      mo0["sg"], mo0["su"], mo0["sd"], xg, og)
        phase_adaln(P, mod, rmod, c_col, ad[1]["ada_w"], ad[1]["ada_b"], ad[1]["norm_w"])
        m1 = A.mark()
        sm1 = P.stream_mark()
        hb = [A.alloc([128, D], BF16) for _ in range(2)]
        rhb = K.regions(2, "hb1")
        shb = [P.stream(), P.stream()]
        junk = A.alloc([128, D], BF16)
        tmp = A.alloc([128, D], F32)
        cols = A.alloc([128, 4], F32)
        rjunk, rtmp, rcols = K.region(), K.region(), K.region()
        rhsh = K.regions(NT, "hsh")
        for ti in range(NT):
            sl = ti % 2
            rms_rstd(P, xres[:, ti, :], rx[ti], junk, rjunk, cols[:, 0:1], rcols)
            dve.op(lambda h: h.scalar_tensor_tensor(out=tmp, in0=xres[:, ti, :], scalar=cols[:, 0:1], in1=mod[:, 0, :],
                                                    op0=ALU.mult, op1=ALU.mult), reads=[rx[ti], rcols, rmod[0]], writes=[rtmp])
            dve.op(lambda h: h.tensor_tensor(out=hb[sl], in0=tmp, in1=mod[:, 1, :], op=ALU.add),
                   reads=[rtmp, rmod[1]], writes=[rhb[sl]])
            sp.dma(shb[sl], lambda h: h.dma_start(out=hsh[ti * 128:(ti + 1) * 128, :], in_=hb[sl]), reads=[rhb[sl]], writes=[rhsh[ti]])
        ssp = P.stream()
        for g in range(4):
            sp.dma(ssp, lambda h, g=g: h.dma_start(out=xsp[g * 512:(g + 1) * 512, :].rearrange("(t p) d -> p t d", p=128),
                                                   in_=xres[:, g * 4:(g + 1) * 4, :]), reads=rx[g * 4:(g + 1) * 4])
        K.barrier()
        for c in range(2):
            scc = P.stream()
            pool.dma(scc, lambda h, c=c: h.collective_compute(
                "AllGather", op=ALU.bypass, replica_groups=PAIRS,
                ins=[hsh[c * 1024:(c + 1) * 1024, :].opt()], outs=[hfull[c * 2048:(c + 1) * 2048, :].opt()]), inc=1)
        K.barrier()
        A.release(mx)
        P.stream_release(sm1)
        if FS != "nogdn":
            phase_gdn(P, mod, rmod, None, hfull, wq_in, wba_in, gcw_in, hp_in, gnw_in, ogh)
        for c in range(2):
            scc = P.stream()
            pool.dma(scc, lambda h, c=c: h.collective_compute(
                "AllGather", op=ALU.bypass, replica_groups=PAIRS,
                ins=[ogh[c * 2048:(c + 1) * 2048, :].opt()], outs=[ogfull[c * 4096:(c + 1) * 4096, :].opt()]), inc=1)
        xres = A.alloc([128, NT, D], F32)
        load_x(P, xres, rx, xsp)
        K.barrier()
        if FS not in ("reload",):
            phase_outproj(P, xres, rx, mod, rmod, None, gout_w, gather=(ogfull, tokidx))
        if FS not in ("reload", "outproj"):
            phase_moe(P, xres, rx, mod, rmod, mo1["wr"], mo1["rb"], ec, mo1["wg"], mo1["wu"], mo1["wd"],
                      mo1["sg"], mo1["su"], mo1["sd"], xg, og)
        store_x(P, xres, rx, out)
        K.finish()
        print("fused ops", K.nops, "dma", K.ndma, "arena peak", A.peak, "sems", K._n)
    return nc


def fused_in_maps(inputs):
    x = np.asarray(inputs["x"])
    com = {}
    com.update(host_common(inputs, 0))
    com.update(host_common(inputs, 1))
    com["conv_in_w"] = _c(inputs["conv_in_w"][0])
    com["conv_out_w"] = _c(inputs["conv_out_w"][0])
    com["conv_cw"] = _c(np.asarray(inputs["conv_w"][0]).reshape(3, 8, 128).transpose(2, 1, 0).reshape(128, 24))
    com["ec"] = EC
    com["gdn_out_w"] = _c(inputs["gdn_out_w"][0])
    com["gnw"] = _c(np.asarray(inputs["gdn_norm_w"][0])[None, :])
    gw = np.asarray(inputs["gdn_in_w"][0])
    gcw = np.asarray(inputs["gdn_conv_w"][0])
    in_maps = []
    for core in range(NCORES):
        b, hf = core // 2, core % 2
        hh = hf
        m = dict(com)
        m["x_in"] = _c(x[b, hf * TOK:(hf + 1) * TOK])
        m["x_halo"] = _c(x[b, TOK - 2:TOK]) if hf == 1 else np.zeros((2, D), np.float32)
        m["hflag"] = np.full((128, 1), float(hf), np.float32)
        m["c_col"] = c_col_of(inputs["c"], b)
        cs = slice(hh * 512, (hh + 1) * 512)
        m["wqkvz"] = _c(np.concatenate([gw[:, 0:1024][:, cs], gw[:, 1024:2048][:, cs], gw[:, 2048:3072][:, cs], gw[:, 3072:4096][:, cs]], axis=1))
        m["wba"] = _c(np.concatenate([gw[:, 4096 + hh * 4:4096 + hh * 4 + 4], gw[:, 4104 + hh * 4:4104 + hh * 4 + 4]], axis=1))
        secs = [gcw[:, s0 + hh * 512:s0 + (hh + 1) * 512] for s0 in (0, 1024, 2048)]
        cwc = np.concatenate(secs, axis=1).reshape(4, 12, 128)
        m["gcw"] = _c(cwc.transpose(2, 1, 0).reshape(128, 48))
        m["hp"] = _c(np.concatenate([np.asarray(inputs["gdn_a_log"][0])[hh * 4:hh * 4 + 4],
                                     np.asarray(inputs["gdn_dt_bias"][0])[hh * 4:hh * 4 + 4]])[None, :])
        ti = np.arange(NT)[None, :, None]
        r = np.arange(2)[None, None, :]
        p = np.arange(128)[:, None, None]
        m["tokidx"] = np.ascontiguousarray((hf * SEQ + r * TOK + ti * 128 + p).reshape(128, 2 * NT).astype(np.int32))
        in_maps.append(m)
    return in_maps


def kernel_fused(**inputs):
    inputs = {k: np.asarray(v) for k, v in inputs.items()}
    nc = build_fused()
    in_maps = fused_in_maps(inputs)
    res = run_bass_kernel_spmd(nc, in_maps, core_ids=list(range(NCORES)))
    return np.stack([r["x_out"] for r in res.results]).reshape(4, SEQ, D).astype(np.float32)


def kernel(**inputs):
    return kernel_fused(**inputs)
```

```python
import types
import numpy as np
from contextlib import ExitStack
import concourse.bass as bass
import concourse.mybir as mybir
from concourse.bass_utils import run_bass_kernel_spmd

F32 = mybir.dt.float32
BF16 = mybir.dt.bfloat16
I32 = mybir.dt.int32
U8 = mybir.dt.uint8
AF = mybir.ActivationFunctionType
ALU = mybir.AluOpType
AX = mybir.AxisListType

D = 1024
NCORES = 8
SEQ = 4096
TOK = 2048
NT = TOK // 128
EPS = 1e-6
NEXP = 64
CAP = 256
NSLOT = NEXP * CAP
BIG = 30000.0
BIGC = 4194304.0
OVF = 1048576.0


class Clock:
    def __init__(self, sem, name):
        self.sem = sem
        self.name = name
        self.count = 0


class Region:
    __slots__ = ("name", "w", "r")

    def __init__(self, name):
        self.name = name
        self.w = None
        self.r = []


def _freeze(fn):
    if fn.__closure__ is None:
        return fn
    cells = tuple(types.CellType(c.cell_contents) for c in fn.__closure__)
    return types.FunctionType(fn.__code__, fn.__globals__, fn.__name__, fn.__defaults__, cells)


def _prune(lst):
    best = {}
    for clk, cnt in lst:
        if best.get(clk, 0) < cnt:
            best[clk] = cnt
    return list(best.items())


class Eng:
    def __init__(self, ctx, name, clock, same_engine_sync=True):
        self.ctx = ctx
        self.name = name
        self.clock = clock
        self.waited = {}
        self.same_engine_sync = same_engine_sync
        self.q = []

    def emit(self, h):
        for a in self.q:
            if a[0] == 0:
                h.wait_ge(a[1], a[2])
            else:
                a[1](h).then_inc(a[2], a[3])

    def _need(self, dep, needs):
        if dep is None:
            return
        clk, cnt = dep
        if clk is self.clock and not self.same_engine_sync:
            return
        if self.waited.get(clk, 0) >= cnt:
            return
        if needs.get(clk, 0) < cnt:
            needs[clk] = cnt

    def deps(self, reads, writes):
        needs = {}
        for r in reads:
            self._need(r.w, needs)
        for w in writes:
            self._need(w.w, needs)
            for d in w.r:
                self._need(d, needs)
        for clk, cnt in needs.items():
            self.q.append((0, clk.sem, cnt))
            self.waited[clk] = cnt

    def _mark(self, me, reads, writes):
        for r in reads:
            r.r.append(me)
            if len(r.r) > 24:
                r.r = _prune(r.r)
        for w in writes:
            w.w = me
            w.r = []

    def op(self, fn, reads=(), writes=()):
        self.deps(reads, writes)
        self.clock.count += 1
        self.q.append((1, _freeze(fn), self.clock.sem, 1))
        self._mark((self.clock, self.clock.count), reads, writes)
        self.ctx.nops += 1

    def dma(self, stream, fn, reads=(), writes=(), inc=16):
        assert getattr(stream, "sw", False) == (self.name == "pool"), (stream.name, self.name)
        self.deps(reads, writes)
        stream.count += inc
        self.q.append((1, _freeze(fn), stream.sem, inc))
        self._mark((stream, stream.count), reads, writes)
        self.ctx.ndma += 1

    def wait_clock(self, clk):
        if clk.count > 0 and self.waited.get(clk, 0) < clk.count:
            if clk is self.clock and not self.same_engine_sync:
                return
            self.q.append((0, clk.sem, clk.count))
            self.waited[clk] = clk.count


class Ctx:
    def __init__(self, nc, stack):
        self.nc = nc
        self.stack = stack
        self.nops = 0
        self.ndma = 0
        self._n = 0
        self.clocks = []
        self.pe = Eng(self, "pe", self.clock("pe"), same_engine_sync=False)
        self.act = Eng(self, "act", self.clock("act"))
        self.dve = Eng(self, "dve", self.clock("dve"))
        self.pool = Eng(self, "pool", self.clock("pool"))
        self.sp = Eng(self, "sp", self.clock("sp"))
        self.engs = [self.pe, self.act, self.dve, self.pool, self.sp]

    def clock(self, name):
        sem = self.stack.enter_context(self.nc.semaphore(f"s{self._n}_{name}"))
        self._n += 1
        c = Clock(sem, name)
        self.clocks.append(c)
        return c

    def stream(self, name="d"):
        return self.clock(name)

    def region(self, name="r"):
        return Region(name)

    def regions(self, n, name="r"):
        return [Region(f"{name}{i}") for i in range(n)]

    def barrier(self):
        for e in self.engs:
            for c in self.clocks:
                e.wait_clock(c)

    def finish(self):
        with self.nc.Block() as block:
            @block.tensor
            def _(h):
                self.pe.emit(h)

            @block.scalar
            def _(h):
                self.act.emit(h)

            @block.vector
            def _(h):
                self.dve.emit(h)

            @block.gpsimd
            def _(h):
                self.pool.emit(h)

            @block.sync
            def _(h):
                self.sp.emit(h)


_DTSIZE = {F32: 4, BF16: 2, I32: 4, U8: 1}


class Arena:
    def __init__(self, tensor, size):
        self.t = tensor
        self.size = size
        self.off = 0
        self.peak = 0

    def alloc(self, shape, dt):
        assert shape[0] == 128
        n = 1
        for s in shape[1:]:
            n *= s
        nb = n * _DTSIZE[dt]
        nb = (nb + 63) // 64 * 64
        assert self.off + nb <= self.size, f"arena overflow {self.off + nb} > {self.size}"
        v = self.t[:, self.off:self.off + nb].bitcast(dt)
        if n * _DTSIZE[dt] != nb:
            v = v[:, 0:n]
        self.off += nb
        self.peak = max(self.peak, self.off)
        if len(shape) == 3:
            v = v.rearrange("p (a b) -> p a b", a=shape[1])
        elif len(shape) == 4:
            v = v.rearrange("p (a b c) -> p a b c", a=shape[1], b=shape[2])
        return v

    def mark(self):
        return self.off

    def release(self, m):
        self.off = m


class Prog:
    def __init__(self, nc, st):
        self.nc = nc
        self.st = st
        self.K = Ctx(nc, st)
        K = self.K
        self.pe, self.act, self.dve, self.pool, self.sp = K.pe, K.act, K.dve, K.pool, K.sp
        ARENA = 200 * 1024
        self.arena = Arena(st.enter_context(nc.sbuf_tensor("arena", [128, ARENA], U8)), ARENA)
        self.pb = [st.enter_context(nc.psum_tensor(f"pb{i}", [128, 512], F32)) for i in range(8)]
        self.rpb = K.regions(8, "pb")
        self._ns = 0
        self._free = []
        self._free_sw = []
        self._live = []
        A = self.arena
        self.identf = A.alloc([128, 128], F32)
        self.identb = A.alloc([128, 128], BF16)
        self.rconst = K.region("const")
        pool = self.pool
        pool.op(lambda h: h.memset(self.identf, 0.0), writes=[self.rconst])
        pool.op(lambda h: h.affine_select(out=self.identf, in_=self.identf, pattern=[[-1, 128]],
                                          compare_op=ALU.not_equal, fill=1.0, base=0, channel_multiplier=1),
                reads=[self.rconst], writes=[self.rconst])
        pool.op(lambda h: h.tensor_copy(out=self.identb, in_=self.identf), reads=[self.rconst], writes=[self.rconst])

    def stream(self, sw=False):
        free = self._free_sw if sw else self._free
        if free:
            c = free.pop()
        else:
            self._ns += 1
            c = self.K.stream(f"d{self._ns}")
            c.sw = sw
        self._live.append(c)
        return c

    def stream_mark(self):
        return len(self._live)

    def stream_release(self, m):
        for c in self._live[m:]:
            (self._free_sw if c.sw else self._free).append(c)
        del self._live[m:]

    def dram(self, name, shape, dt, kind):
        return self.nc.dram_tensor(name, shape, dt, kind=kind).ap()


def rms_rstd(P, src_ap, src_reg, junk, rjunk, col, rcol, ncols=D):
    P.act.op(lambda h: h.activation(out=junk, in_=src_ap, func=AF.Square, accum_out=col),
             reads=[src_reg], writes=[rjunk, rcol])
    P.act.op(lambda h: h.activation(out=col, in_=col, func=AF.Sqrt, scale=1.0 / ncols, bias=EPS),
             reads=[rcol], writes=[rcol])
    P.dve.op(lambda h: h.reciprocal(out=col, in_=col), reads=[rcol], writes=[rcol])


def phase_adaln(P, mod, rmod, c_col, ada_w, ada_b, norm_w):
    K, A = P.K, P.arena
    pe, act, dve, pool, sp = P.pe, P.act, P.dve, P.pool, P.sp
    m0 = A.mark()
    sm0 = P.stream_mark()
    cs = A.alloc([128, 8], F32)
    csb = A.alloc([128, 8], BF16)
    csbb = A.alloc([128, 8, 128], BF16)
    nwb = A.alloc([128, 4, D], F32)
    bb = A.alloc([128, 6 * D], F32)
    wt = [A.alloc([128, 8, 512], BF16) for _ in range(2)]
    rcs, rnwb, rbb = K.region(), K.region(), K.region()
    rwt = K.regions(2, "wt")
    swt = [P.stream(sw=True), P.stream(sw=True)]
    s0 = P.stream()
    sp.dma(s0, lambda h: h.dma_start(out=cs, in_=c_col), writes=[rcs])
    s1 = P.stream()
    sp.dma(s1, lambda h: h.dma_start(out=bb, in_=ada_b.partition_broadcast(128)), writes=[rbb])
    s2 = P.stream()
    sp.dma(s2, lambda h: h.dma_start(out=nwb.rearrange("p a b -> p (a b)"),
                                     in_=norm_w.rearrange("a b -> (a b)").rearrange("(o n) -> o n", o=1).partition_broadcast(128)),
           writes=[rnwb])
    act.op(lambda h: h.activation(out=csb, in_=cs, func=AF.Silu), reads=[rcs], writes=[rcs])
    for k in range(8):
        dve.op(lambda h, k=k: h.tensor_copy(out=csbb[:, k, :], in_=csb[:, k:k + 1].to_broadcast([128, 128])),
               reads=[rcs], writes=[rcs])
    dest = [1, 0, 2, 4, 3, 5]
    for ct in range(12):
        s = ct % 2
        pool.dma(swt[s], lambda h, s=s, ct=ct: h.dma_start(
            out=wt[s], in_=ada_w[:, ct * 512:(ct + 1) * 512].rearrange("(k p) n -> p k n", p=128)),
            writes=[rwt[s]])
        bank = P.pb[s]
        for k in range(8):
            pe.op(lambda h, k=k, s=s, bank=bank: h.matmul(bank[:, :], lhsT=csbb[:, k, :], rhs=wt[s][:, k, :],
                                                          start=(k == 0), stop=(k == 7)),
                  reads=[rcs, rwt[s]], writes=[P.rpb[s]])
        di = dest[ct // 2]
        half = ct % 2
        dve.op(lambda h, bank=bank, di=di, half=half, ct=ct: h.tensor_tensor(
            out=mod[:, di, half * 512:(half + 1) * 512], in0=bank[:, :], in1=bb[:, ct * 512:(ct + 1) * 512], op=ALU.add),
            reads=[P.rpb[s], rbb], writes=[rmod[di]])
    dve.op(lambda h: h.scalar_tensor_tensor(out=mod[:, 0, :], in0=mod[:, 0, :], scalar=1.0, in1=nwb[:, 0, :],
                                            op0=ALU.add, op1=ALU.mult), reads=[rmod[0], rnwb], writes=[rmod[0]])
    dve.op(lambda h: h.tensor_tensor(out=mod[:, 2, :], in0=mod[:, 2, :], in1=nwb[:, 1, :], op=ALU.mult),
           reads=[rmod[2], rnwb], writes=[rmod[2]])
    dve.op(lambda h: h.scalar_tensor_tensor(out=mod[:, 3, :], in0=mod[:, 3, :], scalar=1.0, in1=nwb[:, 2, :],
                                            op0=ALU.add, op1=ALU.mult), reads=[rmod[3], rnwb], writes=[rmod[3]])
    dve.op(lambda h: h.tensor_tensor(out=mod[:, 5, :], in0=mod[:, 5, :], in1=nwb[:, 3, :], op=ALU.mult),
           reads=[rmod[5], rnwb], writes=[rmod[5]])
    K.barrier()
    A.release(m0)
    P.stream_release(sm0)


def phase_conv(P, xres, rx, mod, rmod, x_halo, hflag_in, w_in, cw_in, w_out):
    K, A = P.K, P.arena
    pe, act, dve, pool, sp = P.pe, P.act, P.dve, P.pool, P.sp
    m0 = A.mark()
    sm0 = P.stream_mark()
    Win = A.alloc([128, 8, 3 * D], BF16)
    Wout = A.alloc([128, 8, D], BF16)
    hT = A.alloc([128, 8, 512], BF16)
    hTh = A.alloc([128, 8, 2], BF16)
    ycT = A.alloc([128, 8, 512], BF16)
    hb = [A.alloc([128, D], BF16) for _ in range(2)]
    xh = A.alloc([128, D], F32)
    ysb = xh
    u = [A.alloc([128, 514], F32) for _ in range(2)]
    t1 = [A.alloc([128, 512], F32) for _ in range(2)]
    pcsb = A.alloc([128, 512], F32)
    junk = A.alloc([128, D], BF16)
    tmp = A.alloc([128, D], F32)
    cw = A.alloc([128, 24], F32)
    hflag = A.alloc([128, 1], F32)
    uh = A.alloc([128, 8, 2], F32)
    cols = A.alloc([128, 4], F32)

    rWin = K.regions(3, "win")
    rWout, rhT, rhTh, rycT, rxh, rpcsb, rjunk, rtmp, rcw, ruh, rcols = (K.region() for _ in range(11))
    rhb = K.regions(2, "hb")
    ru = K.regions(2, "u")
    rt1 = K.regions(2, "t1")

    for i in range(3):
        s = P.stream(sw=True)
        pool.dma(s, lambda h, i=i: h.dma_start(
            out=Win[:, :, i * D:(i + 1) * D], in_=w_in[:, i * D:(i + 1) * D].rearrange("(k p) n -> p k n", p=128)),
            writes=[rWin[i]])
    s = P.stream(sw=True)
    pool.dma(s, lambda h: h.dma_start(out=Wout, in_=w_out.rearrange("(k p) n -> p k n", p=128)), writes=[rWout])
    s = P.stream()
    sp.dma(s, lambda h: h.dma_start(out=cw, in_=cw_in), writes=[rcw])
    s = P.stream()
    sp.dma(s, lambda h: h.dma_start(out=hflag, in_=hflag_in), writes=[rcw])
    pool.op(lambda h: h.memset(xh, 0.0), writes=[rxh])
    s = P.stream()
    sp.dma(s, lambda h: h.dma_start(out=xh[0:2, :], in_=x_halo), writes=[rxh])

    def make_h(src, rsrc, slot):
        rms_rstd(P, src, rsrc, junk, rjunk, cols[:, 0:1], rcols)
        dve.op(lambda h: h.scalar_tensor_tensor(out=tmp, in0=src, scalar=cols[:, 0:1], in1=mod[:, 0, :],
                                                op0=ALU.mult, op1=ALU.mult),
               reads=[rsrc, rcols, rmod[0]], writes=[rtmp])
        dve.op(lambda h: h.tensor_tensor(out=hb[slot], in0=tmp, in1=mod[:, 1, :], op=ALU.add),
               reads=[rtmp, rmod[1]], writes=[rhb[slot]])

    def transpose_h(slot, bank, dst, rdst, ncol):
        pst = P.pb[bank][:, :].bitcast(BF16).rearrange("p (k n) -> p k n", k=8)
        for k in range(8):
            pe.op(lambda h, k=k: h.transpose(out=pst[:, k, :], in_=hb[slot][:, k * 128:(k + 1) * 128], identity=P.identb),
                  reads=[rhb[slot], P.rconst], writes=[P.rpb[bank]])
        act.op(lambda h: h.activation(out=dst, in_=pst[:, :, 0:ncol], func=AF.Copy),
               reads=[P.rpb[bank]], writes=[rdst])

    make_h(xh, rxh, 0)
    transpose_h(0, 4, hTh, rhTh, 2)
    for j in range(8):
        pc, px = P.pb[2], P.pb[3]
        for k in range(8):
            pe.op(lambda h, k=k, j=j: h.matmul(pc[:, 0:2], lhsT=Win[:, k, D + j * 128:D + (j + 1) * 128], rhs=hTh[:, k, :],
                                               start=(k == 0), stop=(k == 7)),
                  reads=[rWin[1], rhTh], writes=[P.rpb[2]])
        for k in range(8):
            pe.op(lambda h, k=k, j=j: h.matmul(px[:, 0:2], lhsT=Win[:, k, 2 * D + j * 128:2 * D + (j + 1) * 128], rhs=hTh[:, k, :],
                                               start=(k == 0), stop=(k == 7)),
                  reads=[rWin[2], rhTh], writes=[P.rpb[3]])
        act.op(lambda h: h.activation(out=pcsb[:, 0:2], in_=pc[:, 0:2], func=AF.Copy), reads=[P.rpb[2]], writes=[rpcsb])
        dve.op(lambda h, j=j: h.scalar_tensor_tensor(out=uh[:, j, :], in0=pcsb[:, 0:2], scalar=hflag[:, 0:1], in1=px[:, 0:2],
                                                     op0=ALU.mult, op1=ALU.mult),
               reads=[rpcsb, P.rpb[3], rcw], writes=[ruh])

    for tt in range(TOK // 512):
        for q in range(4):
            ti = tt * 4 + q
            slot = ti % 2
            make_h(xres[:, ti, :], rx[ti], slot)
            transpose_h(slot, 4 + (ti % 2), hT[:, :, q * 128:(q + 1) * 128], rhT, 128)
        for j in range(8):
            banks = (1, 2, 3) if j % 2 == 0 else (6, 7, 0)
            pbk, pck, pxk = (P.pb[b] for b in banks)
            for which, bk in enumerate(banks):
                for k in range(8):
                    pe.op(lambda h, k=k, j=j, which=which, bk=bk: h.matmul(
                        P.pb[bk][:, :], lhsT=Win[:, k, which * D + j * 128:which * D + (j + 1) * 128], rhs=hT[:, k, :],
                        start=(k == 0), stop=(k == 7)),
                        reads=[rWin[which], rhT], writes=[P.rpb[bk]])
            us = u[j % 2]
            rus = ru[j % 2]
            ts = t1[j % 2]
            rts = rt1[j % 2]
            act.op(lambda h, pck=pck: h.activation(out=pcsb, in_=pck[:, :], func=AF.Copy), reads=[P.rpb[banks[1]]], writes=[rpcsb])
            dve.op(lambda h, us=us, pxk=pxk: h.tensor_tensor(out=us[:, 2:514], in0=pcsb, in1=pxk[:, :], op=ALU.mult),
                   reads=[rpcsb, P.rpb[banks[2]]], writes=[rus])
            pool.op(lambda h, us=us, j=j: h.tensor_copy(out=us[:, 0:2], in_=uh[:, j, :]), reads=[ruh], writes=[rus])
            dve.op(lambda h, us=us, ts=ts, j=j: h.tensor_scalar(out=ts, in0=us[:, 2:514], scalar1=cw[:, j * 3 + 2:j * 3 + 3], scalar2=None,
                                                                op0=ALU.mult), reads=[rus, rcw], writes=[rts])
            dve.op(lambda h, us=us, ts=ts, j=j: h.scalar_tensor_tensor(out=ts, in0=us[:, 1:513], scalar=cw[:, j * 3 + 1:j * 3 + 2], in1=ts,
                                                                       op0=ALU.mult, op1=ALU.add), reads=[rus, rcw, rts], writes=[rts])
            dve.op(lambda h, us=us, ts=ts, j=j: h.scalar_tensor_tensor(out=ts, in0=us[:, 0:512], scalar=cw[:, j * 3:j * 3 + 1], in1=ts,
                                                                       op0=ALU.mult, op1=ALU.add), reads=[rus, rcw, rts], writes=[rts])
            dve.op(lambda h, ts=ts, j=j, pbk=pbk: h.tensor_tensor(out=ycT[:, j, :], in0=ts, in1=pbk[:, :], op=ALU.mult),
                   reads=[rts, P.rpb[banks[0]]], writes=[rycT])
            pool.op(lambda h, us=us, j=j: h.tensor_copy(out=uh[:, j, :], in_=us[:, 512:514]), reads=[rus], writes=[ruh])
        for q in range(4):
            ti = tt * 4 + q
            for nh in range(2):
                bank = 4 + nh
                for k in range(8):
                    pe.op(lambda h, k=k, q=q, nh=nh, bank=bank: h.matmul(
                        P.pb[bank][:, :], lhsT=ycT[:, k, q * 128:(q + 1) * 128], rhs=Wout[:, k, nh * 512:(nh + 1) * 512],
                        start=(k == 0), stop=(k == 7)),
                        reads=[rycT, rWout], writes=[P.rpb[bank]])
                act.op(lambda h, nh=nh, bank=bank: h.activation(out=ysb[:, nh * 512:(nh + 1) * 512], in_=P.pb[bank][:, :], func=AF.Copy),
                       reads=[P.rpb[bank]], writes=[rxh])
            rms_rstd(P, ysb, rxh, junk, rjunk, cols[:, 1:2], rcols)
            dve.op(lambda h: h.scalar_tensor_tensor(out=tmp, in0=ysb, scalar=cols[:, 1:2], in1=mod[:, 2, :],
                                                    op0=ALU.mult, op1=ALU.mult),
                   reads=[rxh, rcols, rmod[2]], writes=[rtmp])
            dve.op(lambda h, ti=ti: h.tensor_tensor(out=xres[:, ti, :], in0=xres[:, ti, :], in1=tmp, op=ALU.add),
                   reads=[rx[ti], rtmp], writes=[rx[ti]])
    K.barrier()
    A.release(m0)
    P.stream_release(sm0)


def phase_moe(P, xres, rx, mod, rmod, wr_in, rb_in, ec_in, wg_in, wu_in, wd_in, sg_in, su_in, sd_in, xg, og, stop=None):
    K, A = P.K, P.arena
    pe, act, dve, pool, sp = P.pe, P.act, P.dve, P.pool, P.sp
    m0 = A.mark()
    sm0 = P.stream_mark()
    wr = A.alloc([128, 8, 73], F32)
    rbias = A.alloc([128, 72], F32)
    eC = A.alloc([128, 65], F32)
    cnt = A.alloc([128, 64], F32)
    idx = A.alloc([128, 2 * NT], I32)
    gts = A.alloc([128, 3 * NT], F32)
    triU = A.alloc([128, 128], BF16)
    ones = A.alloc([128, 128], BF16)
    h2T = A.alloc([128, 8, TOK], BF16)
    rwr, rcnt, rgts, rtri = (K.region() for _ in range(4))
    ridx = K.regions(NT, "idx")
    rh2T = K.regions(NT, "h2T")
    m1 = A.mark()
    hf = A.alloc([128, D], F32)
    hb = [A.alloc([128, D], BF16) for _ in range(2)]
    h2Tf = A.alloc([128, 8, 128], F32)
    junk = A.alloc([128, D], BF16)
    tmp = A.alloc([128, D], F32)
    sm = A.alloc([128, 1024], F32)
    rhf, rh2Tf, rjunk, rtmp = (K.region() for _ in range(4))
    rhb = K.regions(2, "hb")
    rsm = K.region()

    s = P.stream()
    sp.dma(s, lambda h: h.dma_start(out=wr, in_=wr_in.rearrange("(k p) n -> p k n", p=128)), writes=[rwr])
    s = P.stream()
    sp.dma(s, lambda h: h.dma_start(out=rbias, in_=rb_in.partition_broadcast(128)), writes=[rwr])
    s = P.stream()
    sp.dma(s, lambda h: h.dma_start(out=eC, in_=ec_in), writes=[rwr])
    pool.op(lambda h: h.memset(cnt, 0.0), writes=[rcnt])
    rogz = K.region("ogz")
    pool.op(lambda h: h.memset(junk, 0.0), writes=[rjunk])
    sz = P.stream()
    sp.dma(sz, lambda h: h.dma_start(out=og[NSLOT:NSLOT + 128, :], in_=junk), reads=[rjunk], writes=[rogz])
    pool.op(lambda h: h.memset(tmp[:, 0:128], 0.0), writes=[rtmp])
    pool.op(lambda h: h.affine_select(out=tmp[:, 0:128], in_=tmp[:, 0:128], pattern=[[-1, 128]], compare_op=ALU.is_ge,
                                      fill=1.0, base=0, channel_multiplier=1), reads=[rtmp], writes=[rtmp])
    pool.op(lambda h: h.tensor_copy(out=triU, in_=tmp[:, 0:128]), reads=[rtmp], writes=[rtri])
    pool.op(lambda h: h.memset(ones, 1.0), writes=[rtri])

    o = [0]

    def col(n):
        v = sm[:, o[0]:o[0] + n]
        o[0] += n
        return v
    lg = col(72)
    gmax, ngmax, sumexp, psel = col(1), col(1), col(1), col(1)
    goh, m18, ejunk = col(8), col(8), col(8)
    elm = col(64)
    top8 = col(8)
    sel = col(64)
    nv1, dcol, e2, gsc = col(1), col(1), col(1), col(1)
    ex = col(64)
    G = col(64)
    pos = col(64)
    val = col(64)
    val2 = col(64)
    mhi = col(64)
    vmax, vmax2, shi, slo, ghi, gtot = col(1), col(1), col(1), col(1), col(1), col(1)
    cols = col(4)
    selb = A.alloc([128, 64], BF16)

    sxg = [P.stream(sw=True), P.stream(sw=True)]
    rxg = K.regions(2 * NT, "xg")

    for ti in range(NT):
        slot = ti % 2
        src = xres[:, ti, :]
        rms_rstd(P, src, rx[ti], junk, rjunk, cols[:, 0:1], rsm)
        dve.op(lambda h, src=src: h.scalar_tensor_tensor(out=tmp, in0=src, scalar=cols[:, 0:1], in1=mod[:, 3, :],
                                                         op0=ALU.mult, op1=ALU.mult),
               reads=[rx[ti], rsm, rmod[3]], writes=[rtmp])
        dve.op(lambda h: h.tensor_tensor(out=hf, in0=tmp, in1=mod[:, 4, :], op=ALU.add),
               reads=[rtmp, rmod[4]], writes=[rhf])
        act.op(lambda h, slot=slot: h.activation(out=hb[slot], in_=hf, func=AF.Copy), reads=[rhf], writes=[rhb[slot]])
        pst = P.pb[0][:, :].bitcast(BF16).rearrange("p (k n) -> p k n", k=8)
        for k in range(8):
            pe.op(lambda h, k=k, slot=slot: h.transpose(out=pst[:, k, :], in_=hb[slot][:, k * 128:(k + 1) * 128], identity=P.identb),
                  reads=[rhb[slot], P.rconst], writes=[P.rpb[0]])
        act.op(lambda h, ti=ti: h.activation(out=h2T[:, :, ti * 128:(ti + 1) * 128], in_=pst, func=AF.Copy),
               reads=[P.rpb[0]], writes=[rh2T[ti]])
        for k in range(8):
            bank = 1 + k // 4
            pe.op(lambda h, k=k, bank=bank: h.transpose(out=P.pb[bank][:, (k % 4) * 128:(k % 4 + 1) * 128],
                                                        in_=hf[:, k * 128:(k + 1) * 128], identity=P.identf),
                  reads=[rhf, P.rconst], writes=[P.rpb[bank]])
        for half in range(2):
            act.op(lambda h, half=half: h.activation(out=h2Tf[:, half * 4:(half + 1) * 4, :],
                                                     in_=P.pb[1 + half][:, :].rearrange("p (k n) -> p k n", k=4), func=AF.Copy),
                   reads=[P.rpb[1 + half]], writes=[rh2Tf])
        for k in range(8):
            pe.op(lambda h, k=k: h.matmul(P.pb[3][:, 0:73], lhsT=h2Tf[:, k, :], rhs=wr[:, k, :], start=(k == 0), stop=(k == 7)),
                  reads=[rh2Tf, rwr], writes=[P.rpb[3]])
        R3 = [P.rpb[3]]
        S = [rsm]
        act.op(lambda h: h.activation(out=lg, in_=P.pb[3][:, 0:72], func=AF.Copy), reads=R3, writes=S)
        dve.op(lambda h: h.tensor_tensor(out=lg, in0=lg, in1=rbias, op=ALU.add), reads=S + [rwr], writes=S)
        act.op(lambda h, ti=ti: h.activation(out=gts[:, 2 * NT + ti:2 * NT + ti + 1], in_=P.pb[3][:, 72:73], func=AF.Sigmoid),
               reads=R3, writes=[rgts])
        dve.op(lambda h: h.tensor_reduce(out=gmax, in_=lg[:, 0:8], axis=AX.X, op=ALU.max), reads=S, writes=S)
        dve.op(lambda h: h.tensor_scalar(out=goh, in0=lg[:, 0:8], scalar1=gmax, scalar2=None, op0=ALU.is_equal), reads=S, writes=S)
        dve.op(lambda h: h.tensor_scalar(out=ngmax, in0=gmax, scalar1=-1.0, scalar2=None, op0=ALU.mult), reads=S, writes=S)
        act.op(lambda h: h.activation(out=ejunk, in_=lg[:, 0:8], func=AF.Exp, bias=ngmax, scale=1.0, accum_out=sumexp), reads=S, writes=S)
        dve.op(lambda h: h.reciprocal(out=psel, in_=sumexp), reads=S, writes=S)
        dve.op(lambda h: h.tensor_scalar(out=m18, in0=goh, scalar1=BIG, scalar2=-BIG, op0=ALU.mult, op1=ALU.add), reads=S, writes=S)
        dve.op(lambda h: h.tensor_tensor(out=elm.rearrange("p (g e) -> p g e", g=8), in0=lg[:, 8:72].rearrange("p (g e) -> p g e", g=8),
                                         in1=m18.rearrange("p (g o) -> p g o", o=1).to_broadcast([128, 8, 8]), op=ALU.add), reads=S, writes=S)
        dve.op(lambda h: h.max(out=top8, in_=elm), reads=S, writes=S)
        dve.op(lambda h: h.tensor_scalar(out=sel, in0=elm, scalar1=top8[:, 1:2], scalar2=None, op0=ALU.is_ge), reads=S, writes=S)
        dve.op(lambda h: h.tensor_copy(out=selb, in_=sel), reads=S, writes=S)
        dve.op(lambda h: h.tensor_scalar(out=nv1, in0=top8[:, 0:1], scalar1=-1.0, scalar2=None, op0=ALU.mult), reads=S, writes=S)
        dve.op(lambda h: h.tensor_scalar(out=ex, in0=elm, scalar1=nv1, scalar2=-80.0, op0=ALU.add, op1=ALU.max), reads=S, writes=S)
        act.op(lambda h: h.activation(out=ex, in_=ex, func=AF.Exp), reads=S, writes=S)
        dve.op(lambda h: h.tensor_tensor(out=dcol, in0=top8[:, 1:2], in1=top8[:, 0:1], op=ALU.subtract), reads=S, writes=S)
        act.op(lambda h: h.activation(out=e2, in_=dcol, func=AF.Exp), reads=S, writes=S)
        dve.op(lambda h: h.tensor_scalar(out=e2, in0=e2, scalar1=1.0, scalar2=None, op0=ALU.add), reads=S, writes=S)
        dve.op(lambda h: h.reciprocal(out=e2, in_=e2), reads=S, writes=S)
        dve.op(lambda h: h.tensor_tensor(out=gsc, in0=e2, in1=psel, op=ALU.mult), reads=S, writes=S)
        dve.op(lambda h: h.scalar_tensor_tensor(out=G, in0=ex, scalar=gsc, in1=sel, op0=ALU.mult, op1=ALU.mult), reads=S, writes=S)
        pe.op(lambda h: h.matmul(P.pb[4][:, 0:64], lhsT=triU, rhs=selb, start=True, stop=True), reads=[rtri, rsm], writes=[P.rpb[4]])
        pe.op(lambda h: h.matmul(P.pb[5][:, 0:64], lhsT=ones, rhs=selb, start=True, stop=True), reads=[rtri, rsm], writes=[P.rpb[5]])
        act.op(lambda h: h.activation(out=pos, in_=P.pb[4][:, 0:64], func=AF.Copy), reads=[P.rpb[4]], writes=S)
        act.op(lambda h: h.activation(out=val, in_=P.pb[5][:, 0:64], func=AF.Copy), reads=[P.rpb[5]], writes=S)
        dve.op(lambda h: h.tensor_tensor(out=pos, in0=pos, in1=cnt, op=ALU.add), reads=S + [rcnt], writes=S)
        dve.op(lambda h: h.tensor_tensor(out=cnt, in0=val, in1=cnt, op=ALU.add), reads=S + [rcnt], writes=[rcnt])
        dve.op(lambda h: h.tensor_scalar(out=val, in0=pos, scalar1=float(CAP), scalar2=None, op0=ALU.is_ge), reads=S, writes=S)
        dve.op(lambda h: h.tensor_tensor(out=pos, in0=pos, in1=eC[:, 0:64], op=ALU.add), reads=S + [rwr], writes=S)
        dve.op(lambda h: h.tensor_scalar(out=val2, in0=pos, scalar1=-1.0, scalar2=eC[:, 64:65], op0=ALU.mult, op1=ALU.add), reads=S + [rwr], writes=S)
        dve.op(lambda h: h.tensor_tensor(out=val2, in0=val2, in1=val, op=ALU.mult), reads=S, writes=S)
        dve.op(lambda h: h.tensor_tensor(out=pos, in0=pos, in1=val2, op=ALU.add), reads=S, writes=S)
        dve.op(lambda h: h.scalar_tensor_tensor(out=val, in0=pos, scalar=1.0, in1=sel, op0=ALU.add, op1=ALU.mult), reads=S, writes=S)
        dve.op(lambda h: h.tensor_scalar(out=val2, in0=pos, scalar1=-1.0, scalar2=BIGC, op0=ALU.mult, op1=ALU.add), reads=S, writes=S)
        dve.op(lambda h: h.tensor_tensor(out=val2, in0=val2, in1=sel, op=ALU.mult), reads=S, writes=S)
        dve.op(lambda h: h.tensor_reduce(out=vmax, in_=val, axis=AX.X, op=ALU.max), reads=S, writes=S)
        dve.op(lambda h: h.tensor_reduce(out=vmax2, in_=val2, axis=AX.X, op=ALU.max), reads=S, writes=S)
        dve.op(lambda h: h.tensor_scalar(out=shi, in0=vmax, scalar1=-1.0, scalar2=None, op0=ALU.add), reads=S, writes=S)
        dve.op(lambda h: h.tensor_scalar(out=slo, in0=vmax2, scalar1=-1.0, scalar2=BIGC, op0=ALU.mult, op1=ALU.add), reads=S, writes=S)
        dve.op(lambda h, ti=ti: h.tensor_copy(out=idx[:, ti:ti + 1], in_=slo), reads=S, writes=[ridx[ti]])
        dve.op(lambda h, ti=ti: h.tensor_copy(out=idx[:, NT + ti:NT + ti + 1], in_=shi), reads=S, writes=[ridx[ti]])
        dve.op(lambda h: h.tensor_scalar(out=mhi, in0=val, scalar1=vmax, scalar2=None, op0=ALU.is_equal), reads=S, writes=S)
        dve.op(lambda h: h.tensor_tensor(out=mhi, in0=mhi, in1=G, op=ALU.mult), reads=S, writes=S)
        dve.op(lambda h: h.tensor_reduce(out=ghi, in_=mhi, axis=AX.X, op=ALU.add), reads=S, writes=S)
        dve.op(lambda h: h.tensor_reduce(out=gtot, in_=G, axis=AX.X, op=ALU.add), reads=S, writes=S)
        dve.op(lambda h, ti=ti: h.tensor_copy(out=gts[:, NT + ti:NT + ti + 1], in_=ghi), reads=S, writes=[rgts])
        dve.op(lambda h, ti=ti: h.tensor_tensor(out=gts[:, ti:ti + 1], in0=gtot, in1=ghi, op=ALU.subtract), reads=S, writes=[rgts])
        for w in range(2):
            pool.dma(sxg[w], lambda h, w=w, ti=ti, slot=slot: h.indirect_dma_start(
                out=xg, out_offset=bass.IndirectOffsetOnAxis(ap=idx[:, w * NT + ti:w * NT + ti + 1], axis=0),
                in_=hb[slot], in_offset=None),
                reads=[rhb[slot], ridx[ti]], writes=[rxg[2 * ti + w]])
    K.barrier()
    A.release(m1)
    if stop == "route":
        A.release(m0)
        P.stream_release(sm0)
        return

    NW = 3
    wgs = [A.alloc([128, 8, 256], BF16) for _ in range(NW)]
    wus = [A.alloc([128, 8, 256], BF16) for _ in range(NW)]
    wds = [A.alloc([128, 2, D], BF16) for _ in range(NW)]
    rwg, rwu, rwd = K.regions(NW, "ewg"), K.regions(NW, "ewu"), K.regions(NW, "ewd")
    swg, swu, swd = ([P.stream(sw=True) for _ in range(NW)] for _ in range(3))
    NXB = 4
    xb = [A.alloc([128, D], BF16) for _ in range(NXB)]
    rxb = K.regions(NXB, "xb")
    sxb = [P.stream() for _ in range(NXB)]
    xbT = [A.alloc([128, 8, 256], BF16) for _ in range(2)]
    rxbT = K.regions(2, "xbT")
    sg = [A.alloc([128, 2, 256], F32) for _ in range(2)]
    rsg = K.regions(2, "sg")
    hid = [A.alloc([128, 2, 256], BF16) for _ in range(2)]
    rhid = K.regions(2, "hid")
    osb = [A.alloc([128, D], BF16) for _ in range(2)]
    rosb = K.regions(2, "osb")
    sob = [P.stream(), P.stream()]
    rog = K.regions(2 * NEXP, "og")

    def issue_weights(e):
        ws = e % NW
        pool.dma(swg[ws], lambda h: h.dma_start(out=wgs[ws], in_=wg_in[e].rearrange("(k p) n -> p k n", p=128)), writes=[rwg[ws]])
        pool.dma(swu[ws], lambda h: h.dma_start(out=wus[ws], in_=wu_in[e].rearrange("(k p) n -> p k n", p=128)), writes=[rwu[ws]])
        pool.dma(swd[ws], lambda h: h.dma_start(out=wds[ws], in_=wd_in[e].rearrange("(k p) n -> p k n", p=128)), writes=[rwd[ws]])

    def issue_loads(e):
        for blk in range(2):
            n = e * 2 + blk
            xs = n % NXB
            sp.dma(sxb[xs], lambda h, n=n, xs=xs: h.dma_start(out=xb[xs], in_=xg[n * 128:(n + 1) * 128, :]),
                   reads=rxg if n == 0 else [], writes=[rxb[xs]])

    import os
    NRUN = int(os.environ.get("NEXP_RUN", NEXP))
    STAGE = int(os.environ.get("EXP_STAGE", 9))
    issue_weights(0)
    issue_weights(1)
    issue_loads(0)
    for e in range(NRUN):
        ws = e % NW
        es = e % 2
        if e + 2 < NRUN:
            issue_weights(e + 2)
        if e + 1 < NRUN:
            issue_loads(e + 1)
        if STAGE < 2:
            continue
        for blk in range(2):
            n = e * 2 + blk
            xs = n % NXB
            bank = n % 2
            pst = P.pb[bank][:, :].bitcast(BF16).rearrange("p (k n) -> p k n", k=8)
            for k in range(8):
                pe.op(lambda h, k=k, xs=xs, pst=pst: h.transpose(out=pst[:, k, :], in_=xb[xs][:, k * 128:(k + 1) * 128], identity=P.identb),
                      reads=[rxb[xs], P.rconst], writes=[P.rpb[bank]])
            act.op(lambda h, es=es, blk=blk, pst=pst: h.activation(out=xbT[es][:, :, blk * 128:(blk + 1) * 128], in_=pst, func=AF.Copy),
                   reads=[P.rpb[bank]], writes=[rxbT[es]])
        if STAGE < 3:
            continue
        for which, (wsrc, rws_, bank) in enumerate(((wgs, rwg, 2), (wus, rwu, 3))):
            for hc in range(2):
                for k in range(8):
                    pe.op(lambda h, k=k, hc=hc, wsrc=wsrc, bank=bank, ws=ws, es=es: h.matmul(
                        P.pb[bank][:, hc * 256:(hc + 1) * 256], lhsT=wsrc[ws][:, k, hc * 128:(hc + 1) * 128], rhs=xbT[es][:, k, :],
                        start=(k == 0), stop=(k == 7)),
                        reads=[rws_[ws], rxbT[es]], writes=[P.rpb[bank]])
        act.op(lambda h, es=es: h.activation(out=sg[es].rearrange("p a b -> p (a b)"), in_=P.pb[2][:, :], func=AF.Silu),
               reads=[P.rpb[2]], writes=[rsg[es]])
        dve.op(lambda h, es=es: h.tensor_tensor(out=hid[es].rearrange("p a b -> p (a b)"), in0=sg[es].rearrange("p a b -> p (a b)"),
                                                in1=P.pb[3][:, :], op=ALU.mult),
               reads=[rsg[es], P.rpb[3]], writes=[rhid[es]])
        if STAGE < 4:
            continue
        for blk in range(2):
            osl = blk
            for nh in range(2):
                bank = 4 + blk * 2 + nh
                for hc in range(2):
                    pe.op(lambda h, hc=hc, nh=nh, blk=blk, bank=bank, ws=ws, es=es: h.matmul(
                        P.pb[bank][:, :], lhsT=hid[es][:, hc, blk * 128:(blk + 1) * 128], rhs=wds[ws][:, hc, nh * 512:(nh + 1) * 512],
                        start=(hc == 0), stop=(hc == 1)),
                        reads=[rhid[es], rwd[ws]], writes=[P.rpb[bank]])
                if nh == 0:
                    act.op(lambda h, bank=bank, osl=osl: h.activation(out=osb[osl][:, 0:512], in_=P.pb[bank][:, :], func=AF.Copy),
                           reads=[P.rpb[bank]], writes=[rosb[osl]])
                else:
                    dve.op(lambda h, bank=bank, osl=osl: h.tensor_copy(out=osb[osl][:, 512:1024], in_=P.pb[bank][:, :]),
                           reads=[P.rpb[bank]], writes=[rosb[osl]])
            if STAGE < 5:
                continue
            sp.dma(sob[osl], lambda h, e=e, blk=blk, osl=osl: h.dma_start(out=og[(e * 2 + blk) * 128:(e * 2 + blk + 1) * 128, :], in_=osb[osl]),
                   reads=[rosb[osl]], writes=[rog[e * 2 + blk]])
    K.barrier()
    A.release(m1)
    if stop == "experts":
        A.release(m0)
        P.stream_release(sm0)
        return

    wsg = A.alloc([128, 8, 512], BF16)
    wsu = A.alloc([128, 8, 512], BF16)
    wsd = A.alloc([128, 4, D], BF16)
    rws = K.regions(3, "ws")
    for i, (dst, srcw) in enumerate(((wsg, sg_in), (wsu, su_in), (wsd, sd_in))):
        s = P.stream(sw=True)
        pool.dma(s, lambda h, dst=dst, srcw=srcw: h.dma_start(out=dst, in_=srcw.rearrange("(k p) n -> p k n", p=128)), writes=[rws[i]])
    hsT = A.alloc([128, 4, 512], BF16)
    rhsT = K.region()
    ssg = A.alloc([128, 512], F32)
    rssg = K.region()
    ra = [A.alloc([128, D], BF16) for _ in range(2)]
    rbb = [A.alloc([128, D], BF16) for _ in range(2)]
    rra = K.regions(2, "ra")
    rrb = K.regions(2, "rb")
    sga = [P.stream(sw=True), P.stream(sw=True)]
    sgb = [P.stream(sw=True), P.stream(sw=True)]
    acc = A.alloc([128, D], F32)
    racc = K.region()
    junk = A.alloc([128, D], BF16)
    rjunk = K.region()
    tmp = A.alloc([128, D], F32)
    rtmp = K.region()
    cols = A.alloc([128, 4], F32)
    rcols = K.region()
    for tt in range(TOK // 512):
        for hc in range(4):
            for which, (wsrc, bank) in enumerate(((wsg, 0), (wsu, 1))):
                for k in range(8):
                    pe.op(lambda h, k=k, hc=hc, wsrc=wsrc, bank=bank, tt=tt: h.matmul(
                        P.pb[bank][:, :], lhsT=wsrc[:, k, hc * 128:(hc + 1) * 128], rhs=h2T[:, k, tt * 512:(tt + 1) * 512],
                        start=(k == 0), stop=(k == 7)),
                        reads=[rws[which]] + rh2T[tt * 4:tt * 4 + 4], writes=[P.rpb[bank]])
            act.op(lambda h: h.activation(out=ssg, in_=P.pb[0][:, :], func=AF.Silu), reads=[P.rpb[0]], writes=[rssg])
            dve.op(lambda h, hc=hc: h.tensor_tensor(out=hsT[:, hc, :], in0=ssg, in1=P.pb[1][:, :], op=ALU.mult),
                   reads=[rssg, P.rpb[1]], writes=[rhsT])
        for q in range(4):
            ti = tt * 4 + q
            gs = ti % 2
            pool.op(lambda h, gs=gs: h.memset(ra[gs], 0.0), writes=[rra[gs]])
            pool.op(lambda h, gs=gs: h.memset(rbb[gs], 0.0), writes=[rrb[gs]])
            pool.dma(sga[gs], lambda h, gs=gs, ti=ti: h.indirect_dma_start(
                out=ra[gs], out_offset=None, in_=og, in_offset=bass.IndirectOffsetOnAxis(ap=idx[:, ti:ti + 1], axis=0)), reads=rog + [rogz, ridx[ti]], writes=[rra[gs]])
            pool.dma(sgb[gs], lambda h, gs=gs, ti=ti: h.indirect_dma_start(
                out=rbb[gs], out_offset=None, in_=og, in_offset=bass.IndirectOffsetOnAxis(ap=idx[:, NT + ti:NT + ti + 1], axis=0)), reads=rog + [rogz, ridx[ti]], writes=[rrb[gs]])
            for nh in range(2):
                bank = 2 + nh
                for hc in range(4):
                    pe.op(lambda h, hc=hc, nh=nh, q=q, bank=bank: h.matmul(
                        P.pb[bank][:, :], lhsT=hsT[:, hc, q * 128:(q + 1) * 128], rhs=wsd[:, hc, nh * 512:(nh + 1) * 512],
                        start=(hc == 0), stop=(hc == 3)),
                        reads=[rhsT, rws[2]], writes=[P.rpb[bank]])
                act.op(lambda h, nh=nh, bank=bank, ti=ti: h.activation(out=acc[:, nh * 512:(nh + 1) * 512], in_=P.pb[bank][:, :], func=AF.Copy,
                                                                       scale=gts[:, 2 * NT + ti:2 * NT + ti + 1]),
                       reads=[P.rpb[bank], rgts], writes=[racc])
            dve.op(lambda h, gs=gs, ti=ti: h.scalar_tensor_tensor(out=acc, in0=ra[gs], scalar=gts[:, ti:ti + 1], in1=acc,
                                                                  op0=ALU.mult, op1=ALU.add), reads=[rra[gs], rgts, racc], writes=[racc])
            dve.op(lambda h, gs=gs, ti=ti: h.scalar_tensor_tensor(out=acc, in0=rbb[gs], scalar=gts[:, NT + ti:NT + ti + 1], in1=acc,
                                                                  op0=ALU.mult, op1=ALU.add), reads=[rrb[gs], rgts, racc], writes=[racc])
            rms_rstd(P, acc, racc, junk, rjunk, cols[:, 0:1], rcols)
            dve.op(lambda h: h.scalar_tensor_tensor(out=tmp, in0=acc, scalar=cols[:, 0:1], in1=mod[:, 5, :],
                                                    op0=ALU.mult, op1=ALU.mult), reads=[racc, rcols, rmod[5]], writes=[rtmp])
            dve.op(lambda h, ti=ti: h.tensor_tensor(out=xres[:, ti, :], in0=xres[:, ti, :], in1=tmp, op=ALU.add),
                   reads=[rx[ti], rtmp], writes=[rx[ti]])
    K.barrier()
    A.release(m0)
    P.stream_release(sm0)


def load_x(P, xres, rx, x_in):
    for g in range(4):
        s = P.stream()
        P.sp.dma(s, lambda h, g=g: h.dma_start(out=xres[:, g * 4:(g + 1) * 4, :],
                                               in_=x_in[g * 512:(g + 1) * 512, :].rearrange("(t p) d -> p t d", p=128)),
                 writes=rx[g * 4:(g + 1) * 4])


def store_x(P, xres, rx, out):
    s = P.stream()
    for g in range(4):
        P.sp.dma(s, lambda h, g=g: h.dma_start(out=out[g * 512:(g + 1) * 512, :].rearrange("(t p) d -> p t d", p=128),
                                               in_=xres[:, g * 4:(g + 1) * 4, :]),
                 reads=rx[g * 4:(g + 1) * 4])
    P.sp.wait_clock(s)


def moe_inputs(P, tag):
    d = {}
    d["wr"] = P.dram(f"wr{tag}", [D, 73], F32, "ExternalInput")
    d["rb"] = P.dram(f"rb{tag}", [1, 72], F32, "ExternalInput")
    d["wg"] = P.dram(f"wg{tag}", [NEXP, D, 256], F32, "ExternalInput")
    d["wu"] = P.dram(f"wu{tag}", [NEXP, D, 256], F32, "ExternalInput")
    d["wd"] = P.dram(f"wd{tag}", [NEXP, 256, D], F32, "ExternalInput")
    d["sg"] = P.dram(f"sg{tag}", [D, 512], F32, "ExternalInput")
    d["su"] = P.dram(f"su{tag}", [D, 512], F32, "ExternalInput")
    d["sd"] = P.dram(f"sd{tag}", [512, D], F32, "ExternalInput")
    return d


def adaln_inputs(P, tag):
    return dict(c_col=P.dram(f"c_col{tag}", [128, 8], F32, "ExternalInput"),
                ada_w=P.dram(f"ada_w{tag}", [D, 6 * D], F32, "ExternalInput"),
                ada_b=P.dram(f"ada_b{tag}", [1, 6 * D], F32, "ExternalInput"),
                norm_w=P.dram(f"norm_w{tag}", [4, D], F32, "ExternalInput"))


def build_prog1(stop_after=None):
    nc = bass.Bass("TRN2", target_bir_lowering=False)
    with ExitStack() as st:
        P = Prog(nc, st)
        K, A = P.K, P.arena
        x_in = P.dram("x_in", [TOK, D], F32, "ExternalInput")
        x_halo = P.dram("x_halo", [2, D], F32, "ExternalInput")
        hflag = P.dram("hflag", [128, 1], F32, "ExternalInput")
        ad = adaln_inputs(P, "0")
        w_in = P.dram("conv_in_w", [D, 3 * D], F32, "ExternalInput")
        cw = P.dram("conv_cw", [128, 24], F32, "ExternalInput")
        w_out = P.dram("conv_out_w", [D, D], F32, "ExternalInput")
        ec = P.dram("ec", [128, 65], F32, "ExternalInput")
        mo = moe_inputs(P, "0")
        xg = P.dram("xg", [NSLOT + 128, D], BF16, "Internal")
        og = P.dram("og", [NSLOT + 128, D], BF16, "Internal")
        out = P.dram("x_out", [TOK, D], F32, "ExternalOutput")

        xres = A.alloc([128, NT, D], F32)
        rx = K.regions(NT, "x")
        mod = A.alloc([128, 6, D], F32)
        rmod = K.regions(6, "mod")
        load_x(P, xres, rx, x_in)
        phase_adaln(P, mod, rmod, ad["c_col"], ad["ada_w"], ad["ada_b"], ad["norm_w"])
        if stop_after != "adaln":
            phase_conv(P, xres, rx, mod, rmod, x_halo, hflag, w_in, cw, w_out)
            if stop_after != "conv":
                phase_moe(P, xres, rx, mod, rmod, mo["wr"], mo["rb"], ec, mo["wg"], mo["wu"], mo["wd"],
                          mo["sg"], mo["su"], mo["sd"], xg, og, stop=stop_after)
        if stop_after == "adaln":
            s = P.stream()
            P.sp.dma(s, lambda h: h.dma_start(out=out[0:128 * 6, :].rearrange("(t p) d -> p t d", p=128), in_=mod), reads=rmod)
            P.sp.wait_clock(s)
        else:
            store_x(P, xres, rx, out)
        K.finish()
        print("prog1 ops", K.nops, "dma", K.ndma, "arena peak", A.peak)
    return nc


def _c(a):
    return np.ascontiguousarray(a, dtype=np.float32)


def host_common(inputs, layer):
    i = layer
    d = {}
    t = str(i)
    d[f"ada_w{t}"] = _c(inputs["ada_w"][i])
    d[f"ada_b{t}"] = _c(inputs["ada_b"][i][None, :])
    d[f"norm_w{t}"] = _c(inputs["norm_w"][i])
    d[f"wr{t}"] = _c(np.concatenate([inputs["moe_group_w"][i], inputs["moe_expert_w"][i], inputs["shared_gate_w"][i]], axis=1))
    d[f"rb{t}"] = _c(np.concatenate([inputs["moe_group_b"][i], inputs["moe_expert_b"][i]])[None, :])
    d[f"wg{t}"] = _c(inputs["moe_w_gate"][i])
    d[f"wu{t}"] = _c(inputs["moe_w_up"][i])
    d[f"wd{t}"] = _c(inputs["moe_w_down"][i])
    d[f"sg{t}"] = _c(inputs["shared_w_gate"][i])
    d[f"su{t}"] = _c(inputs["shared_w_up"][i])
    d[f"sd{t}"] = _c(inputs["shared_w_down"][i])
    return d


def c_col_of(c, b):
    return _c(np.asarray(c)[b].reshape(8, 128).T)


EC = np.concatenate([np.tile((np.arange(NEXP, dtype=np.float32) * CAP)[None, :], (128, 1)),
                     (NSLOT + np.arange(128, dtype=np.float32))[:, None]], axis=1).astype(np.float32)


def run_prog1(inputs, stop_after=None):
    x = np.asarray(inputs["x"])
    nc = build_prog1(stop_after)
    com = host_common(inputs, 0)
    com["conv_in_w"] = _c(inputs["conv_in_w"][0])
    com["conv_out_w"] = _c(inputs["conv_out_w"][0])
    com["conv_cw"] = _c(np.asarray(inputs["conv_w"][0]).reshape(3, 8, 128).transpose(2, 1, 0).reshape(128, 24))
    com["ec"] = EC
    in_maps = []
    for core in range(NCORES):
        b, hf = core // 2, core % 2
        m = dict(com)
        m["x_in"] = _c(x[b, hf * TOK:(hf + 1) * TOK])
        m["x_halo"] = _c(x[b, TOK - 2:TOK]) if hf == 1 else np.zeros((2, D), np.float32)
        m["hflag"] = np.full((128, 1), float(hf), np.float32)
        m["c_col0"] = c_col_of(inputs["c"], b)
        in_maps.append(m)
    import os
    ncr = int(os.environ.get("NCORES_RUN", NCORES))
    res = run_bass_kernel_spmd(nc, in_maps[:ncr], core_ids=list(range(ncr)))
    outs = [r["x_out"] for r in res.results]
    outs = outs + [np.zeros_like(outs[0])] * (NCORES - ncr)
    xo = np.stack(outs).reshape(4, SEQ, D)
    return xo


HL = 4
GT = SEQ // 128
QSCALE = 128.0 ** -0.5


def phase_gdn(P, mod, rmod, x_in, h_in, wq_in, wba_in, cw_in, hp_in, gnw_in, out):
    import os
    K, A = P.K, P.arena
    pe, act, dve, pool, sp = P.pe, P.act, P.dve, P.pool, P.sp
    m0 = A.mark()
    sm0 = P.stream_mark()
    W = A.alloc([128, 8, 2048], BF16)
    Wba = A.alloc([128, 8, 8], BF16)
    cw = A.alloc([128, 48], F32)
    hp = A.alloc([128, 8], F32)
    negA = A.alloc([128, 4], F32)
    gnw = A.alloc([128, 128], F32)
    Mtri = A.alloc([128, 128], F32)
    SL = A.alloc([128, 128], F32)
    onesf = A.alloc([128, 128], F32)
    onesb = A.alloc([128, 128], BF16)
    rW = K.regions(4, "W")
    rc = K.region("c2")
    for i in range(4):
        s = P.stream(sw=True)
        pool.dma(s, lambda h, i=i: h.dma_start(out=W[:, :, i * 512:(i + 1) * 512],
                                               in_=wq_in[:, i * 512:(i + 1) * 512].rearrange("(k p) n -> p k n", p=128)), writes=[rW[i]])
    s = P.stream(sw=True)
    pool.dma(s, lambda h: h.dma_start(out=Wba, in_=wba_in.rearrange("(k p) n -> p k n", p=128)), writes=[rc])
    s = P.stream()
    sp.dma(s, lambda h: h.dma_start(out=cw, in_=cw_in), writes=[rc])
    s = P.stream()
    sp.dma(s, lambda h: h.dma_start(out=hp, in_=hp_in.partition_broadcast(128)), writes=[rc])
    s = P.stream()
    sp.dma(s, lambda h: h.dma_start(out=gnw, in_=gnw_in.partition_broadcast(128)), writes=[rc])
    act.op(lambda h: h.activation(out=negA, in_=hp[:, 0:4], func=AF.Exp), reads=[rc], writes=[rc])
    dve.op(lambda h: h.tensor_scalar(out=negA, in0=negA, scalar1=-1.0, scalar2=None, op0=ALU.mult), reads=[rc], writes=[rc])
    pool.op(lambda h: h.memset(onesf, 1.0), writes=[rc])
    pool.op(lambda h: h.memset(onesb, 1.0), writes=[rc])
    pool.op(lambda h: h.memset(Mtri, 1.0), writes=[rc])
    pool.op(lambda h: h.affine_select(out=Mtri, in_=Mtri, pattern=[[1, 128]], compare_op=ALU.is_ge, fill=0.0,
                                      base=0, channel_multiplier=-1), reads=[rc], writes=[rc])
    pool.op(lambda h: h.memset(SL, 1.0), writes=[rc])
    pool.op(lambda h: h.affine_select(out=SL, in_=SL, pattern=[[-1, 128]], compare_op=ALU.is_ge, fill=0.0,
                                      base=-1, channel_multiplier=1), reads=[rc], writes=[rc])

    import os
    GTR = int(os.environ.get("GT_RUN", GT))
    GST = int(os.environ.get("GDN_STAGE", 99))
    BT = 256
    NB = SEQ // BT
    xt = [A.alloc([128, D], F32) for _ in range(2)] if h_in is None else None
    rxt = K.regions(2, "xt")
    sxt = [P.stream(), P.stream()]
    hb = [A.alloc([128, D], BF16) for _ in range(2)]
    rhb = K.regions(2, "hb")
    hTb = [A.alloc([128, 8, BT], BF16) for _ in range(2)]
    rhTb = K.regions(2, "hTb")
    junk = A.alloc([128, D], BF16)
    tmp = A.alloc([128, D], F32) if h_in is None else None
    cols = A.alloc([128, 8], F32)
    rjunk, rtmp, rcols = K.region(), K.region(), K.region()
    pre = A.alloc([128, 12, BT + 3], F32)
    rpre = K.regions(12, "pre")
    qkv = A.alloc([128, 12, BT], F32)
    rqkv = K.regions(12, "qkv")
    acc = [A.alloc([128, BT], F32) for _ in range(2)]
    racc = K.regions(2, "acc")
    sq = [A.alloc([128, BT], BF16) for _ in range(2)]
    rsq = K.regions(2, "sq")
    rn = [A.alloc([128, BT], F32) for _ in range(2)]
    rrn = K.regions(2, "rn")
    zs = [A.alloc([128, 2, 512], BF16) for _ in range(2)]
    rzs = [K.regions(2, "zsa"), K.regions(2, "zsb")]
    sm = A.alloc([128, 2, 8, 8], F32)
    rsm = K.regions(2, "sm")
    sm2 = A.alloc([128, 16], F32)
    rsm2 = K.region()

    def F(name):
        return [[A.alloc([128, 128], F32) for _ in range(HL)] for _ in range(2)], \
               [K.regions(HL, name + "a"), K.regions(HL, name + "b")]
    Ub, rU = F("U")
    WTb, rWT = F("WT")
    ATb, rAT = F("AT")
    kdb, rkd = F("kd")
    QTb, rQT = F("QT")

    def G(name):
        return [A.alloc([128, 128], F32) for _ in range(HL)], K.regions(HL, name)
    Kbg, rKbg = G("Kbg")
    Vb, rVb = G("Vb")
    dec, rdec = G("dec")
    decT, rdecT = G("decT")
    TriG, rTriG = G("TriG")
    t1b, rt1 = G("t1")
    Lm, rL = G("L")
    Nm, rN = G("N")
    Xa, rXa = G("Xa")
    Xb_, rXb = G("Xb")
    Pa, rPa = G("Pa")
    Pb_, rPb = G("Pb")
    PTa, rPTa = G("PTa")
    PTb, rPTb = G("PTb")
    stgA, rstgA = G("stgA")
    stgB, rstgB = G("stgB")
    vnew, rvnew = G("vnew")
    o1s, ro1s = G("o1s")
    otok, rotok = G("otok")
    Sst = [[A.alloc([128, 128], F32) for _ in range(HL)] for _ in range(2)]
    rS = [K.regions(HL, "Sa"), K.regions(HL, "Sb")]
    ogt = [A.alloc([128, 512], BF16) for _ in range(2)]
    rogt = K.regions(2, "ogt")
    sog = [P.stream(), P.stream()]
    for h_ in range(HL):
        pool.op(lambda h, h_=h_: h.memset(Sst[0][h_], 0.0), writes=[rS[0][h_]])
    for c in range(12):
        pool.op(lambda h, c=c: h.memset(pre[:, c, 0:3], 0.0), writes=[rpre[c]])

    ring = [0]

    def nb():
        b = ring[0] % 8
        ring[0] += 1
        return b

    def load_tile(ti):
        sl = ti % 2
        if h_in is None:
            sp.dma(sxt[sl], lambda h: h.dma_start(out=xt[sl], in_=x_in[ti * 128:(ti + 1) * 128, :]), writes=[rxt[sl]])
        else:
            r_, c_, i0 = ti // 16, (ti % 16) // 8, (ti % 8) * 128
            row = c_ * 2048 + r_ * 1024 + i0
            sp.dma(sxt[sl], lambda h: h.dma_start(out=hb[sl], in_=h_in[row:row + 128, :]), writes=[rhb[sl]])

    load_tile(0)
    load_tile(1)

    def project_block(bb):
        bp = bb % 2
        for q in range(2):
            ti = bb * 2 + q
            sl = ti % 2
            if h_in is None:
                rms_rstd(P, xt[sl], rxt[sl], junk, rjunk, cols[:, 0:1], rcols)
                dve.op(lambda h: h.scalar_tensor_tensor(out=tmp, in0=xt[sl], scalar=cols[:, 0:1], in1=mod[:, 0, :],
                                                        op0=ALU.mult, op1=ALU.mult), reads=[rxt[sl], rcols, rmod[0]], writes=[rtmp])
                dve.op(lambda h: h.tensor_tensor(out=hb[sl], in0=tmp, in1=mod[:, 1, :], op=ALU.add),
                       reads=[rtmp, rmod[1]], writes=[rhb[sl]])
                if ti + 2 < GTR:
                    load_tile(ti + 2)
            bank = nb()
            pst = P.pb[bank][:, :].bitcast(BF16).rearrange("p (k n) -> p k n", k=8)
            for k in range(8):
                pe.op(lambda h, k=k: h.transpose(out=pst[:, k, :], in_=hb[sl][:, k * 128:(k + 1) * 128], identity=P.identb),
                      reads=[rhb[sl], P.rconst], writes=[P.rpb[bank]])
            if h_in is not None and ti + 2 < GTR:
                load_tile(ti + 2)
            act.op(lambda h: h.activation(out=hTb[bp][:, :, q * 128:(q + 1) * 128], in_=pst, func=AF.Copy),
                   reads=[P.rpb[bank]], writes=[rhTb[bp]])
        for c in range(12):
            bank = nb()
            for k in range(8):
                pe.op(lambda h, k=k: h.matmul(P.pb[bank][:, 0:BT], lhsT=W[:, k, c * 128:(c + 1) * 128], rhs=hTb[bp][:, k, :],
                                              start=(k == 0), stop=(k == 7)), reads=[rW[c // 4], rhTb[bp]], writes=[P.rpb[bank]])
            act.op(lambda h: h.activation(out=pre[:, c, 3:3 + BT], in_=P.pb[bank][:, 0:BT], func=AF.Copy),
                   reads=[P.rpb[bank]], writes=[rpre[c]])
            a = acc[c % 2]
            ra = racc[c % 2]
            pool.op(lambda h: h.tensor_scalar(out=a, in0=pre[:, c, 0:BT], scalar1=cw[:, c * 4:c * 4 + 1], scalar2=None, op0=ALU.mult),
                    reads=[rpre[c], rc], writes=[ra])
            dve.op(lambda h: h.scalar_tensor_tensor(out=a, in0=pre[:, c, 1:1 + BT], scalar=cw[:, c * 4 + 1:c * 4 + 2], in1=a,
                                                    op0=ALU.mult, op1=ALU.add), reads=[rpre[c], rc, ra], writes=[ra])
            dve.op(lambda h: h.scalar_tensor_tensor(out=a, in0=pre[:, c, 2:2 + BT], scalar=cw[:, c * 4 + 2:c * 4 + 3], in1=a,
                                                    op0=ALU.mult, op1=ALU.add), reads=[rpre[c], rc, ra], writes=[ra])
            dve.op(lambda h: h.scalar_tensor_tensor(out=a, in0=pre[:, c, 3:3 + BT], scalar=cw[:, c * 4 + 3:c * 4 + 4], in1=a,
                                                    op0=ALU.mult, op1=ALU.add), reads=[rpre[c], rc, ra], writes=[ra])
            act.op(lambda h: h.activation(out=qkv[:, c, :], in_=a, func=AF.Silu), reads=[ra], writes=[rqkv[c]])
            pool.op(lambda h: h.tensor_copy(out=pre[:, c, 0:3], in_=pre[:, c, BT:BT + 3]), reads=[rpre[c]], writes=[rpre[c]])
            if c < 8:
                s_ = sq[c % 2]
                act.op(lambda h: h.activation(out=s_, in_=qkv[:, c, :], func=AF.Square), reads=[rqkv[c]], writes=[rsq[c % 2]])
                bank2 = nb()
                pe.op(lambda h: h.matmul(P.pb[bank2][:, 0:BT], lhsT=onesb, rhs=s_, start=True, stop=True),
                      reads=[rc, rsq[c % 2]], writes=[P.rpb[bank2]])
                r_ = rn[c % 2]
                act.op(lambda h: h.activation(out=r_, in_=P.pb[bank2][:, 0:BT], func=AF.Sqrt, bias=EPS, scale=1.0),
                       reads=[P.rpb[bank2]], writes=[rrn[c % 2]])
                dve.op(lambda h: h.reciprocal(out=r_, in_=r_), reads=[rrn[c % 2]], writes=[rrn[c % 2]])
                sc = QSCALE if c < 4 else 1.0
                dve.op(lambda h: h.scalar_tensor_tensor(out=qkv[:, c, :], in0=qkv[:, c, :], scalar=sc, in1=r_,
                                                        op0=ALU.mult, op1=ALU.mult), reads=[rqkv[c], rrn[c % 2]], writes=[rqkv[c]])
        for q in range(2):
            bank = nb()
            for k in range(8):
                pe.op(lambda h, k=k: h.matmul(P.pb[bank][:, :], lhsT=hTb[bp][:, k, q * 128:(q + 1) * 128], rhs=W[:, k, 1536:2048],
                                              start=(k == 0), stop=(k == 7)), reads=[rhTb[bp], rW[3]], writes=[P.rpb[bank]])
            act.op(lambda h: h.activation(out=zs[bp][:, q, :], in_=P.pb[bank][:, :], func=AF.Silu), reads=[P.rpb[bank]], writes=[rzs[bp][q]])
        bank = nb()
        for q in range(2):
            for k in range(8):
                pe.op(lambda h, k=k: h.matmul(P.pb[bank][:, q * 8:(q + 1) * 8], lhsT=hTb[bp][:, k, q * 128:(q + 1) * 128], rhs=Wba[:, k, :],
                                              start=(k == 0), stop=(k == 7)), reads=[rhTb[bp], rc], writes=[P.rpb[bank]])
        S_ = sm[:, bp]
        R = [rsm[bp]]
        pba = P.pb[bank][:, 0:16].rearrange("p (q j) -> p q j", q=2)
        v3 = lambda kind: S_[:, kind, :].rearrange("p (q j) -> p q j", q=2)
        act.op(lambda h: h.activation(out=v3(0), in_=pba[:, :, 0:4], func=AF.Sigmoid), reads=[P.rpb[bank]], writes=R)
        act.op(lambda h: h.activation(out=v3(1), in_=pba[:, :, 4:8], func=AF.Copy), reads=[P.rpb[bank]], writes=R)
        dve.op(lambda h: h.tensor_tensor(out=v3(1), in0=v3(1), in1=hp[:, 4:8].rearrange("p (o j) -> p o j", o=1).to_broadcast([128, 2, 4]),
                                         op=ALU.add), reads=R + [rc], writes=R)
        act.op(lambda h: h.activation(out=S_[:, 1, :], in_=S_[:, 1, :], func=AF.Exp), reads=R, writes=R)
        act.op(lambda h: h.activation(out=S_[:, 1, :], in_=S_[:, 1, :], func=AF.Ln, bias=1.0, scale=1.0), reads=R, writes=R)
        dve.op(lambda h: h.tensor_tensor(out=v3(1), in0=v3(1), in1=negA.rearrange("p (o j) -> p o j", o=1).to_broadcast([128, 2, 4]),
                                         op=ALU.mult), reads=R + [rc], writes=R)
        bank = nb()
        pe.op(lambda h: h.matmul(P.pb[bank][:, 0:8], lhsT=Mtri, rhs=S_[:, 1, :], start=True, stop=True), reads=R + [rc], writes=[P.rpb[bank]])
        pe.op(lambda h: h.matmul(P.pb[bank][:, 8:16], lhsT=onesf, rhs=S_[:, 1, :], start=True, stop=True), reads=R + [rc], writes=[P.rpb[bank]])
        act.op(lambda h: h.activation(out=S_[:, 2:4, :], in_=P.pb[bank][:, 0:16].rearrange("p (a b) -> p a b", a=2), func=AF.Copy),
               reads=[P.rpb[bank]], writes=R)
        act.op(lambda h: h.activation(out=S_[:, 4, :], in_=S_[:, 2, :], func=AF.Exp), reads=R, writes=R)
        dve.op(lambda h: h.tensor_tensor(out=S_[:, 5, :], in0=S_[:, 3, :], in1=S_[:, 2, :], op=ALU.subtract), reads=R, writes=R)
        act.op(lambda h: h.activation(out=S_[:, 5, :], in_=S_[:, 5, :], func=AF.Exp), reads=R, writes=R)
        act.op(lambda h: h.activation(out=S_[:, 6, :], in_=S_[:, 3, :], func=AF.Exp), reads=R, writes=R)
        dve.op(lambda h: h.tensor_tensor(out=S_[:, 7, :], in0=S_[:, 0, :], in1=S_[:, 4, :], op=ALU.mult), reads=R, writes=R)

    def scal(bp, kind, q, hl):
        j = q * 4 + hl
        return sm[:, bp, kind, j:j + 1]

    def precompute(ti):
        bb, q = ti // 2, ti % 2
        bp = bb % 2
        tp = ti % 2
        R = [rsm[bp]]
        tok = slice(q * 128, (q + 1) * 128)
        hs = range(HL)
        KT = lambda hl: qkv[:, 4 + hl, tok]
        QT = lambda hl: qkv[:, hl, tok]
        VT = lambda hl: qkv[:, 8 + hl, tok]
        for hl in hs:
            b1 = nb()
            pe.op(lambda h: h.transpose(out=P.pb[b1][:, 0:128], in_=KT(hl), identity=P.identf), reads=[rqkv[4 + hl], P.rconst], writes=[P.rpb[b1]])
            act.op(lambda h: h.activation(out=t1b[hl], in_=P.pb[b1][:, 0:128], func=AF.Copy), reads=[P.rpb[b1]], writes=[rt1[hl]])
            pool.op(lambda h: h.tensor_scalar(out=Kbg[hl], in0=t1b[hl], scalar1=scal(bp, 7, q, hl), scalar2=None, op0=ALU.mult),
                    reads=[rt1[hl]] + R, writes=[rKbg[hl]])
            dve.op(lambda h: h.tensor_scalar(out=kdb[tp][hl], in0=t1b[hl], scalar1=scal(bp, 5, q, hl), scalar2=None, op0=ALU.mult),
                   reads=[rt1[hl]] + R, writes=[rkd[tp][hl]])
            b2 = nb()
            pe.op(lambda h: h.transpose(out=P.pb[b2][:, 0:128], in_=VT(hl), identity=P.identf), reads=[rqkv[8 + hl], P.rconst], writes=[P.rpb[b2]])
            act.op(lambda h: h.activation(out=Vb[hl], in_=P.pb[b2][:, 0:128], func=AF.Copy, scale=scal(bp, 0, q, hl)),
                   reads=[P.rpb[b2]] + R, writes=[rVb[hl]])
            pool.op(lambda h: h.tensor_copy(out=QTb[tp][hl], in_=QT(hl)), reads=[rqkv[hl]], writes=[rQT[tp][hl]])
            pool.op(lambda h: h.tensor_scalar(out=TriG[hl], in0=Mtri, scalar1=scal(bp, 1, q, hl), scalar2=None, op0=ALU.mult),
                    reads=[rc] + R, writes=[rTriG[hl]])
        if GST < 3:
            return
        for hl in hs:
            b1 = nb()
            pe.op(lambda h: h.matmul(P.pb[b1][:, 0:128], lhsT=TriG[hl], rhs=SL, start=True, stop=True), reads=[rTriG[hl], rc], writes=[P.rpb[b1]])
            act.op(lambda h: h.activation(out=dec[hl], in_=P.pb[b1][:, 0:128], func=AF.Exp), reads=[P.rpb[b1]], writes=[rdec[hl]])
            b2 = nb()
            pe.op(lambda h: h.matmul(P.pb[b2][:, 0:128], lhsT=SL, rhs=TriG[hl], start=True, stop=True), reads=[rTriG[hl], rc], writes=[P.rpb[b2]])
            act.op(lambda h: h.activation(out=decT[hl], in_=P.pb[b2][:, 0:128], func=AF.Exp), reads=[P.rpb[b2]], writes=[rdecT[hl]])
        for hl in hs:
            b1 = nb()
            pe.op(lambda h: h.matmul(P.pb[b1][:, 0:128], lhsT=KT(hl), rhs=KT(hl), start=True, stop=True), reads=[rqkv[4 + hl]], writes=[P.rpb[b1]])
            act.op(lambda h: h.activation(out=stgA[hl], in_=P.pb[b1][:, 0:128], func=AF.Copy), reads=[P.rpb[b1]], writes=[rstgA[hl]])
            dve.op(lambda h: h.tensor_tensor(out=t1b[hl], in0=stgA[hl], in1=dec[hl], op=ALU.mult),
                   reads=[rstgA[hl], rdec[hl]], writes=[rt1[hl]])
            pool.op(lambda h: h.tensor_tensor(out=t1b[hl], in0=t1b[hl], in1=SL, op=ALU.mult),
                    reads=[rt1[hl], rc], writes=[rt1[hl]])
            pool.op(lambda h: h.tensor_scalar(out=Lm[hl], in0=t1b[hl], scalar1=scal(bp, 0, q, hl), scalar2=None, op0=ALU.mult),
                    reads=[rt1[hl]] + R, writes=[rL[hl]])
            b2 = nb()
            pe.op(lambda h: h.matmul(P.pb[b2][:, 0:128], lhsT=KT(hl), rhs=QT(hl), start=True, stop=True), reads=[rqkv[4 + hl], rqkv[hl]], writes=[P.rpb[b2]])
            act.op(lambda h: h.activation(out=stgB[hl], in_=P.pb[b2][:, 0:128], func=AF.Copy), reads=[P.rpb[b2]], writes=[rstgB[hl]])
            dve.op(lambda h: h.tensor_tensor(out=ATb[tp][hl], in0=stgB[hl], in1=decT[hl], op=ALU.mult),
                   reads=[rstgB[hl], rdecT[hl]], writes=[rAT[tp][hl]])
            pool.op(lambda h: h.tensor_tensor(out=ATb[tp][hl], in0=ATb[tp][hl], in1=Mtri, op=ALU.mult),
                    reads=[rAT[tp][hl], rc], writes=[rAT[tp][hl]])
        if GST < 4:
            return
        for hl in hs:
            b1 = nb()
            pe.op(lambda h: h.transpose(out=P.pb[b1][:, 0:128], in_=Lm[hl], identity=P.identf), reads=[rL[hl], P.rconst], writes=[P.rpb[b1]])
            act.op(lambda h: h.activation(out=Nm[hl], in_=P.pb[b1][:, 0:128], func=AF.Copy), reads=[P.rpb[b1]], writes=[rN[hl]])
            dve.op(lambda h: h.tensor_tensor(out=Xa[hl], in0=P.identf, in1=Nm[hl], op=ALU.subtract),
                   reads=[P.rconst, rN[hl]], writes=[rXa[hl]])
        if GST < 5:
            return
        Pc = [(Nm[hl], rN[hl]) for hl in hs]
        PTc = [(Lm[hl], rL[hl]) for hl in hs]
        Xc = [(Xa[hl], rXa[hl]) for hl in hs]
        for lvl in range(1, 7):
            newP, newPT, newX = [], [], []
            for hl in hs:
                (p_, rp_), (pt_, rpt_), (x_, rx_) = Pc[hl], PTc[hl], Xc[hl]
                pn, rpn = (Pa[hl], rPa[hl]) if lvl % 2 == 1 else (Pb_[hl], rPb[hl])
                ptn, rptn = (PTa[hl], rPTa[hl]) if lvl % 2 == 1 else (PTb[hl], rPTb[hl])
                xn, rxn = (Xb_[hl], rXb[hl]) if lvl % 2 == 1 else (Xa[hl], rXa[hl])
                if lvl < 6:
                    b1 = nb()
                    pe.op(lambda h: h.matmul(P.pb[b1][:, 0:128], lhsT=pt_, rhs=p_, start=True, stop=True), reads=[rpt_, rp_], writes=[P.rpb[b1]])
                    act.op(lambda h: h.activation(out=pn, in_=P.pb[b1][:, 0:128], func=AF.Copy), reads=[P.rpb[b1]], writes=[rpn])
                b2 = nb()
                pe.op(lambda h: h.matmul(P.pb[b2][:, 0:128], lhsT=p_, rhs=pt_, start=True, stop=True), reads=[rpt_, rp_], writes=[P.rpb[b2]])
                act.op(lambda h: h.activation(out=ptn, in_=P.pb[b2][:, 0:128], func=AF.Copy), reads=[P.rpb[b2]], writes=[rptn])
                newP.append((pn, rpn))
                newPT.append((ptn, rptn))
                newX.append((xn, rxn))
            for hl in hs:
                (x_, rx_) = Xc[hl]
                (ptn, rptn) = newPT[hl]
                (xn, rxn) = newX[hl]
                b3 = nb()
                pe.op(lambda h: h.matmul(P.pb[b3][:, 0:128], lhsT=ptn, rhs=x_, start=True, stop=True), reads=[rptn, rx_], writes=[P.rpb[b3]])
                act.op(lambda h: h.activation(out=stgA[hl], in_=P.pb[b3][:, 0:128], func=AF.Copy), reads=[P.rpb[b3]], writes=[rstgA[hl]])
                dve.op(lambda h: h.tensor_tensor(out=xn, in0=stgA[hl], in1=x_, op=ALU.add), reads=[rstgA[hl], rx_], writes=[rxn])
            Pc, PTc, Xc = newP, newPT, newX
        if GST < 6:
            return
        for hl in hs:
            (x_, rx_) = Xc[hl]
            b1 = nb()
            pe.op(lambda h: h.matmul(P.pb[b1][:, 0:128], lhsT=x_, rhs=Vb[hl], start=True, stop=True), reads=[rx_, rVb[hl]], writes=[P.rpb[b1]])
            act.op(lambda h: h.activation(out=Ub[tp][hl], in_=P.pb[b1][:, 0:128], func=AF.Copy), reads=[P.rpb[b1]], writes=[rU[tp][hl]])
            b2 = nb()
            pe.op(lambda h: h.matmul(P.pb[b2][:, 0:128], lhsT=Kbg[hl], rhs=x_, start=True, stop=True), reads=[rx_, rKbg[hl]], writes=[P.rpb[b2]])
            act.op(lambda h: h.activation(out=WTb[tp][hl], in_=P.pb[b2][:, 0:128], func=AF.Copy), reads=[P.rpb[b2]], writes=[rWT[tp][hl]])

    def scan(ti):
        bb, q = ti // 2, ti % 2
        bp = bb % 2
        tp = ti % 2
        si, so = ti % 2, (ti + 1) % 2
        R = [rsm[bp]]
        hs = range(HL)
        bpv, bpo1, bpo2, bpS = {}, {}, {}, {}
        for hl in hs:
            bpv[hl] = nb()
            pe.op(lambda h: h.matmul(P.pb[bpv[hl]][:, 0:128], lhsT=WTb[tp][hl], rhs=Sst[si][hl], start=True, stop=True),
                  reads=[rWT[tp][hl], rS[si][hl]], writes=[P.rpb[bpv[hl]]])
            act.op(lambda h: h.activation(out=stgA[hl], in_=P.pb[bpv[hl]][:, 0:128], func=AF.Copy), reads=[P.rpb[bpv[hl]]], writes=[rstgA[hl]])
            dve.op(lambda h: h.tensor_tensor(out=vnew[hl], in0=Ub[tp][hl], in1=stgA[hl], op=ALU.subtract),
                   reads=[rU[tp][hl], rstgA[hl]], writes=[rvnew[hl]])
        for hl in hs:
            bpo1[hl] = nb()
            pe.op(lambda h: h.matmul(P.pb[bpo1[hl]][:, 0:128], lhsT=QTb[tp][hl], rhs=Sst[si][hl], start=True, stop=True),
                  reads=[rQT[tp][hl], rS[si][hl]], writes=[P.rpb[bpo1[hl]]])
            act.op(lambda h: h.activation(out=o1s[hl], in_=P.pb[bpo1[hl]][:, 0:128], func=AF.Copy, scale=scal(bp, 4, q, hl)),
                   reads=[P.rpb[bpo1[hl]]] + R, writes=[ro1s[hl]])
        for hl in hs:
            bpS[hl] = nb()
            pe.op(lambda h: h.matmul(P.pb[bpS[hl]][:, 0:128], lhsT=kdb[tp][hl], rhs=vnew[hl], start=True, stop=True),
                  reads=[rkd[tp][hl], rvnew[hl]], writes=[P.rpb[bpS[hl]]])
            act.op(lambda h: h.activation(out=stgB[hl], in_=P.pb[bpS[hl]][:, 0:128], func=AF.Copy), reads=[P.rpb[bpS[hl]]], writes=[rstgB[hl]])
            dve.op(lambda h: h.scalar_tensor_tensor(out=Sst[so][hl], in0=Sst[si][hl], scalar=scal(bp, 6, q, hl), in1=stgB[hl],
                                                    op0=ALU.mult, op1=ALU.add), reads=[rS[si][hl], rstgB[hl]] + R, writes=[rS[so][hl]])
        for hl in hs:
            bpo2[hl] = nb()
            pe.op(lambda h: h.matmul(P.pb[bpo2[hl]][:, 0:128], lhsT=ATb[tp][hl], rhs=vnew[hl], start=True, stop=True),
                  reads=[rAT[tp][hl], rvnew[hl]], writes=[P.rpb[bpo2[hl]]])
            act.op(lambda h: h.activation(out=stgA[hl], in_=P.pb[bpo2[hl]][:, 0:128], func=AF.Copy), reads=[P.rpb[bpo2[hl]]], writes=[rstgA[hl]])
            dve.op(lambda h: h.tensor_tensor(out=otok[hl], in0=o1s[hl], in1=stgA[hl], op=ALU.add),
                   reads=[ro1s[hl], rstgA[hl]], writes=[rotok[hl]])
        og_ = ogt[tp]
        for hl in hs:
            cc = cols[:, 1 + hl:2 + hl]
            rms_rstd(P, otok[hl], rotok[hl], junk[:, 0:128], rjunk, cc, rcols, ncols=128)
            dve.op(lambda h: h.scalar_tensor_tensor(out=otok[hl], in0=otok[hl], scalar=cc, in1=gnw, op0=ALU.mult, op1=ALU.mult),
                   reads=[rotok[hl], rcols, rc], writes=[rotok[hl]])
            pool.op(lambda h: h.tensor_tensor(out=og_[:, hl * 128:(hl + 1) * 128], in0=otok[hl], in1=zs[bp][:, q, hl * 128:(hl + 1) * 128], op=ALU.mult),
                    reads=[rotok[hl], rzs[bp][q]], writes=[rogt[tp]])
        sp.dma(sog[tp], lambda h: h.dma_start(out=out[ti * 128:(ti + 1) * 128, :], in_=og_), reads=[rogt[tp]])

    project_block(0)
    if GST >= 2:
        precompute(0)
    for ti in range(GTR):
        nxt = ti + 1
        if nxt < GTR:
            if nxt % 2 == 0:
                project_block(nxt // 2)
            if GST >= 2:
                precompute(nxt)
        if GST >= 7:
            scan(ti)
    for s_ in sog:
        sp.wait_clock(s_)
    K.barrier()
    A.release(m0)
    P.stream_release(sm0)


def build_prog2():
    nc = bass.Bass("TRN2", target_bir_lowering=False)
    with ExitStack() as st:
        P = Prog(nc, st)
        K, A = P.K, P.arena
        x_in = P.dram("x_in", [SEQ, D], F32, "ExternalInput")
        ad = adaln_inputs(P, "1")
        wq_in = P.dram("wqkvz", [D, 2048], F32, "ExternalInput")
        wba_in = P.dram("wba", [D, 8], F32, "ExternalInput")
        cw_in = P.dram("gcw", [128, 48], F32, "ExternalInput")
        hp_in = P.dram("hp", [1, 8], F32, "ExternalInput")
        gnw_in = P.dram("gnw", [1, 128], F32, "ExternalInput")
        out = P.dram("og_out", [SEQ, 512], BF16, "ExternalOutput")
        mod = A.alloc([128, 6, D], F32)
        rmod = K.regions(6, "mod")
        phase_adaln(P, mod, rmod, ad["c_col"], ad["ada_w"], ad["ada_b"], ad["norm_w"])
        phase_gdn(P, mod, rmod, x_in, None, wq_in, wba_in, cw_in, hp_in, gnw_in, out)
        K.finish()
        print("prog2 ops", K.nops, "dma", K.ndma, "arena peak", A.peak)
    return nc


def run_prog2(inputs, x1):
    nc = build_prog2()
    i = 1
    com = {"ada_w1": _c(inputs["ada_w"][i]), "ada_b1": _c(inputs["ada_b"][i][None, :]), "norm_w1": _c(inputs["norm_w"][i])}
    gw = np.asarray(inputs["gdn_in_w"][0])
    gcw = np.asarray(inputs["gdn_conv_w"][0])
    in_maps = []
    for core in range(NCORES):
        b, hh = core // 2, core % 2
        m = dict(com)
        cs = slice(hh * 512, (hh + 1) * 512)
        m["wqkvz"] = _c(np.concatenate([gw[:, 0:1024][:, cs], gw[:, 1024:2048][:, cs], gw[:, 2048:3072][:, cs], gw[:, 3072:4096][:, cs]], axis=1))
        m["wba"] = _c(np.concatenate([gw[:, 4096 + hh * 4:4096 + hh * 4 + 4], gw[:, 4104 + hh * 4:4104 + hh * 4 + 4]], axis=1))
        secs = [gcw[:, s0 + hh * 512:s0 + (hh + 1) * 512] for s0 in (0, 1024, 2048)]
        cwc = np.concatenate(secs, axis=1).reshape(4, 12, 128)
        m["gcw"] = _c(cwc.transpose(2, 1, 0).reshape(128, 48))
        m["hp"] = _c(np.concatenate([np.asarray(inputs["gdn_a_log"][0])[hh * 4:hh * 4 + 4],
                                     np.asarray(inputs["gdn_dt_bias"][0])[hh * 4:hh * 4 + 4]])[None, :])
        m["gnw"] = _c(np.asarray(inputs["gdn_norm_w"][0])[None, :])
        m["x_in"] = _c(x1[b])
        m["c_col1"] = c_col_of(inputs["c"], b)
        in_maps.append(m)
    import os
    ncr = int(os.environ.get("NCORES_RUN", NCORES))
    res = run_bass_kernel_spmd(nc, in_maps[:ncr], core_ids=list(range(ncr)))
    r0 = np.asarray(res.results[0]["og_out"])
    o = np.zeros((4, SEQ, D), r0.dtype)
    for core in range(ncr):
        b, hh = core // 2, core % 2
        o[b, :, hh * 512:(hh + 1) * 512] = np.asarray(res.results[core]["og_out"])
    return o


def phase_outproj(P, xres, rx, mod, rmod, og_in, w_out, gather=None):
    K, A = P.K, P.arena
    pe, act, dve, pool, sp = P.pe, P.act, P.dve, P.pool, P.sp
    m0 = A.mark()
    sm0 = P.stream_mark()
    if gather is not None:
        tidx = A.alloc([128, 2 * NT], I32)
        rtidx = K.region()
        s = P.stream()
        sp.dma(s, lambda h: h.dma_start(out=tidx, in_=gather[1]), writes=[rtidx])
    Wout = A.alloc([128, 8, D], BF16)
    rWout = K.region()
    s = P.stream(sw=True)
    pool.dma(s, lambda h: h.dma_start(out=Wout, in_=w_out.rearrange("(k p) n -> p k n", p=128)), writes=[rWout])
    ogb = [A.alloc([128, D], BF16) for _ in range(2)]
    rogb = K.regions(2, "ogb")
    sogb = [P.stream(), P.stream()]
    sogb2 = [[P.stream(sw=True), P.stream(sw=True)], [P.stream(sw=True), P.stream(sw=True)]]
    rogb2 = [K.regions(2, "ogb2a"), K.regions(2, "ogb2b")]
    ogh2 = [[A.alloc([128, 512], BF16) for _ in range(2)] for _ in range(2)] if gather is not None else None
    ogT = [A.alloc([128, 8, 128], BF16) for _ in range(2)]
    rogT = K.regions(2, "ogT")
    ysb = A.alloc([128, D], F32)
    junk = A.alloc([128, D], BF16)
    tmp = A.alloc([128, D], F32)
    cols = A.alloc([128, 4], F32)
    rysb, rjunk, rtmp, rcols = (K.region() for _ in range(4))
    for ti in range(NT):
        sl = ti % 2
        if gather is None:
            sp.dma(sogb[sl], lambda h: h.dma_start(out=ogb[sl], in_=og_in[ti * 128:(ti + 1) * 128, :]), writes=[rogb[sl]])
        else:
            for r in range(2):
                pool.dma(sogb2[sl][r], lambda h: h.indirect_dma_start(
                    out=ogh2[sl][r], out_offset=None, in_=gather[0],
                    in_offset=bass.IndirectOffsetOnAxis(ap=tidx[:, ti * 2 + r:ti * 2 + r + 1], axis=0)),
                    reads=[rtidx], writes=[rogb2[sl][r]])
        bank = sl
        pst = P.pb[bank][:, :].bitcast(BF16).rearrange("p (k n) -> p k n", k=8)
        for k in range(8):
            src_ = ogb[sl][:, k * 128:(k + 1) * 128] if gather is None else ogh2[sl][k // 4][:, (k % 4) * 128:(k % 4 + 1) * 128]
            pe.op(lambda h, k=k: h.transpose(out=pst[:, k, :], in_=src_, identity=P.identb),
                  reads=[rogb[sl], rogb2[sl][k // 4], P.rconst], writes=[P.rpb[bank]])
        act.op(lambda h: h.activation(out=ogT[sl], in_=pst, func=AF.Copy), reads=[P.rpb[bank]], writes=[rogT[sl]])
        for nh in range(2):
            b2 = 2 + sl * 2 + nh
            for k in range(8):
                pe.op(lambda h, k=k: h.matmul(P.pb[b2][:, :], lhsT=ogT[sl][:, k, :], rhs=Wout[:, k, nh * 512:(nh + 1) * 512],
                                              start=(k == 0), stop=(k == 7)), reads=[rogT[sl], rWout], writes=[P.rpb[b2]])
            act.op(lambda h: h.activation(out=ysb[:, nh * 512:(nh + 1) * 512], in_=P.pb[b2][:, :], func=AF.Copy),
                   reads=[P.rpb[b2]], writes=[rysb])
        rms_rstd(P, ysb, rysb, junk, rjunk, cols[:, 0:1], rcols)
        dve.op(lambda h: h.scalar_tensor_tensor(out=tmp, in0=ysb, scalar=cols[:, 0:1], in1=mod[:, 2, :], op0=ALU.mult, op1=ALU.mult),
               reads=[rysb, rcols, rmod[2]], writes=[rtmp])
        dve.op(lambda h: h.tensor_tensor(out=xres[:, ti, :], in0=xres[:, ti, :], in1=tmp, op=ALU.add),
               reads=[rx[ti], rtmp], writes=[rx[ti]])
    K.barrier()
    A.release(m0)
    P.stream_release(sm0)


def build_prog3(stop_after=None):
    nc = bass.Bass("TRN2", target_bir_lowering=False)
    with ExitStack() as st:
        P = Prog(nc, st)
        K, A = P.K, P.arena
        x_in = P.dram("x_in", [TOK, D], F32, "ExternalInput")
        og_in = P.dram("og_in", [TOK, D], BF16, "ExternalInput")
        ad = adaln_inputs(P, "1")
        w_out = P.dram("gdn_out_w", [D, D], F32, "ExternalInput")
        ec = P.dram("ec", [128, 65], F32, "ExternalInput")
        mo = moe_inputs(P, "1")
        xg = P.dram("xg", [NSLOT + 128, D], BF16, "Internal")
        og = P.dram("og", [NSLOT + 128, D], BF16, "Internal")
        out = P.dram("x_out", [TOK, D], F32, "ExternalOutput")
        xres = A.alloc([128, NT, D], F32)
        rx = K.regions(NT, "x")
        mod = A.alloc([128, 6, D], F32)
        rmod = K.regions(6, "mod")
        load_x(P, xres, rx, x_in)
        phase_adaln(P, mod, rmod, ad["c_col"], ad["ada_w"], ad["ada_b"], ad["norm_w"])
        phase_outproj(P, xres, rx, mod, rmod, og_in, w_out)
        if stop_after != "outproj":
            phase_moe(P, xres, rx, mod, rmod, mo["wr"], mo["rb"], ec, mo["wg"], mo["wu"], mo["wd"],
                      mo["sg"], mo["su"], mo["sd"], xg, og)
        store_x(P, xres, rx, out)
        K.finish()
        print("prog3 ops", K.nops, "dma", K.ndma, "arena peak", A.peak)
    return nc


def run_prog3(inputs, x1, ogf, stop_after=None):
    nc = build_prog3(stop_after)
    com = host_common(inputs, 1)
    com["gdn_out_w"] = _c(inputs["gdn_out_w"][0])
    com["ec"] = EC
    in_maps = []
    for core in range(NCORES):
        b, hf = core // 2, core % 2
        m = dict(com)
        m["x_in"] = _c(x1[b, hf * TOK:(hf + 1) * TOK])
        m["og_in"] = np.ascontiguousarray(ogf[b, hf * TOK:(hf + 1) * TOK])
        m["c_col1"] = c_col_of(inputs["c"], b)
        in_maps.append(m)
    res = run_bass_kernel_spmd(nc, in_maps, core_ids=list(range(NCORES)))
    return np.stack([r["x_out"] for r in res.results]).reshape(4, SEQ, D)


def kernel_unfused(**inputs):
    inputs = {k: np.asarray(v) for k, v in inputs.items()}
    x1 = run_prog1(inputs)
    og = run_prog2(inputs, x1)
    out = run_prog3(inputs, x1, og)
    return out.astype(np.float32)


PAIRS = [[0, 1], [2, 3], [4, 5], [6, 7]]


def build_fused():
    nc = bass.Bass("TRN2", target_bir_lowering=False)
    with ExitStack() as st:
        P = Prog(nc, st)
        K, A = P.K, P.arena
        pe, act, dve, pool, sp = P.pe, P.act, P.dve, P.pool, P.sp
        x_in = P.dram("x_in", [TOK, D], F32, "ExternalInput")
        x_halo = P.dram("x_halo", [2, D], F32, "ExternalInput")
        hflag = P.dram("hflag", [128, 1], F32, "ExternalInput")
        c_col = P.dram("c_col", [128, 8], F32, "ExternalInput")
        ad = []
        for t in ("0", "1"):
            ad.append(dict(ada_w=P.dram(f"ada_w{t}", [D, 6 * D], F32, "ExternalInput"),
                           ada_b=P.dram(f"ada_b{t}", [1, 6 * D], F32, "ExternalInput"),
                           norm_w=P.dram(f"norm_w{t}", [4, D], F32, "ExternalInput")))
        w_in = P.dram("conv_in_w", [D, 3 * D], F32, "ExternalInput")
        cw = P.dram("conv_cw", [128, 24], F32, "ExternalInput")
        w_out = P.dram("conv_out_w", [D, D], F32, "ExternalInput")
        ec = P.dram("ec", [128, 65], F32, "ExternalInput")
        mo0 = moe_inputs(P, "0")
        mo1 = moe_inputs(P, "1")
        wq_in = P.dram("wqkvz", [D, 2048], F32, "ExternalInput")
        wba_in = P.dram("wba", [D, 8], F32, "ExternalInput")
        gcw_in = P.dram("gcw", [128, 48], F32, "ExternalInput")
        hp_in = P.dram("hp", [1, 8], F32, "ExternalInput")
        gnw_in = P.dram("gnw", [1, 128], F32, "ExternalInput")
        gout_w = P.dram("gdn_out_w", [D, D], F32, "ExternalInput")
        tokidx = P.dram("tokidx", [128, 2 * NT], I32, "ExternalInput")
        xg = P.dram("xg", [NSLOT + 128, D], BF16, "Internal")
        og = P.dram("og", [NSLOT + 128, D], BF16, "Internal")
        hsh = P.dram("hsh", [TOK, D], BF16, "Internal")
        hfull = P.dram("hfull", [SEQ, D], BF16, "Internal")
        ogh = P.dram("ogh", [SEQ, 512], BF16, "Internal")
        ogfull = P.dram("ogfull", [2 * SEQ, 512], BF16, "Internal")
        xsp = P.dram("xsp", [TOK, D], F32, "Internal")
        out = P.dram("x_out", [TOK, D], F32, "ExternalOutput")

        mod = A.alloc([128, 6, D], F32)
        rmod = K.regions(6, "mod")
        mx = A.mark()
        xres = A.alloc([128, NT, D], F32)
        rx = K.regions(NT, "x")
        import os
        FS = os.environ.get("FUSED_STOP", "")
        load_x(P, xres, rx, x_in)
        phase_adaln(P, mod, rmod, c_col, ad[0]["ada_w"], ad[0]["ada_b"], ad[0]["norm_w"])
        if FS != "skipl0":
            phase_conv(P, xres, rx, mod, rmod, x_halo, hflag, w_in, cw, w_out)
            phase_moe(P, xres, rx, mod, rmod, mo0["wr"], mo0["rb"], ec, mo0["wg"], mo0["wu"], mo0["wd"],
                      mo0["sg"], mo0["su"], mo0["sd"], xg, og)
        phase_adaln(P, mod, rmod, c_col, ad[1]["ada_w"], ad[1]["ada_b"], ad[1]["norm_w"])
        m1 = A.mark()
        sm1 = P.stream_mark()
        hb = [A.alloc([128, D], BF16) for _ in range(2)]
        rhb = K.regions(2, "hb1")
        shb = [P.stream(), P.stream()]
        junk = A.alloc([128, D], BF16)
        tmp = A.alloc([128, D], F32)
        cols = A.alloc([128, 4], F32)
        rjunk, rtmp, rcols = K.region(), K.region(), K.region()
        rhsh = K.regions(NT, "hsh")
        for ti in range(NT):
            sl = ti % 2
            rms_rstd(P, xres[:, ti, :], rx[ti], junk, rjunk, cols[:, 0:1], rcols)
            dve.op(lambda h: h.scalar_tensor_tensor(out=tmp, in0=xres[:, ti, :], scalar=cols[:, 0:1], in1=mod[:, 0, :],
                                                    op0=ALU.mult, op1=ALU.mult), reads=[rx[ti], rcols, rmod[0]], writes=[rtmp])
            dve.op(lambda h: h.tensor_tensor(out=hb[sl], in0=tmp, in1=mod[:, 1, :], op=ALU.add),
                   reads=[rtmp, rmod[1]], writes=[rhb[sl]])
            sp.dma(shb[sl], lambda h: h.dma_start(out=hsh[ti * 128:(ti + 1) * 128, :], in_=hb[sl]), reads=[rhb[sl]], writes=[rhsh[ti]])
        ssp = P.stream()
        for g in range(4):
            sp.dma(ssp, lambda h, g=g: h.dma_start(out=xsp[g * 512:(g + 1) * 512, :].rearrange("(t p) d -> p t d", p=128),
                                                   in_=xres[:, g * 4:(g + 1) * 4, :]), reads=rx[g * 4:(g + 1) * 4])
        K.barrier()
        for c in range(2):
            scc = P.stream(sw=True)
            pool.dma(scc, lambda h, c=c: h.collective_compute(
                "AllGather", op=ALU.bypass, replica_groups=PAIRS,
                ins=[hsh[c * 1024:(c + 1) * 1024, :].opt()], outs=[hfull[c * 2048:(c + 1) * 2048, :].opt()]), inc=1)
        K.barrier()
        A.release(mx)
        P.stream_release(sm1)
        if FS != "nogdn":
            phase_gdn(P, mod, rmod, None, hfull, wq_in, wba_in, gcw_in, hp_in, gnw_in, ogh)
        for c in range(2):
            scc = P.stream(sw=True)
            pool.dma(scc, lambda h, c=c: h.collective_compute(
                "AllGather", op=ALU.bypass, replica_groups=PAIRS,
                ins=[ogh[c * 2048:(c + 1) * 2048, :].opt()], outs=[ogfull[c * 4096:(c + 1) * 4096, :].opt()]), inc=1)
        xres = A.alloc([128, NT, D], F32)
        load_x(P, xres, rx, xsp)
        K.barrier()
        if FS not in ("reload",):
            phase_outproj(P, xres, rx, mod, rmod, None, gout_w, gather=(ogfull, tokidx))
        if FS not in ("reload", "outproj"):
            phase_moe(P, xres, rx, mod, rmod, mo1["wr"], mo1["rb"], ec, mo1["wg"], mo1["wu"], mo1["wd"],
                      mo1["sg"], mo1["su"], mo1["sd"], xg, og)
        store_x(P, xres, rx, out)
        K.finish()
        print("fused ops", K.nops, "dma", K.ndma, "arena peak", A.peak, "sems", K._n)
    return nc


def fused_in_maps(inputs):
    x = np.asarray(inputs["x"])
    com = {}
    com.update(host_common(inputs, 0))
    com.update(host_common(inputs, 1))
    com["conv_in_w"] = _c(inputs["conv_in_w"][0])
    com["conv_out_w"] = _c(inputs["conv_out_w"][0])
    com["conv_cw"] = _c(np.asarray(inputs["conv_w"][0]).reshape(3, 8, 128).transpose(2, 1, 0).reshape(128, 24))
    com["ec"] = EC
    com["gdn_out_w"] = _c(inputs["gdn_out_w"][0])
    com["gnw"] = _c(np.asarray(inputs["gdn_norm_w"][0])[None, :])
    gw = np.asarray(inputs["gdn_in_w"][0])
    gcw = np.asarray(inputs["gdn_conv_w"][0])
    in_maps = []
    for core in range(NCORES):
        b, hf = core // 2, core % 2
        hh = hf
        m = dict(com)
        m["x_in"] = _c(x[b, hf * TOK:(hf + 1) * TOK])
        m["x_halo"] = _c(x[b, TOK - 2:TOK]) if hf == 1 else np.zeros((2, D), np.float32)
        m["hflag"] = np.full((128, 1), float(hf), np.float32)
        m["c_col"] = c_col_of(inputs["c"], b)
        cs = slice(hh * 512, (hh + 1) * 512)
        m["wqkvz"] = _c(np.concatenate([gw[:, 0:1024][:, cs], gw[:, 1024:2048][:, cs], gw[:, 2048:3072][:, cs], gw[:, 3072:4096][:, cs]], axis=1))
        m["wba"] = _c(np.concatenate([gw[:, 4096 + hh * 4:4096 + hh * 4 + 4], gw[:, 4104 + hh * 4:4104 + hh * 4 + 4]], axis=1))
        secs = [gcw[:, s0 + hh * 512:s0 + (hh + 1) * 512] for s0 in (0, 1024, 2048)]
        cwc = np.concatenate(secs, axis=1).reshape(4, 12, 128)
        m["gcw"] = _c(cwc.transpose(2, 1, 0).reshape(128, 48))
        m["hp"] = _c(np.concatenate([np.asarray(inputs["gdn_a_log"][0])[hh * 4:hh * 4 + 4],
                                     np.asarray(inputs["gdn_dt_bias"][0])[hh * 4:hh * 4 + 4]])[None, :])
        ti = np.arange(NT)[None, :, None]
        r = np.arange(2)[None, None, :]
        p = np.arange(128)[:, None, None]
        m["tokidx"] = np.ascontiguousarray((hf * SEQ + r * TOK + ti * 128 + p).reshape(128, 2 * NT).astype(np.int32))
        in_maps.append(m)
    return in_maps


def kernel_fused(**inputs):
    inputs = {k: np.asarray(v) for k, v in inputs.items()}
    nc = build_fused()
    in_maps = fused_in_maps(inputs)
    res = run_bass_kernel_spmd(nc, in_maps, core_ids=list(range(NCORES)))
    return np.stack([r["x_out"] for r in res.results]).reshape(4, SEQ, D).astype(np.float32)


def kernel(**inputs):
    return kernel_fused(**inputs)
```
